# Optimizing a Trainium2 kernel written in Bass

```python
import math
import jax, jax.numpy as jnp
from jax import lax
import numpy as np

D_MODEL = 1024
BATCH = 2
SEQ = 16384
DEPTH = 4

GRID_W = 64
CTX_LEN = 256
HEAD_DIM = 128
ATTN_HEADS = 4
ATTN_KV_HEADS = 2
DN_HEADS = 4
ATTN_Q_W = ATTN_HEADS * HEAD_DIM
ATTN_KV_W = ATTN_KV_HEADS * HEAD_DIM
DN_W = DN_HEADS * HEAD_DIM
MIX_WIDTH = ATTN_Q_W + DN_W
IN_SPLITS = (ATTN_Q_W, ATTN_KV_W, ATTN_KV_W, 3 * DN_W, DN_W, 2 * DN_HEADS, 2 * DN_HEADS)
IN_WIDTH = sum(IN_SPLITS)
ROPE_THETA = 10000.0
Q_BLOCK = 128
DN_CHUNK = 64
CONV_K = 5
PEER_HEADS = 8
PEER_QDIM = 256
PEER_HALF = PEER_QDIM // 2
N_KEYS = 128
N_EXPERTS = N_KEYS * N_KEYS
PEER_TOPK = 16
PEER_BLOCK = 128
EPS = 1e-6

kernel_name = 'hybrid_gqa_deltanet_peer_dit'


def rmsnorm(x, w):
    xf = x.astype(jnp.float32)
    y = xf * lax.rsqrt(jnp.mean(xf * xf, axis=-1, keepdims=True) + EPS)
    return (y * w.astype(jnp.float32)).astype(x.dtype)


def l2norm(x):
    return x * lax.rsqrt(jnp.sum(x * x, axis=-1, keepdims=True) + EPS)


def axial_rope_tables(n_tokens):
    rows = n_tokens // GRID_W
    row = jnp.repeat(jnp.arange(rows, dtype=jnp.float32), GRID_W)
    col = jnp.tile(jnp.arange(GRID_W, dtype=jnp.float32), rows)
    axis_dim = HEAD_DIM // 2
    inv_freq = ROPE_THETA ** (-jnp.arange(0, axis_dim, 2, dtype=jnp.float32) / axis_dim)
    ang_r = row[:, None] * inv_freq[None, :]
    ang_c = col[:, None] * inv_freq[None, :]
    ang = jnp.concatenate([ang_r, ang_r, ang_c, ang_c], axis=-1)
    return jnp.cos(ang), jnp.sin(ang)


def apply_axial_rope(x, cos, sin):
    B, T, H, Dh = x.shape
    xr = x.reshape(B, T, H, 2, 2, Dh // 4)
    rot = jnp.stack([-xr[..., 1, :], xr[..., 0, :]], axis=-2).reshape(B, T, H, Dh)
    return (x * cos[None, :, None, :] + rot * sin[None, :, None, :]).astype(x.dtype)


def split_in(p):
    offs = np.cumsum(IN_SPLITS)[:-1].tolist()
    return jnp.split(p, offs, axis=-1)


def attn_group(qa, ka, va, qn_w, kn_w, rope):
    B, T, _ = qa.shape
    q = rmsnorm(qa.reshape(B, T, ATTN_HEADS, HEAD_DIM), qn_w)
    k = rmsnorm(ka.reshape(B, T, ATTN_KV_HEADS, HEAD_DIM), kn_w)
    v = va.reshape(B, T, ATTN_KV_HEADS, HEAD_DIM)
    if rope is not None:
        q = apply_axial_rope(q, rope[0], rope[1])
        k = apply_axial_rope(k, rope[0], rope[1])
    return q, k, v


def latent_attention(q, k_lat, v_lat, k_ctx, v_ctx):
    B, T = q.shape[:2]
    G = ATTN_HEADS // ATTN_KV_HEADS
    scale = HEAD_DIM ** -0.5
    k_all = jnp.concatenate([k_lat, k_ctx], axis=1).transpose(0, 2, 1, 3)
    v_all = jnp.concatenate([v_lat, v_ctx], axis=1).transpose(0, 2, 1, 3)
    nb = T // Q_BLOCK
    qb = q.reshape(B, nb, Q_BLOCK, ATTN_KV_HEADS, G, HEAD_DIM).transpose(1, 0, 3, 4, 2, 5)

    def block(qi):
        s = jnp.einsum('bhgqd,bhkd->bhgqk', qi, k_all).astype(jnp.float32) * scale
        p = jax.nn.softmax(s, axis=-1).astype(v_all.dtype)
        return jnp.einsum('bhgqk,bhkd->bhgqd', p, v_all)

    o = lax.map(block, qb)
    return o.transpose(1, 0, 4, 2, 3, 5).reshape(B, T, ATTN_Q_W)


def context_attention(q, k, v):
    B, T = q.shape[:2]
    G = ATTN_HEADS // ATTN_KV_HEADS
    qg = q.reshape(B, T, ATTN_KV_HEADS, G, HEAD_DIM)
    s = jnp.einsum('bqhgd,bkhd->bhgqk', qg, k).astype(jnp.float32) * (HEAD_DIM ** -0.5)
    p = jax.nn.softmax(s, axis=-1).astype(v.dtype)
    o = jnp.einsum('bhgqk,bkhd->bqhgd', p, v)
    return o.reshape(B, T, ATTN_Q_W)


def centred_short_conv(x, w):
    pad = CONV_K // 2
    T = x.shape[1]
    xp = jnp.pad(x, ((0, 0), (pad, pad), (0, 0)))
    y = xp[:, 0:T] * w[0]
    for j in range(1, CONV_K):
        y = y + xp[:, j:j + T] * w[j]
    return jax.nn.silu(y)


def gated_delta_chunked(q, k, v, g, beta, S0):
    B, T, H, Dk = q.shape
    C = DN_CHUNK
    N = T // C
    qc, kc, vc = [t.reshape(B, N, C, H, -1).transpose(1, 0, 3, 2, 4) for t in (q, k, v)]
    gcum = jnp.cumsum(g.reshape(B, N, C, H).transpose(1, 0, 3, 2), axis=-1)
    bc = beta.reshape(B, N, C, H).transpose(1, 0, 3, 2)
    idx = jnp.arange(C)
    incl = idx[:, None] >= idx[None, :]
    strict = idx[:, None] > idx[None, :]
    decay = jnp.exp(jnp.where(incl, gcum[..., :, None] - gcum[..., None, :], -jnp.inf))
    kb = kc * bc[..., None]
    A = jnp.where(strict, jnp.einsum('nbhid,nbhjd->nbhij', kb, kc) * decay, 0.0)
    Tm = jnp.eye(C, dtype=jnp.float32) + A
    u = lax.linalg.triangular_solve(Tm, vc * bc[..., None], left_side=True, lower=True, unit_diagonal=True)
    w = lax.linalg.triangular_solve(Tm, kb * jnp.exp(gcum)[..., None], left_side=True, lower=True, unit_diagonal=True)
    qk = jnp.einsum('nbhid,nbhjd->nbhij', qc, kc) * decay

    def step(S, xs):
        q_i, k_i, u_i, w_i, g_i, qk_i = xs
        v_new = u_i - jnp.einsum('bhcd,bhde->bhce', w_i, S)
        o_i = (jnp.einsum('bhcd,bhde->bhce', q_i * jnp.exp(g_i)[..., None], S)
               + jnp.einsum('bhij,bhje->bhie', qk_i, v_new))
        g_last = g_i[..., -1:]
        S = (S * jnp.exp(g_last)[..., None]
             + jnp.einsum('bhcd,bhce->bhde', k_i * jnp.exp(g_last - g_i)[..., None], v_new))
        return S, o_i

    S_final, o = lax.scan(step, S0, (qc, kc, u, w, gcum, qk))
    o = o.transpose(1, 0, 3, 2, 4).reshape(B, T, H, v.shape[-1])
    return o, S_final


def dn_prepare(qkv, b_fb, a_fb, conv_w, A_log, dt_bias):
    B, T, _ = qkv.shape
    y = centred_short_conv(qkv, conv_w).astype(jnp.float32)
    q, k, v = jnp.split(y, 3, axis=-1)
    q = l2norm(q.reshape(B, T, DN_HEADS, HEAD_DIM)) * (HEAD_DIM ** -0.5)
    k = l2norm(k.reshape(B, T, DN_HEADS, HEAD_DIM))
    v = v.reshape(B, T, DN_HEADS, HEAD_DIM)
    a = a_fb.astype(jnp.float32).reshape(B, T, 2, DN_HEADS)
    g = -jnp.exp(A_log.astype(jnp.float32)) * jax.nn.softplus(a + dt_bias.astype(jnp.float32))
    beta = jax.nn.sigmoid(b_fb.astype(jnp.float32).reshape(B, T, 2, DN_HEADS))
    return q, k, v, g, beta


def dn_bidirectional(lat, ctx):
    q_l, k_l, v_l, g_l, b_l = lat
    q_c, k_c, v_c, g_c, b_c = ctx
    B = q_l.shape[0]
    S0 = jnp.zeros((B, DN_HEADS, HEAD_DIM, HEAD_DIM), jnp.float32)

    def flip(t):
        return jnp.flip(t, axis=1)

    o_cf, S_f = gated_delta_chunked(q_c, k_c, v_c, g_c[:, :, 0], b_c[:, :, 0], S0)
    o_lf, _ = gated_delta_chunked(q_l, k_l, v_l, g_l[:, :, 0], b_l[:, :, 0], S_f)
    o_cb, S_b = gated_delta_chunked(flip(q_c), flip(k_c), flip(v_c), flip(g_c[:, :, 1]), flip(b_c[:, :, 1]), S0)
    o_lb, _ = gated_delta_chunked(flip(q_l), flip(k_l), flip(v_l), flip(g_l[:, :, 1]), flip(b_l[:, :, 1]), S_b)
    return o_lf + flip(o_lb), o_cf + flip(o_cb)


def dn_output(o, gate, norm_w, dtype):
    B, T = o.shape[:2]
    y = rmsnorm(o, norm_w) * jax.nn.silu(gate.astype(jnp.float32).reshape(B, T, DN_HEADS, HEAD_DIM))
    return y.reshape(B, T, DN_W).astype(dtype)


def hybrid_mixer(h_lat, h_ctx, w_in, qn_w, kn_w, conv_w, A_log, dt_bias, dn_norm_w, w_out, rope, with_ctx):
    qa_l, ka_l, va_l, qkv_l, gate_l, b_l, a_l = split_in(h_lat @ w_in)
    qa_c, ka_c, va_c, qkv_c, gate_c, b_c, a_c = split_in(h_ctx @ w_in)
    q_l, k_l, v_l = attn_group(qa_l, ka_l, va_l, qn_w, kn_w, rope)
    q_c, k_c, v_c = attn_group(qa_c, ka_c, va_c, qn_w, kn_w, None)
    attn_lat = latent_attention(q_l, k_l, v_l, k_c, v_c)
    dn_lat, dn_ctx = dn_bidirectional(dn_prepare(qkv_l, b_l, a_l, conv_w, A_log, dt_bias),
                                      dn_prepare(qkv_c, b_c, a_c, conv_w, A_log, dt_bias))
    o_lat = jnp.concatenate([attn_lat, dn_output(dn_lat, gate_l, dn_norm_w, h_lat.dtype)], axis=-1) @ w_out
    if not with_ctx:
        return o_lat, None
    attn_ctx = context_attention(q_c, k_c, v_c)
    o_ctx = jnp.concatenate([attn_ctx, dn_output(dn_ctx, gate_c, dn_norm_w, h_ctx.dtype)], axis=-1) @ w_out
    return o_lat, o_ctx


def peer_ffn(h, wq, subkeys, u_tab, v_tab):
    B, T, D = h.shape
    tok = h.reshape(-1, PEER_BLOCK, D)

    def block(hb):
        q = (hb @ wq).reshape(PEER_BLOCK, PEER_HEADS, 2, PEER_HALF)
        s = jnp.einsum('thpd,hpkd->thpk', q, subkeys).astype(jnp.float32)
        sv, si = lax.top_k(s, PEER_TOPK)
        cand_s = (sv[..., 0, :, None] + sv[..., 1, None, :]).reshape(PEER_BLOCK, PEER_HEADS, PEER_TOPK * PEER_TOPK)
        cand_i = (si[..., 0, :, None] * N_KEYS + si[..., 1, None, :]).reshape(PEER_BLOCK, PEER_HEADS, PEER_TOPK * PEER_TOPK)
        top_s, pos = lax.top_k(cand_s, PEER_TOPK)
        expert = jnp.take_along_axis(cand_i, pos, axis=-1)
        gate = jax.nn.softmax(top_s, axis=-1)
        u = jnp.take(u_tab, expert, axis=0)
        act = jax.nn.gelu(jnp.einsum('thkd,td->thk', u, hb).astype(jnp.float32), approximate=False)
        vv = jnp.take(v_tab, expert, axis=0)
        return jnp.einsum('thk,thkd->td', (gate * act).astype(hb.dtype), vv)

    return lax.map(block, tok).reshape(B, T, D)


def setup_inputs(seed: int = 0) -> dict:
    key = jax.random.key(seed)
    ks = jax.random.split(key, 20)
    f32 = jnp.float32

    def nrm(k, shape, scale):
        return jax.random.normal(k, shape, f32) * scale

    dt = jnp.exp(jax.random.uniform(ks[13], (DEPTH, 2, DN_HEADS), f32, math.log(1e-3), math.log(1e-1)))
    return {
        'x': nrm(ks[0], (BATCH, SEQ, D_MODEL), 1.0),
        'c': nrm(ks[1], (BATCH, D_MODEL), 1.0),
        'ctx': nrm(ks[2], (BATCH, CTX_LEN, D_MODEL), 1.0),
        'c_ctx': nrm(ks[3], (D_MODEL,), 1.0),
        'ada_w': nrm(ks[4], (DEPTH, D_MODEL, 6 * D_MODEL), 0.5 * D_MODEL ** -0.5),
        'ada_b': nrm(ks[5], (DEPTH, 6 * D_MODEL), 0.02),
        'norm1_w': 1.0 + nrm(ks[6], (DEPTH, D_MODEL), 0.02),
        'norm2_w': 1.0 + nrm(ks[7], (DEPTH, D_MODEL), 0.02),
        'w_in': nrm(ks[8], (DEPTH, D_MODEL, IN_WIDTH), D_MODEL ** -0.5),
        'attn_qnorm_w': 1.0 + nrm(ks[9], (DEPTH, HEAD_DIM), 0.02),
        'attn_knorm_w': 1.0 + nrm(ks[10], (DEPTH, HEAD_DIM), 0.02),
        'dn_conv_w': nrm(ks[11], (DEPTH, CONV_K, 3 * DN_W), CONV_K ** -0.5),
        'dn_A_log': jnp.log(jax.random.uniform(ks[12], (DEPTH, 2, DN_HEADS), f32, 1.0, 16.0)),
        'dn_dt_bias': dt + jnp.log(-jnp.expm1(-dt)),
        'dn_norm_w': 1.0 + nrm(ks[14], (DEPTH, HEAD_DIM), 0.02),
        'w_out': nrm(ks[15], (DEPTH, MIX_WIDTH, D_MODEL), MIX_WIDTH ** -0.5),
        'peer_wq': nrm(ks[16], (DEPTH, D_MODEL, PEER_HEADS * PEER_QDIM), D_MODEL ** -0.5),
        'peer_subkeys': nrm(ks[17], (DEPTH, PEER_HEADS, 2, N_KEYS, PEER_HALF), PEER_HALF ** -0.5),
        'peer_u': nrm(ks[18], (DEPTH, N_EXPERTS, D_MODEL), D_MODEL ** -0.5),
        'peer_v': nrm(ks[19], (DEPTH, N_EXPERTS, D_MODEL), PEER_HEADS ** -0.5),
    }


def reference(x, c, ctx, c_ctx, ada_w, ada_b, norm1_w, norm2_w, w_in, attn_qnorm_w, attn_knorm_w,
              dn_conv_w, dn_A_log, dn_dt_bias, dn_norm_w, w_out, peer_wq, peer_subkeys, peer_u, peer_v):
    T = x.shape[1]
    rope = axial_rope_tables(T)
    for l in range(DEPTH):
        with_ctx = l < DEPTH - 1
        mod_lat = (jax.nn.silu(c) @ ada_w[l] + ada_b[l])[:, None, :]
        mod_ctx = (jax.nn.silu(c_ctx) @ ada_w[l] + ada_b[l])[None, None, :]
        sh1, sc1, g1, sh2, sc2, g2 = jnp.split(mod_lat, 6, axis=-1)
        csh1, csc1, cg1, csh2, csc2, cg2 = jnp.split(mod_ctx, 6, axis=-1)
        h_lat = rmsnorm(x, norm1_w[l]) * (1.0 + sc1) + sh1
        h_ctx = rmsnorm(ctx, norm1_w[l]) * (1.0 + csc1) + csh1
        o_lat, o_ctx = hybrid_mixer(h_lat, h_ctx, w_in[l], attn_qnorm_w[l], attn_knorm_w[l], dn_conv_w[l],
                                    dn_A_log[l], dn_dt_bias[l], dn_norm_w[l], w_out[l], rope, with_ctx)
        x = x + g1 * o_lat
        h2 = rmsnorm(x, norm2_w[l]) * (1.0 + sc2) + sh2
        x = x + g2 * peer_ffn(h2, peer_wq[l], peer_subkeys[l], peer_u[l], peer_v[l])
        if with_ctx:
            ctx = ctx + cg1 * o_ctx
            hc2 = rmsnorm(ctx, norm2_w[l]) * (1.0 + csc2) + csh2
            ctx = ctx + cg2 * peer_ffn(hc2, peer_wq[l], peer_subkeys[l], peer_u[l], peer_v[l])
    return x
```

```python
import numpy as np
import concourse.bass as bass
import concourse.mybir as mybir
from concourse.bass_utils import run_bass_kernel_spmd

F32 = mybir.dt.float32
BF16 = mybir.dt.bfloat16
U32 = mybir.dt.uint32
AF = mybir.ActivationFunctionType
ALU = mybir.AluOpType
AX = mybir.AxisListType


class Tile:
    def __init__(self, h, is_dram=False, base=None):
        self.h = h
        self.lw = None
        self.rd = {}
        self.is_dram = is_dram
        if base is not None:
            self._ap = base
        else:
            self._ap = h.ap() if is_dram else h[:]

    def __getitem__(self, idx):
        return self._ap[idx]

    excl = False

    def view(self, idx):
        return Tile(self.h, self.is_dram, base=self._ap[idx])

    def sub(self, idx):
        return SubTile(self, self._ap[idx])


class SubTile:
    def __init__(self, parent, ap):
        self.parent = parent
        self._ap = ap

    def __getitem__(self, idx):
        return self._ap[idx]

    @property
    def excl(self):
        return self.parent.excl

    @property
    def lw(self):
        return self.parent.lw

    @lw.setter
    def lw(self, v):
        self.parent.lw = v

    @property
    def rd(self):
        return self.parent.rd

    @rd.setter
    def rd(self, v):
        self.parent.rd = v


class KB:
    ND = 24

    def __init__(self, num_devices=None):
        if num_devices:
            nc = bass.Bass("TRN2", target_bir_lowering=False, num_devices=num_devices)
        else:
            nc = bass.Bass("TRN2", target_bir_lowering=False)
        self.nc = nc
        self.engs = {"pe": nc.tensor, "dve": nc.vector, "act": nc.scalar,
                     "pool": nc.gpsimd, "sp": nc.sync}
        self.sems = {k: nc.alloc_semaphore("s_" + k) for k in self.engs}
        self.cnt = {k: 0 for k in self.engs}
        self.known = {k: {} for k in self.engs}
        self.dsem = []
        self.dq = {}
        for q, n in (("sp", 16), ("pool", 12), ("act", 2), ("dve", 2), ("pe", 2)):
            self.dq[q] = []
            for i in range(n):
                k = "d%s%d" % (q, i)
                self.sems[k] = nc.alloc_semaphore("s_" + k)
                self.dsem.append(k)
                self.dq[q].append(k)
        self.dval = {k: 0 for k in self.dsem}
        self.dnext = {q: 0 for q in self.dq}
        self.nuniq = 0
        try:
            nc.allow_low_precision("bf16 matmul operands, fp32 accumulate")
        except Exception:
            pass
        try:
            nc.allow_non_contiguous_dma("strided layouts")
        except Exception:
            pass

    def _nm(self, p):
        self.nuniq += 1
        return "%s_%d" % (p, self.nuniq)

    def sb(self, shape, dt=F32, name="sb"):
        return Tile(self.nc.alloc_sbuf_tensor(self._nm(name), list(shape), dt))

    def ps(self, shape, dt=F32, name="ps"):
        t = Tile(self.nc.alloc_psum_tensor(self._nm(name), list(shape), dt))
        t.excl = True
        return t

    def dram_in(self, name, shape, dt=F32):
        return Tile(self.nc.dram_tensor(name, list(shape), dt, kind="ExternalInput"), True)

    def dram_out(self, name, shape, dt=F32):
        return Tile(self.nc.dram_tensor(name, list(shape), dt, kind="ExternalOutput"), True)

    def dram_tmp(self, name, shape, dt=F32):
        return Tile(self.nc.dram_tensor(name, list(shape), dt, kind="Internal"), True)

    def _waits(self, eng, reads, writes):
        need = {}

        def req(ev):
            if ev is None:
                return
            k, v = ev
            if k == "pe" and eng == "pe":
                return
            if need.get(k, 0) < v:
                need[k] = v

        for t in reads:
            req(t.lw)
        for t in writes:
            req(t.lw)
            for k, v in t.rd.items():
                req((k, v))
        e = self.engs[eng]
        kn = self.known[eng]
        for k, v in need.items():
            if kn.get(k, 0) >= v:
                continue
            e.wait_ge(self.sems[k], v)
            kn[k] = v

    def _mark(self, ev, reads, writes):
        k, v = ev
        for t in reads:
            if t.rd.get(k, 0) < v:
                t.rd[k] = v
        for t in writes:
            t.lw = ev
            t.rd = {}

    rec = None

    def replay(self, *lists):
        self.rec = None
        n = max(len(l) for l in lists)
        for i in range(n):
            for l in lists:
                if i < len(l):
                    kind, a, fn, r, w, kw = l[i]
                    if kind == "op":
                        self.op(a, fn, r, w)
                    else:
                        self.dma(a, fn[0], fn[1], r, w, **kw)

    def op(self, eng, fn, reads=(), writes=()):
        if self.rec is not None:
            self.rec.append(("op", eng, fn, list(reads), list(writes), None))
            return
        xr = [t for t in reads if t.excl]
        if xr:
            reads = [t for t in reads if not t.excl]
            writes = list(writes) + xr
        self._waits(eng, reads, writes)
        inst = fn(self.engs[eng])
        self.cnt[eng] += 1
        inst.then_inc(self.sems[eng], 1)
        self._mark((eng, self.cnt[eng]), reads, writes)

    def dma(self, q, out, in_, reads=(), writes=(), **kw):
        if self.rec is not None:
            self.rec.append(("dma", q, (out, in_), list(reads), list(writes), kw))
            return
        pool_ = self.dq[q]
        k = pool_[self.dnext[q]]
        self.dnext[q] = (self.dnext[q] + 1) % len(pool_)
        e = self.engs[q]
        kn = self.known[q]
        if kn.get(k, 0) < self.dval[k]:
            e.wait_ge(self.sems[k], self.dval[k])
            kn[k] = self.dval[k]
        self._waits(q, reads, writes)
        inst = e.dma_start(out=out, in_=in_, **kw)
        self.dval[k] += 16
        inst.then_inc(self.sems[k], 16)
        self._mark((k, self.dval[k]), reads, writes)

    def barrier(self):
        for q, e in self.engs.items():
            kn = self.known[q]
            for k in self.dsem:
                if self.dval[k] > kn.get(k, 0):
                    e.wait_ge(self.sems[k], self.dval[k])
                    kn[k] = self.dval[k]
            for k in ("pe", "dve", "act", "pool", "sp"):
                if k != q and self.cnt[k] > kn.get(k, 0):
                    e.wait_ge(self.sems[k], self.cnt[k])
                    kn[k] = self.cnt[k]

    def finish(self):
        e = self.engs["sp"]
        kn = self.known["sp"]
        for k in self.dsem:
            if self.dval[k] > kn.get(k, 0):
                e.wait_ge(self.sems[k], self.dval[k])
                kn[k] = self.dval[k]
        for k in ("pe", "dve", "act", "pool"):
            if self.cnt[k] > kn.get(k, 0):
                e.wait_ge(self.sems[k], self.cnt[k])
                kn[k] = self.cnt[k]

    def mm(self, out_t, out_ap, lhsT_t, lhsT_ap, rhs_t, rhs_ap, start=True, stop=True, skip=False):
        if skip:
            self.op("pe", lambda e: e.matmul(out_ap, lhsT_ap, rhs_ap, start=start, stop=stop, skip_group_check=True),
                    reads=[lhsT_t, rhs_t], writes=[out_t])
        else:
            self.op("pe", lambda e: e.matmul(out_ap, lhsT_ap, rhs_ap, start=start, stop=stop),
                    reads=[lhsT_t, rhs_t], writes=[out_t])

    def tr(self, out_t, out_ap, in_t, in_ap, ident_t, ident_ap):
        self.op("pe", lambda e: e.transpose(out_ap, in_ap, ident_ap),
                reads=[in_t, ident_t], writes=[out_t])

    def act(self, out_t, out_ap, in_t, in_ap, func, extra_reads=(), eng="act", **kw):
        self.op(eng, lambda e: e.activation(out=out_ap, in_=in_ap, func=func, **kw),
                reads=[in_t] + list(extra_reads), writes=[out_t])

    def tt(self, ot, oap, at, aap, bt, bap, op, eng="dve"):
        self.op(eng, lambda e: e.tensor_tensor(out=oap, in0=aap, in1=bap, op=op), reads=[at, bt], writes=[ot])

    def ts(self, ot, oap, at, aap, s1, op0, s2=None, op1=None, sr=(), eng="dve"):
        if op1 is None:
            self.op(eng, lambda e: e.tensor_scalar(out=oap, in0=aap, scalar1=s1, scalar2=None, op0=op0),
                    reads=[at] + list(sr), writes=[ot])
        else:
            self.op(eng, lambda e: e.tensor_scalar(out=oap, in0=aap, scalar1=s1, scalar2=s2, op0=op0, op1=op1),
                    reads=[at] + list(sr), writes=[ot])

    def stt(self, ot, oap, at, aap, sc, bt, bap, op0, op1, sr=()):
        self.op("dve", lambda e: e.scalar_tensor_tensor(out=oap, in0=aap, scalar=sc, in1=bap, op0=op0, op1=op1),
                reads=[at, bt] + list(sr), writes=[ot])

    def cp(self, ot, oap, it, iap, eng="act"):
        if eng == "act":
            self.op("act", lambda e: e.activation(out=oap, in_=iap, func=AF.Copy), reads=[it], writes=[ot])
        else:
            self.op(eng, lambda e: e.tensor_copy(out=oap, in_=iap), reads=[it], writes=[ot])

    def run(self, in_maps, n=8, trace=False):
        self.finish()
        if trace:
            return run_bass_kernel_spmd(self.nc, in_maps, core_ids=list(range(n)), trace=True)
        return run_bass_kernel_spmd(self.nc, in_maps, core_ids=list(range(n)))


D = 1024
KC = 8
INW = 3088


def emit_mod(kb, cT, ada_w, ada_bT, j0, j1, wt=None, bw=512, modp=None, col_base=0):
    nj = j1 - j0
    sc = kb.sb([128, KC, 2], name="silu_c")
    craw = kb.sb([128, KC, 2], name="craw")
    kb.dma("sp", craw[:], cT[:], writes=[craw])
    kb.act(sc, sc[:], craw, craw[:], AF.Silu)
    if modp is None:
        modp = kb.ps([128, nj, 2], name="modp")
    else:
        modp = modp.sub((slice(None), slice(0, nj * 2)))
        modp = SubTile(modp.parent, modp[:].rearrange("p (a b) -> p a b", b=2))
    if wt is None:
        wt = [kb.sb([128, KC, bw], name="adaw") for _ in range(2)]
    ada_v = ada_w[:].rearrange("(k p) n -> p k n", p=128)
    npb = bw // 128
    for jb in range(0, nj, npb):
        t = wt[(jb // npb) % 2]
        c0 = (j0 + jb) * 128 - col_base
        kb.dma("sp", t[:], ada_v[:, :, c0:c0 + bw], writes=[t])
        for jj in range(npb):
            j = jb + jj
            for k in range(KC):
                kb.mm(modp, modp[:, j, :], t, t[:, k, jj * 128:(jj + 1) * 128], sc, sc[:, k, :],
                      start=(k == 0), stop=(k == KC - 1))
    bT = kb.sb([128, nj], name="adab")
    kb.dma("sp", bT[:], ada_bT[:, j0:j1], writes=[bT])
    mod = kb.sb([128, nj, 2], name="mod")
    for s in range(2):
        kb.op("dve", lambda e, s=s: e.tensor_tensor(out=mod[:, :, s], in0=modp[:, :, s], in1=bT[:], op=ALU.add),
              reads=[modp, bT], writes=[mod])
    return mod


def emit_norm_mod(kb, xt, hT, ones, eps_t, a_sc, b_sh, s, ncol, sqs, ssp, rstd, hdt_tmp):
    for k in range(KC):
        sq = sqs[k % 2]
        kb.act(sq, sq[:, :ncol], xt, xt[:, k, :ncol], AF.Square)
        kb.mm(ssp, ssp[:, :ncol], ones, ones[:], sq, sq[:, :ncol], start=(k == 0), stop=(k == KC - 1))
    kb.act(rstd, rstd[:, :ncol], ssp, ssp[:, :ncol], AF.Sqrt, extra_reads=[eps_t], bias=eps_t[:, 0:1], scale=1.0 / D)
    kb.op("dve", lambda e: e.reciprocal(out=rstd[:, :ncol], in_=rstd[:, :ncol]), reads=[rstd], writes=[rstd])
    for k in range(KC):
        tmp = hdt_tmp[k % 2]
        kb.op("dve", lambda e, k=k, tmp=tmp: e.scalar_tensor_tensor(
            out=tmp[:, :ncol], in0=xt[:, k, :ncol], scalar=a_sc[:, k, s:s + 1], in1=rstd[:, :ncol],
            op0=ALU.mult, op1=ALU.mult), reads=[xt, a_sc, rstd], writes=[tmp])
        kb.act(hT, hT[:, k, :ncol], tmp, tmp[:, :ncol], AF.Identity, extra_reads=[b_sh],
               bias=b_sh[:, k, s:s + 1], scale=1.0)


def build_A(NLAT=4096, NCTX=64):
    kb = KB()
    NT = NLAT + NCTX
    xT = kb.dram_in("xT", [D, NT])
    cT = kb.dram_in("cT", [128, KC, 2])
    ada_w = kb.dram_in("ada_w", [D, 2 * D])
    ada_bT = kb.dram_in("ada_bT", [128, 48])
    n1T = kb.dram_in("n1T", [128, KC])
    w_in = kb.dram_in("w_in", [D, INW])
    pT = kb.dram_out("pT", [INW, NT])

    ones = kb.sb([128, 128], name="ones")
    kb.op("dve", lambda e: e.memset(ones[:], 1.0), writes=[ones])
    eps_t = kb.sb([128, 1], name="eps")
    kb.op("dve", lambda e: e.memset(eps_t[:], 1e-6), writes=[eps_t])

    w_bf = kb.sb([128, KC, INW], BF16, name="w_bf")
    wv = w_in[:].rearrange("(k p) n -> p k n", p=128)
    for k in range(KC):
        kb.dma("pool", w_bf[:, k, :], wv[:, k, :], writes=[w_bf])

    mod = emit_mod(kb, cT, ada_w, ada_bT, 0, 16)
    n1 = kb.sb([128, KC], name="n1")
    kb.dma("sp", n1[:], n1T[:], writes=[n1])
    a_sc = kb.sb([128, KC, 2], name="a_sc")
    b_sh = kb.sb([128, KC, 2], name="b_sh")
    for s in range(2):
        kb.op("dve", lambda e, s=s: e.scalar_tensor_tensor(
            out=a_sc[:, :, s], in0=mod[:, 8:16, s], scalar=1.0, in1=n1[:], op0=ALU.add, op1=ALU.mult),
            reads=[mod, n1], writes=[a_sc])
        kb.op("dve", lambda e, s=s: e.tensor_copy(out=b_sh[:, :, s], in_=mod[:, 0:8, s]), reads=[mod], writes=[b_sh])

    xts = [kb.sb([128, KC, 512], name="xt") for _ in range(2)]
    hTs = [kb.sb([128, KC, 512], BF16, name="hT") for _ in range(2)]
    sqs = [kb.sb([128, 512], name="sq") for _ in range(2)]
    tmps = [kb.sb([128, 512], name="tmp") for _ in range(2)]
    rstd = kb.sb([128, 512], name="rstd")
    ssp = kb.ps([128, 512], name="ssp")
    pps = [kb.ps([128, 512], name="pp") for _ in range(4)]
    stg = [kb.sb([128, 512], name="stg") for _ in range(4)]
    xv = xT[:].rearrange("(k p) n -> p k n", p=128)

    tiles = [(t0, 512, 0) for t0 in range(0, NLAT, 512)]
    if NCTX:
        tiles.append((NLAT, NCTX, 1))
    MCH = [(m * 128, 128) for m in range(INW // 128)] + [(INW // 128 * 128, INW % 128)]
    cnt = 0
    def load_x(ti):
        t0, ncol, s = tiles[ti]
        for k in range(KC):
            kb.dma("sp", xts[ti % 2][:, k, :ncol], xv[:, k, t0:t0 + ncol], writes=[xts[ti % 2]])
    load_x(0)
    for ti, (t0, ncol, s) in enumerate(tiles):
        xt = xts[ti % 2]
        hT = hTs[ti % 2]
        if ti + 1 < len(tiles):
            load_x(ti + 1)
        emit_norm_mod(kb, xt, hT, ones, eps_t, a_sc, b_sh, s, ncol, sqs, ssp, rstd, tmps)
        for (m0, mw) in MCH:
            pp = pps[cnt % 4]
            st = stg[cnt % 4]
            for k in range(KC):
                kb.mm(pp, pp[:mw, :ncol], w_bf, w_bf[:, k, m0:m0 + mw], hT, hT[:, k, :ncol],
                      start=(k == 0), stop=(k == KC - 1))
            if cnt % 2 == 0:
                kb.act(st, st[:mw, :ncol], pp, pp[:mw, :ncol], AF.Copy)
            else:
                kb.op("dve", lambda e, st=st, pp=pp, mw=mw: e.tensor_copy(out=st[:mw, :ncol], in_=pp[:mw, :ncol]),
                      reads=[pp], writes=[st])
            kb.dma("sp", pT[m0:m0 + mw, t0:t0 + ncol], st[:mw, :ncol], reads=[st], writes=[])
            cnt += 1
    return kb


HD = 128


def build_attn(TL=16384, TC=256):
    kb = KB()
    TT = TL + TC
    qT = kb.dram_in("qT", [HD, TT])
    kT = kb.dram_in("kT", [HD, TT])
    vT = kb.dram_in("vT", [HD, TT])
    cosT = kb.dram_in("cosT", [HD, TL])
    sinT = kb.dram_in("sinT", [HD, TL])
    RmT_d = kb.dram_in("RmT", [HD, HD])
    ident_d = kb.dram_in("ident", [HD, HD])
    qkw_d = kb.dram_in("qkw", [HD, 2])
    oT = kb.dram_out("oT", [HD, TT])

    ones = kb.sb([128, 128], name="ones")
    kb.op("dve", lambda e: e.memset(ones[:], 1.0), writes=[ones])
    ones_bf = kb.sb([128, 128], BF16, name="ones_bf")
    kb.op("dve", lambda e: e.memset(ones_bf[:], 1.0), writes=[ones_bf])
    eps_t = kb.sb([128, 1], name="eps")
    kb.op("dve", lambda e: e.memset(eps_t[:], 1e-6), writes=[eps_t])
    RmT = kb.sb([128, 128], name="RmT")
    ident = kb.sb([128, 128], name="ident")
    qkw = kb.sb([128, 2], name="qkw")
    kb.dma("sp", RmT[:], RmT_d[:], writes=[RmT])
    kb.dma("sp", ident[:], ident_d[:], writes=[ident])
    kb.dma("sp", qkw[:], qkw_d[:], writes=[qkw])

    Q_bf = kb.sb([128, TT], BF16, name="Q_bf")
    K_bf = kb.sb([128, TT], BF16, name="K_bf")
    NKT = TT // 128
    V_bf = kb.sb([128, NKT, 128], BF16, name="V_bf")

    P = [kb.ps([128, 512], name="P") for _ in range(8)]
    raw = [kb.sb([128, 512], name="raw") for _ in range(2)]
    cs = [kb.sb([128, 512], name="cs") for _ in range(2)]
    sn = [kb.sb([128, 512], name="sn") for _ in range(2)]
    sq = kb.sb([128, 512], name="sq")
    rstd = kb.sb([128, 512], name="rstd")
    xn = kb.sb([128, 512], name="xn")
    t1 = kb.sb([128, 512], name="t1")
    t2 = kb.sb([128, 512], name="t2")

    tiles = [(t0, 512, True) for t0 in range(0, TL, 512)]
    for t0 in range(TL, TT, 512):
        tiles.append((t0, min(512, TT - t0), False))
    it = 0
    for ti, (t0, nc_, lat) in enumerate(tiles):
        if lat:
            c_t = cs[ti % 2]; s_t = sn[ti % 2]
            kb.dma("sp", c_t[:, :nc_], cosT[:, t0:t0 + nc_], writes=[c_t])
            kb.dma("sp", s_t[:, :nc_], sinT[:, t0:t0 + nc_], writes=[s_t])
        for wi, (src, dst) in enumerate(((qT, Q_bf), (kT, K_bf))):
            r = raw[it % 2]; it += 1
            kb.dma("sp", r[:, :nc_], src[:, t0:t0 + nc_], writes=[r])
            kb.act(sq, sq[:, :nc_], r, r[:, :nc_], AF.Square)
            kb.mm(P[0], P[0][:, :nc_], ones, ones[:], sq, sq[:, :nc_])
            kb.act(rstd, rstd[:, :nc_], P[0], P[0][:, :nc_], AF.Sqrt, extra_reads=[eps_t], bias=eps_t[:, 0:1], scale=1.0 / HD)
            kb.op("dve", lambda e: e.reciprocal(out=rstd[:, :nc_], in_=rstd[:, :nc_]), reads=[rstd], writes=[rstd])
            if lat:
                kb.op("dve", lambda e, r=r, wi=wi: e.scalar_tensor_tensor(
                    out=xn[:, :nc_], in0=r[:, :nc_], scalar=qkw[:, wi:wi + 1], in1=rstd[:, :nc_], op0=ALU.mult, op1=ALU.mult),
                    reads=[r, qkw, rstd], writes=[xn])
                kb.mm(P[1], P[1][:, :nc_], RmT, RmT[:], xn, xn[:, :nc_])
                kb.op("dve", lambda e, c_t=c_t: e.tensor_tensor(out=t1[:, :nc_], in0=xn[:, :nc_], in1=c_t[:, :nc_], op=ALU.mult),
                      reads=[xn, c_t], writes=[t1])
                kb.op("dve", lambda e, s_t=s_t: e.tensor_tensor(out=t2[:, :nc_], in0=P[1][:, :nc_], in1=s_t[:, :nc_], op=ALU.mult),
                      reads=[P[1], s_t], writes=[t2])
                kb.op("dve", lambda e, dst=dst: e.tensor_tensor(out=dst[:, t0:t0 + nc_], in0=t1[:, :nc_], in1=t2[:, :nc_], op=ALU.add),
                      reads=[t1, t2], writes=[dst])
            else:
                kb.op("dve", lambda e, r=r, wi=wi, dst=dst: e.scalar_tensor_tensor(
                    out=dst[:, t0:t0 + nc_], in0=r[:, :nc_], scalar=qkw[:, wi:wi + 1], in1=rstd[:, :nc_], op0=ALU.mult, op1=ALU.mult),
                    reads=[r, qkw, rstd], writes=[dst])
        r = raw[it % 2]; it += 1
        kb.dma("sp", r[:, :nc_], vT[:, t0:t0 + nc_], writes=[r])
        nb = nc_ // 128
        for bi in range(nb):
            kb.tr(P[2], P[2][:, bi * 128:(bi + 1) * 128], r, r[:, bi * 128:(bi + 1) * 128], ident, ident[:])
        kt0 = t0 // 128
        kb.act(V_bf, V_bf[:, kt0:kt0 + nb, :], P[2], P[2][:, :nb * 128].rearrange("p (a b) -> p a b", b=128), AF.Copy)

    scale = float(HD) ** -0.5
    pts = [kb.sb([128, 512], BF16, name="pt") for _ in range(3)]
    rs = kb.sb([128, 512], name="rs")
    ost = [kb.sb([128, 512], name="ost") for _ in range(2)]
    acc = [(P[0], P[1]), (P[5], P[6])]
    sacc = [[kb.sb([128, 512], name="sacc") for _ in range(2)] for _ in range(2)]
    stot = kb.sb([128, 512], name="stot")
    sTs = [P[3], P[4], P[7]]
    qtiles = [(q0, 512, 0, NKT) for q0 in range(0, TL, 512)]
    for q0 in range(TL, TT, 512):
        qtiles.append((q0, min(512, TT - q0), TL // 128, NKT))
    work = []
    for qi, (q0, nq, kta, ktb) in enumerate(qtiles):
        for kt in range(kta, ktb):
            work.append((qi, q0, nq, kt, kt == kta, kt == ktb - 1))

    def issue_s(n):
        qi, q0, nq, kt, first, last = work[n]
        sT = sTs[n % 3]
        kb.mm(sT, sT[:, :nq], K_bf, K_bf[:, kt * 128:(kt + 1) * 128], Q_bf, Q_bf[:, q0:q0 + nq])

    issue_s(0)
    for n in range(len(work)):
        qi, q0, nq, kt, first, last = work[n]
        oP, sP = acc[qi % 2]
        sT = sTs[n % 3]; pt = pts[n % 3]
        if n + 1 < len(work):
            issue_s(n + 1)
        kb.act(pt, pt[:, :nq], sT, sT[:, :nq], AF.Exp, scale=scale)
        kb.mm(oP, oP[:, :nq], V_bf, V_bf[:, kt, :], pt, pt[:, :nq], start=first, stop=last)
        kk = (kt - work[n][3] + (kt % 2)) % 2 if False else (kt % 2)
        sa = sacc[qi % 2][kk]
        eng_ = "dve" if kk == 0 else "pool"
        kfirst = (kt - (TL // 128 if q0 >= TL else 0)) < 2
        if kfirst:
            kb.cp(sa, sa[:, :nq], pt, pt[:, :nq], eng=eng_)
        else:
            kb.tt(sa, sa[:, :nq], sa, sa[:, :nq], pt, pt[:, :nq], ALU.add, eng=eng_)
        if last:
            st = ost[qi % 2]
            kb.tt(stot, stot[:, :nq], sacc[qi % 2][0], sacc[qi % 2][0][:, :nq], sacc[qi % 2][1], sacc[qi % 2][1][:, :nq], ALU.add)
            kb.mm(sP, sP[:, :nq], ones, ones[:], stot, stot[:, :nq])
            kb.op("dve", lambda e, sP=sP, nq=nq: e.reciprocal(out=rs[:, :nq], in_=sP[:, :nq]), reads=[sP], writes=[rs])
            kb.op("dve", lambda e, oP=oP, st=st, nq=nq: e.tensor_tensor(out=st[:, :nq], in0=oP[:, :nq], in1=rs[:, :nq], op=ALU.mult),
                  reads=[oP, rs], writes=[st])
            kb.dma("sp", oT[:, q0:q0 + nq], st[:, :nq], reads=[st], writes=[])
    return kb


def rope_tables(T=16384, GRID_W=64):
    rows = T // GRID_W
    row = np.repeat(np.arange(rows, dtype=np.float32), GRID_W)
    col = np.tile(np.arange(GRID_W, dtype=np.float32), rows)
    axis_dim = HD // 2
    inv_freq = (10000.0 ** (-np.arange(0, axis_dim, 2, dtype=np.float32) / axis_dim)).astype(np.float32)
    ang_r = row[:, None] * inv_freq[None, :]
    ang_c = col[:, None] * inv_freq[None, :]
    ang = np.concatenate([ang_r, ang_r, ang_c, ang_c], axis=-1)
    return np.cos(ang).astype(np.float32), np.sin(ang).astype(np.float32)


def rot_matrix_T():
    R = np.zeros((128, 128), np.float32)
    for dp in range(128):
        if (dp % 64) < 32:
            R[dp, dp + 32] = -1.0
        else:
            R[dp, dp - 32] = 1.0
    return np.ascontiguousarray(R.T)


def dn_consts():
    p = np.arange(128)[:, None]
    q = np.arange(128)[None, :]
    same = (p // 64) == (q // 64)
    cm = np.zeros((6, 128, 128), np.float32)
    cm[0] = np.eye(128)
    cm[1] = (same & (q <= p))
    cm[2] = (same & (q >= p))
    cm[3] = same
    cm[4] = (p < 64) * np.ones((1, 128))
    cm[5] = (p >= 64) * np.ones((1, 128))
    return cm


def build_dn(TL=16384, TC=256, stage=9):
    kb = KB()
    TT = TL + TC
    NB = TT // 128
    NBL = TL // 128
    PADW = TL + 4 + TC + 4
    qkvP = kb.dram_in("qkvP", [128, 3, PADW])
    gateT = kb.dram_in("gateT", [128, TT])
    baT = kb.dram_in("baT", [4, TT])
    cwT = kb.dram_in("cwT", [128, 3, 5])
    cst_d = kb.dram_in("cst", [128, 8])
    cm_d = kb.dram_in("cm", [6, 128, 128])
    yT = kb.dram_out("yT", [128, TT])
    oS = [kb.dram_tmp("oF", [128, TT]), kb.dram_tmp("oB", [128, TT])]
    oSv = [[oS[d].view((slice(None), slice(n * 128, (n + 1) * 128))) for n in range(NB)] for d in range(2)]

    cmt = kb.sb([128, 6, 128], name="cm")
    kb.dma("sp", cmt[:], cm_d[:].rearrange("c p q -> p c q"), writes=[cmt])
    ident_ap = cmt[:, 0, :]
    LO = cmt[:, 1, :]; UP = cmt[:, 2, :]; BLK = cmt[:, 3, :]
    HALF = [cmt[:, 4, :], cmt[:, 5, :]]
    ones = kb.sb([128, 128], name="ones")
    kb.op("dve", lambda e: e.memset(ones[:], 1.0), writes=[ones])
    eps_t = kb.sb([128, 1], name="eps")
    kb.op("dve", lambda e: e.memset(eps_t[:], 1e-6), writes=[eps_t])
    one_c = kb.sb([128, 1], name="onec")
    kb.op("dve", lambda e: e.memset(one_c[:], 1.0), writes=[one_c])
    msk = kb.sb([128, 3, 128], name="msk")
    kb.ts(msk, msk[:, 0, :], cmt, LO, -1e4, ALU.mult, 1e4, ALU.add)
    kb.ts(msk, msk[:, 1, :], cmt, UP, -1e4, ALU.mult, 1e4, ALU.add)
    kb.ts(msk, msk[:, 2, :], cmt, ident_ap, -1.0, ALU.mult, 1.0, ALU.add)
    MLO = msk[:, 0, :]; MUP = msk[:, 1, :]; NOTI = msk[:, 2, :]
    cw = kb.sb([128, 3, 5], name="cw")
    kb.dma("sp", cw[:], cwT[:], writes=[cw])
    cst = kb.sb([128, 8], name="cst")
    kb.dma("sp", cst[:], cst_d[:], writes=[cst])
    nega = kb.sb([128, 2], name="nega")
    kb.act(nega, nega[:], cst, cst[:, 0:2], AF.Exp)
    kb.ts(nega, nega[:], nega, nega[:], -1.0, ALU.mult)

    banks = [kb.ps([128, 512], name="bank") for _ in range(8)]

    def v(b, c0, w):
        return banks[b].sub((slice(None), slice(c0, c0 + w)))

    TS = kb.sb([128, NB, 4], name="TS")
    bat = [kb.sb([4, 2048], name="bat") for _ in range(2)]
    pts = [v(0, 0, 512), v(1, 0, 512)]
    for pi, c0 in enumerate(range(0, TT, 2048)):
        w = min(2048, TT - c0)
        bt = bat[pi % 2]
        kb.dma("sp", bt[:, :w], baT[:, c0:c0 + w], writes=[bt])
        pt = pts[pi % 2]
        nb = w // 128
        for i in range(nb):
            kb.tr(pt, pt[:, i * 4:(i + 1) * 4], bt, bt[:, i * 128:(i + 1) * 128], cmt, cmt[0:4, 0, 0:4])
        n0 = c0 // 128
        kb.cp(TS, TS[:, n0:n0 + nb, :], pt, pt[:, :nb * 4].rearrange("p (a b) -> p a b", b=4), eng="dve")
    if stage == 0:
        o = kb.dram_out("dTS", [128, NB * 4])
        kb.dma("sp", o[:], TS[:].rearrange("p a b -> p (a b)"), reads=[TS], writes=[])
        o = kb.dram_out("dmsk", [128, 3 * 128])
        kb.dma("sp", o[:], msk[:].rearrange("p a b -> p (a b)"), reads=[msk], writes=[])
        o = kb.dram_out("dnega", [128, 2])
        kb.dma("sp", o[:], nega[:], reads=[nega], writes=[])
        return kb
    BETA = kb.sb([128, NB, 2], name="BETA")
    kb.act(BETA, BETA[:], TS, TS[:, :, 0:2], AF.Sigmoid)
    G = kb.sb([128, NB, 2], name="G")
    for d in range(2):
        kb.act(G, G[:, :, d], TS, TS[:, :, 2 + d], AF.Exp, extra_reads=[cst], bias=cst[:, 2 + d:3 + d], scale=1.0)
        kb.act(G, G[:, :, d], G, G[:, :, d], AF.Ln, extra_reads=[one_c], bias=one_c[:, 0:1], scale=1.0)
        kb.ts(G, G[:, :, d], G, G[:, :, d], nega[:, d:d + 1], ALU.mult, sr=[nega])
    GC = kb.sb([128, NB, 2], name="GC")
    GT = kb.sb([128, NB, 2], name="GT")
    EGL = kb.sb([128, NB, 2, 2], name="EGL")
    pa = v(2, 0, 512)
    for d in range(2):
        kb.mm(pa, pa[:, 0:NB], cmt, (UP if d == 0 else LO), G, G[:, :, d])
        kb.cp(GC, GC[:, :, d], pa, pa[:, 0:NB], eng="dve")
        kb.mm(pa, pa[:, 0:NB], cmt, BLK, G, G[:, :, d])
        kb.cp(GT, GT[:, :, d], pa, pa[:, 0:NB], eng="dve")
        for h in range(2):
            kb.mm(pa, pa[:, 0:NB], cmt, HALF[h], G, G[:, :, d])
            kb.act(EGL, EGL[:, :, d, h], pa, pa[:, 0:NB], AF.Exp)
    EGC = kb.sb([128, NB, 2], name="EGC")
    kb.act(EGC, EGC[:], GC, GC[:], AF.Exp)
    EKL = kb.sb([128, NB, 2], name="EKL")
    kb.tt(EKL, EKL[:], GT, GT[:], GC, GC[:], ALU.subtract)
    kb.act(EKL, EKL[:], EKL, EKL[:], AF.Exp)
    BEG = kb.sb([128, NB, 2], name="BEG")
    kb.tt(BEG, BEG[:], BETA, BETA[:], EGC, EGC[:], ALU.mult)
    NBETA = kb.sb([128, NB, 2], name="NBETA")
    kb.ts(NBETA, NBETA[:], BETA, BETA[:], -1.0, ALU.mult)
    EKLH = kb.sb([128, NB, 2, 2], name="EKLH")
    kb.op("dve", lambda e: e.memset(EKLH[:], 0.0), writes=[EKLH])
    for h in range(2):
        kb.cp(EKLH, EKLH[h * 64:(h + 1) * 64, :, :, h], EKL, EKL[h * 64:(h + 1) * 64, :, :], eng="dve")

    if stage == 1:
        for nm, t, sh in (("dGC", GC, [128, NB * 2]), ("dBETA", BETA, [128, NB * 2]), ("dEGL", EGL, [128, NB * 4]),
                          ("dEKLH", EKLH, [128, NB * 4]), ("dG", G, [128, NB * 2])):
            o = kb.dram_out(nm, sh)
            kb.dma("sp", o[:], t[:].rearrange("p a b -> p (a b)") if len(sh) == 2 and nm in ("dGC", "dBETA", "dG") else t[:].rearrange("p a b c -> p (a b c)"), reads=[t], writes=[])
        return kb
    kb.barrier()
    def mk(name, shape=(128, 128)):
        return [kb.sb(list(shape), name=name + str(d)) for d in range(2)]

    XIN = mk("xin", (128, 3, 132)); ACC = mk("acc", (128, 3, 128)); Y = mk("y", (128, 3, 128))
    SQ = mk("sq", (128, 2, 128)); RN = mk("rn", (128, 2, 128)); QN = mk("qn"); KN = mk("kn")
    VB = mk("vb"); KBG = mk("kbg"); KEL = mk("kel", (128, 2, 128)); DG = mk("dg"); T0 = mk("t0")
    X1 = mk("x1"); X2 = mk("x2"); DM = mk("dm"); DMT = mk("dmt"); EBC = mk("ebc"); QG = mk("qg"); QKM = mk("qkm")
    AD = mk("ad"); MTa = [mk("mta"), mk("mtb")]; Ma = [mk("ma"), mk("mb")]
    Ra = [mk("ra"), mk("rb")]
    VN = mk("vn"); OSB = mk("osb"); OTS = mk("ots")
    U2 = [mk("u_a"), mk("u_b")]; WT2 = [mk("wt_a"), mk("wt_b")]; QG2 = [mk("qg_a"), mk("qg_b")]
    QKM2 = [mk("qkm_a"), mk("qkm_b")]; KEL2 = [mk("kel_a", (128, 2, 128)), mk("kel_b", (128, 2, 128))]
    S = [mk("s0"), mk("s1")]
    for d in range(2):
        kb.op("dve", lambda e, d=d: e.memset(VN[d][:], 0.0), writes=[VN[d]])
        kb.op("dve", lambda e, d=d: e.memset(S[0][d][:], 0.0), writes=[S[0][d]])
    spar = [0, 0]
    P_ss = [v(4 * d + 0, 0, 256) for d in range(2)]
    P_g = [v(4 * d + 0, 256, 128) for d in range(2)]
    P_uw = [v(4 * d + 0, 384, 128) for d in range(2)]
    P_tr = [v(4 * d + 1, 0, 256) for d in range(2)]
    P_kk = [v(4 * d + 1, 256, 256) for d in range(2)]
    P_mm = [v(4 * d + 2, 0, 256) for d in range(2)]
    P_m0 = [v(4 * d + 2, 256, 128) for d in range(2)]
    P_r = [v(4 * d + 2, 384, 128) for d in range(2)]
    P_p1 = [v(4 * d + 3, 0, 128) for d in range(2)]
    P_o = [v(4 * d + 3, 128, 128) for d in range(2)]
    P_s = [v(4 * d + 3, 256, 128) for d in range(2)]
    P_ot = [v(4 * d + 3, 384, 128) for d in range(2)]

    def prep(n, d, pp=0):
        U = U2[pp]; WT = WT2[pp]; QG = QG2[pp]; QKM = QKM2[pp]; KEL = KEL2[pp]
        c0 = n * 128 if n < NBL else TL + 4 + (n - NBL) * 128
        xin = XIN[d]; acc = ACC[d]; y = Y[d]
        kb.dma("sp", xin[:], qkvP[:, :, c0:c0 + 132], writes=[xin])
        for w in range(3):
            kb.ts(acc, acc[:, w, :], xin, xin[:, w, 0:128], cw[:, w, 0:1], ALU.mult, sr=[cw])
            for jj in range(1, 5):
                kb.stt(acc, acc[:, w, :], xin, xin[:, w, jj:jj + 128], cw[:, w, jj:jj + 1], acc, acc[:, w, :],
                       ALU.mult, ALU.add, sr=[cw])
        kb.act(y, y[:], acc, acc[:], AF.Silu)
        if stage == 21: return
        sq = SQ[d]; rn = RN[d]; pss = P_ss[d]
        kb.act(sq, sq[:], y, y[:, 0:2, :], AF.Square)
        kb.mm(pss, pss[:], ones, ones[:], sq, sq[:].rearrange("p a b -> p (a b)"))
        kb.act(rn, rn[:].rearrange("p a b -> p (a b)"), pss, pss[:], AF.Sqrt, extra_reads=[eps_t], bias=eps_t[:, 0:1], scale=1.0)
        kb.op("dve", lambda e: e.reciprocal(out=rn[:], in_=rn[:]), reads=[rn], writes=[rn])
        qn = QN[d]; kn = KN[d]
        kb.stt(qn, qn[:], y, y[:, 0, :], float(HD) ** -0.5, rn, rn[:, 0, :], ALU.mult, ALU.mult)
        kb.tt(kn, kn[:], y, y[:, 1, :], rn, rn[:, 1, :], ALU.mult)
        if stage == 22: return
        ptr = P_tr[d]
        kb.tr(ptr, ptr[:, 0:128], kn, kn[:], cmt, ident_ap)
        kb.tr(ptr, ptr[:, 128:256], y, y[:, 2, :], cmt, ident_ap)
        kb.ts(KBG[d], KBG[d][:], ptr, ptr[:, 0:128], BEG[:, n, d:d + 1], ALU.mult, sr=[BEG])
        for h in range(2):
            kb.ts(KEL[d], KEL[d][:, h, :], ptr, ptr[:, 0:128], EKLH[:, n, d, h:h + 1], ALU.mult, sr=[EKLH])
        kb.ts(VB[d], VB[d][:], ptr, ptr[:, 128:256], BETA[:, n, d:d + 1], ALU.mult, sr=[BETA])
        if stage == 23: return
        pkk = P_kk[d]
        kb.mm(pkk, pkk[:, 0:128], kn, kn[:], kn, kn[:])
        kb.mm(pkk, pkk[:, 128:256], kn, kn[:], qn, qn[:])
        kb.ts(DG[d], DG[d][:], cmt, ident_ap, GC[:, n, d:d + 1], ALU.mult, sr=[GC])
        pg = P_g[d]
        kb.mm(pg, pg[:], ones, ones[:], DG[d], DG[d][:])
        kb.ts(T0[d], T0[d][:], pg, pg[:], GC[:, n, d:d + 1], ALU.subtract, sr=[GC])
        kb.act(EBC[d], EBC[d][:], pg, pg[:], AF.Exp)
        if stage == 24: return
        M1 = MLO if d == 0 else MUP
        M2 = MUP if d == 0 else MLO
        kb.tt(X1[d], X1[d][:], T0[d], T0[d][:], msk, M1, ALU.add)
        kb.tt(X2[d], X2[d][:], T0[d], T0[d][:], msk, M2, ALU.subtract)
        kb.act(DM[d], DM[d][:], X1[d], X1[d][:], AF.Exp, scale=-1.0)
        kb.act(DMT[d], DMT[d][:], X2[d], X2[d][:], AF.Exp)
        kb.tt(QG[d], QG[d][:], qn, qn[:], EBC[d], EBC[d][:], ALU.mult)
        kb.tt(QKM[d], QKM[d][:], pkk, pkk[:, 128:256], DMT[d], DMT[d][:], ALU.mult)
        kb.tt(AD[d], AD[d][:], pkk, pkk[:, 0:128], DM[d], DM[d][:], ALU.mult)
        if stage == 25: return
        mt = MTa[0][d]
        kb.stt(mt, mt[:], AD[d], AD[d][:], NBETA[:, n, d:d + 1], msk, NOTI, ALU.mult, ALU.mult, sr=[NBETA])
        if stage == 261: return
        pm0 = P_m0[d]
        kb.tr(pm0, pm0[:], mt, mt[:], cmt, ident_ap)
        if stage == 262: return
        m = Ma[0][d]
        kb.cp(m, m[:], pm0, pm0[:])
        if stage == 263: return
        r = Ra[0][d]
        kb.tt(r, r[:], m, m[:], cmt, ident_ap, ALU.add)
        if stage == 26: return
        pmm = P_mm[d]; pr = P_r[d]
        for k in range(1, 6):
            mtp = MTa[(k - 1) % 2][d]; mp = Ma[(k - 1) % 2][d]
            mtn = MTa[k % 2][d]; mn = Ma[k % 2][d]
            kb.mm(pmm, pmm[:, 0:128], mp, mp[:], mtp, mtp[:])
            kb.cp(mtn, mtn[:], pmm, pmm[:, 0:128])
            if k < 5:
                kb.mm(pmm, pmm[:, 128:256], mtp, mtp[:], mp, mp[:])
                kb.cp(mn, mn[:], pmm, pmm[:, 128:256], eng="dve")
            rp = Ra[(k - 1) % 2][d]; rn_ = Ra[k % 2][d]
            kb.mm(pr, pr[:], mtn, mtn[:], rp, rp[:])
            kb.tt(rn_, rn_[:], pr, pr[:], rp, rp[:], ALU.add)
        if stage == 27: return
        R = Ra[5 % 2][d]
        puw = P_uw[d]
        kb.mm(puw, puw[:], R, R[:], VB[d], VB[d][:])
        kb.cp(U[d], U[d][:], puw, puw[:])
        kb.mm(puw, puw[:], KBG[d], KBG[d][:], R, R[:])
        kb.cp(WT[d], WT[d][:], puw, puw[:])

    def scan(n, d, h, pp=0):
        U = U2[pp]; WT = WT2[pp]; QG = QG2[pp]; QKM = QKM2[pp]; KEL = KEL2[pp]
        rows = slice(h * 64, (h + 1) * 64)
        Sc = S[spar[d]][d]; Sn = S[1 - spar[d]][d]
        p1 = P_p1[d]; po = P_o[d]; ps_ = P_s[d]
        kb.mm(p1, p1[:], WT[d], WT[d][:], Sc, Sc[:])
        if stage == 291: return
        kb.tt(VN[d], VN[d][rows, :], U[d], U[d][rows, :], p1, p1[rows, :], ALU.subtract)
        if stage == 292: return
        kb.mm(po, po[:], QG[d], QG[d][:], Sc, Sc[:], start=True, stop=False)
        kb.mm(po, po[:], QKM[d], QKM[d][:], VN[d], VN[d][:], start=False, stop=True)
        if stage == 293: return
        kb.cp(OSB[d], OSB[d][rows, :], po, po[rows, :])
        if stage == 294: return
        kb.mm(ps_, ps_[:], KEL[d], KEL[d][:, h, :], VN[d], VN[d][:])
        if stage == 295: return
        kb.stt(Sn, Sn[:], Sc, Sc[:], EGL[:, n, d, h:h + 1], ps_, ps_[:], ALU.mult, ALU.add, sr=[EGL])
        spar[d] = 1 - spar[d]

    def fin(n, d):
        pot = P_ot[d]
        kb.tr(pot, pot[:], OSB[d], OSB[d][:], cmt, ident_ap)
        kb.cp(OTS[d], OTS[d][:], pot, pot[:], eng="dve")
        kb.dma("sp", oS[d][:, n * 128:(n + 1) * 128], OTS[d][:], reads=[OTS[d]], writes=[oSv[d][n]])

    NBC = NB - NBL
    ordF = list(range(NBL, NB)) + list(range(NBL))
    ordB = list(range(NB - 1, NBL - 1, -1)) + list(range(NBL - 1, -1, -1))
    if 20 < stage < 30 or 260 < stage < 300:
        prep(ordF[0], 0)
        if stage == 29 or stage > 290:
            scan(ordF[0], 0, 0)
    if stage == 9:
        kb.rec = []
        prep(ordF[0], 0, 0)
        PF0 = kb.rec
        kb.rec = []
        prep(ordB[0], 1, 0)
        PB0 = kb.rec
        kb.replay(PF0, PB0)
    for t in range(NB if stage != 2 else 1):
        if 20 < stage < 30 or 260 < stage < 300: break
        nf, nb_ = ordF[t], ordB[t]
        pp = t % 2
        kb.rec = []
        scan(nf, 0, 0, pp); scan(nf, 0, 1, pp); fin(nf, 0)
        SF = kb.rec
        kb.rec = []
        scan(nb_, 1, 1, pp); scan(nb_, 1, 0, pp); fin(nb_, 1)
        SB = kb.rec
        PF = []; PB = []
        if t + 1 < NB:
            kb.rec = []
            prep(ordF[t + 1], 0, 1 - pp)
            PF = kb.rec
            kb.rec = []
            prep(ordB[t + 1], 1, 1 - pp)
            PB = kb.rec
        kb.replay(SF, SB, PF, PB)
    if stage in (2, 3) or 20 < stage < 30 or 260 < stage < 300:
        for nm, t in (("dY", Y[0]), ("dQN", QN[0]), ("dKN", KN[0]), ("dU", U2[0][0]), ("dWT", WT2[0][0]), ("dOSB", OSB[0]),
                      ("dR", Ra[1][0]), ("dMT0", MTa[0][0]), ("dOSB1", OSB[1]), ("dS0", S[spar[0]][0])):
            sh = [128, 384] if nm == "dY" else [128, 128]
            o = kb.dram_out(nm, sh)
            kb.dma("sp", o[:], t[:].rearrange("p a b -> p (a b)") if nm == "dY" else t[:], reads=[t], writes=[])
        return kb
    kb.barrier()
    oa = [kb.sb([128, 512], name="oa") for _ in range(2)]
    ob = [kb.sb([128, 512], name="ob") for _ in range(2)]
    gt = [kb.sb([128, 512], name="gt") for _ in range(2)]
    osum = kb.sb([128, 512], name="osum"); sq2 = kb.sb([128, 512], name="sq2"); rs = kb.sb([128, 512], name="rs")
    yo = [kb.sb([128, 512], name="yo") for _ in range(2)]
    pf = v(0, 0, 512)
    for ti, c0 in enumerate(range(0, TT, 512)):
        w = min(512, TT - c0)
        a = oa[ti % 2]; b = ob[ti % 2]; g = gt[ti % 2]; yy = yo[ti % 2]
        blks = range(c0 // 128, (c0 + w) // 128)
        kb.dma("sp", a[:, :w], oS[0][:, c0:c0 + w], reads=[oSv[0][n] for n in blks], writes=[a])
        kb.dma("sp", b[:, :w], oS[1][:, c0:c0 + w], reads=[oSv[1][n] for n in blks], writes=[b])
        kb.dma("sp", g[:, :w], gateT[:, c0:c0 + w], writes=[g])
        kb.tt(osum, osum[:, :w], a, a[:, :w], b, b[:, :w], ALU.add)
        kb.act(sq2, sq2[:, :w], osum, osum[:, :w], AF.Square)
        kb.mm(pf, pf[:, :w], ones, ones[:], sq2, sq2[:, :w])
        kb.act(rs, rs[:, :w], pf, pf[:, :w], AF.Sqrt, extra_reads=[eps_t], bias=eps_t[:, 0:1], scale=1.0 / HD)
        kb.op("dve", lambda e: e.reciprocal(out=rs[:, :w], in_=rs[:, :w]), reads=[rs], writes=[rs])
        kb.act(g, g[:, :w], g, g[:, :w], AF.Silu)
        kb.stt(osum, osum[:, :w], osum, osum[:, :w], cst[:, 4:5], rs, rs[:, :w], ALU.mult, ALU.mult, sr=[cst])
        kb.tt(yy, yy[:, :w], osum, osum[:, :w], g, g[:, :w], ALU.mult)
        kb.dma("sp", yT[:, c0:c0 + w], yy[:, :w], reads=[yy], writes=[])
    return kb


NEXP = 16384
NCH = NEXP // 128


def peer_consts():
    bm = np.zeros((128, 8, 16), np.float32)
    for p in range(128):
        bm[p, p // 16, :] = 1.0
    return bm.reshape(128, 128)


def build_C(NLAT=4096, NCTX=64, TW=256, dbg=False):
    kb = KB()
    NT = NLAT + NCTX
    xT = kb.dram_in("xT", [D, NT])
    mixT = kb.dram_in("mixT", [D, NT])
    cT = kb.dram_in("cT", [128, KC, 2])
    ada_w = kb.dram_in("ada_w", [D, 4 * D])
    ada_bT = kb.dram_in("ada_bT", [128, 48])
    n2T = kb.dram_in("n2T", [128, KC])
    w_out = kb.dram_in("w_out", [D, D])
    wq = kb.dram_in("wq", [D, 2048])
    subkT = kb.dram_in("subkT", [16, 128, 128])
    uTr = kb.dram_in("uTr", [NCH, 128, KC, 128])
    vtab = kb.dram_in("vtab", [NEXP, D])
    bm_d = kb.dram_in("bm", [128, 128])
    id_d = kb.dram_in("ident", [128, 128])
    xoT = kb.dram_out("xoT", [D, NT])
    if dbg:
        d_h2 = kb.dram_out("d_h2", [D, NT])
        d_peer = kb.dram_out("d_peer", [D, NT])
        d_x1 = kb.dram_out("d_x1", [D, NT])

    ones = kb.sb([128, 128], name="ones")
    kb.op("dve", lambda e: e.memset(ones[:], 1.0), writes=[ones])
    eps_t = kb.sb([128, 1], name="eps")
    kb.op("dve", lambda e: e.memset(eps_t[:], 1e-6), writes=[eps_t])
    iot = kb.sb([128, 128], name="iota")
    kb.op("pool", lambda e: e.iota(iot[:], pattern=[[1, 128]], base=0, channel_multiplier=0,
                                   allow_small_or_imprecise_dtypes=True), writes=[iot])
    iot_bf = kb.sb([128, 128], BF16, name="iota_bf")
    kb.cp(iot_bf, iot_bf[:], iot, iot[:], eng="pool")
    bm = kb.sb([128, 128], name="bm")
    kb.dma("sp", bm[:], bm_d[:], writes=[bm])
    ident = kb.sb([128, 128], name="ident")
    kb.dma("sp", ident[:], id_d[:], writes=[ident])
    ident_bf = kb.sb([128, 128], BF16, name="ident_bf")
    kb.cp(ident_bf, ident_bf[:], ident, ident[:], eng="dve")
    subk = kb.sb([128, 16, 128], name="subk")
    kb.dma("sp", subk[:], subkT[:].rearrange("c p q -> p c q"), writes=[subk])
    wo_bf = kb.sb([128, KC, D], BF16, name="wo_bf")
    wov = w_out[:].rearrange("(k p) n -> p k n", p=128)
    for k in range(KC):
        kb.dma("pool", wo_bf[:, k, :], wov[:, k, :], writes=[wo_bf])

    P = [kb.ps([128, 512], name="P") for _ in range(7)]
    P7b = kb.ps([128, 1024], BF16, name="P7b")
    WQW = 256
    wqs = [kb.sb([128, KC, WQW], name="wqs") for _ in range(2)]
    uB = kb.dram_tmp("uB", [NCH, 128, KC, 128], BF16)
    vB = kb.dram_tmp("vB", [NEXP, D], BF16)
    uBv = [uB.view((c,)) for c in range(NCH)]
    vBv = [vB.view((slice(c * 128, (c + 1) * 128),)) for c in range(NCH)]
    mod = emit_mod(kb, cT, ada_w, ada_bT, 16, 48, wt=wqs, bw=WQW, modp=P[0], col_base=2 * D)
    for c in range(NCH):
        kb.dma("pool", uB[c], uTr[c], writes=[uBv[c]])
        kb.dma("pool", vB[c * 128:(c + 1) * 128, :], vtab[c * 128:(c + 1) * 128, :], writes=[vBv[c]])
    n2 = kb.sb([128, KC], name="n2")
    kb.dma("sp", n2[:], n2T[:], writes=[n2])
    a_sc = kb.sb([128, KC, 2], name="a_sc")
    for s in range(2):
        kb.stt(a_sc, a_sc[:, :, s], mod, mod[:, 16:24, s], 1.0, n2, n2[:], ALU.add, ALU.mult)

    wqv = wq[:].rearrange("(k p) n -> p k n", p=128)
    xt = kb.sb([128, KC, TW], name="xt")
    mx_bf = kb.sb([128, KC, TW], BF16, name="mx_bf")
    h2 = kb.sb([128, KC, TW], name="h2")
    h2_bf = kb.sb([128, KC, TW], BF16, name="h2_bf")
    sqs = [kb.sb([128, TW], name="sq") for _ in range(2)]
    tmps = [kb.sb([128, TW], name="tmp") for _ in range(2)]
    rstd = kb.sb([128, TW], name="rstd")
    qps = [kb.sb([128, 2, 128], name="qps") for _ in range(2)]
    s_sb = kb.sb([128, 16, 128], name="s_sb")
    cand = kb.sb([128, 16, 8, 16], name="cand")
    mtmp = kb.sb([128, 128], name="mtmp")
    ctmp = kb.sb([128, 16, 16], name="ctmp")
    sv = kb.sb([128, 8, 2, 16], name="sv")
    si = kb.sb([128, 8, 2, 16], U32, name="si")
    sif = kb.sb([128, 2, 8, 16], name="sif")
    c16 = kb.sb([128, 8, 16], name="c16")
    nmx = kb.sb([128, 8], name="nmx")
    zz = kb.sb([128, 8], name="zz")
    w_bf = kb.sb([128, 16, 8, 16], BF16, name="w_bf")
    WT = kb.sb([128, 16, TW], BF16, name="WT")
    SIT = kb.sb([128, 2, TW], name="SIT")
    GT = kb.sb([128, TW, NCH], BF16, name="GT")
    oh1q = [kb.sb([128, 4, 128], BF16, name="oh1q") for _ in range(3)]
    oh2q = [kb.sb([128, 4, 128], BF16, name="oh2q") for _ in range(3)]
    wbq = [kb.sb([128, 4, 128], BF16, name="wbq") for _ in range(3)]
    XS = [kb.sb([128, 4, 128], BF16, name="XS") for _ in range(3)]
    NUB = 2 if dbg else 3
    dbgp = kb.sb([128, TW], name="dbgp") if dbg else None
    u_bf = [kb.sb([128, KC, 128], BF16, name="u_bf") for _ in range(NUB)]
    v_bf = [kb.sb([128, D], BF16, name="v_bf") for _ in range(NUB)]
    gl = [kb.sb([128, TW], name="gl") for _ in range(2)]
    ga = [kb.sb([128, TW], BF16, name="ga") for _ in range(2)]
    ost = [kb.sb([128, TW], name="ost") for _ in range(2)]
    xv = xT[:].rearrange("(k p) n -> p k n", p=128)
    mv = mixT[:].rearrange("(k p) n -> p k n", p=128)

    tiles = [(t0, TW, 0) for t0 in range(0, NLAT, TW)]
    if NCTX:
        tiles.append((NLAT, NCTX, 1))
    nld = 0
    for ti, (t0, ncol, s) in enumerate(tiles):
        for k in range(KC):
            kb.dma("sp", xt[:, k, :ncol], xv[:, k, t0:t0 + ncol], writes=[xt])
            kb.dma("pool", mx_bf[:, k, :ncol], mv[:, k, t0:t0 + ncol], writes=[mx_bf])
        for m in range(KC):
            pp = P[4 + m % 2]
            for k in range(KC):
                kb.mm(pp, pp[:, :ncol], wo_bf, wo_bf[:, k, m * 128:(m + 1) * 128], mx_bf, mx_bf[:, k, :ncol],
                      start=(k == 0), stop=(k == KC - 1))
            kb.stt(xt, xt[:, m, :ncol], pp, pp[:, :ncol], mod[:, m, s:s + 1], xt, xt[:, m, :ncol], ALU.mult, ALU.add, sr=[mod])
        if dbg:
            for k in range(KC):
                kb.dma("sp", d_x1[k * 128:(k + 1) * 128, t0:t0 + ncol], xt[:, k, :ncol], reads=[xt], writes=[])
        ssp = P[6]
        for k in range(KC):
            sq = sqs[k % 2]
            kb.act(sq, sq[:, :ncol], xt, xt[:, k, :ncol], AF.Square)
            kb.mm(ssp, ssp[:, :ncol], ones, ones[:], sq, sq[:, :ncol], start=(k == 0), stop=(k == KC - 1))
        kb.act(rstd, rstd[:, :ncol], ssp, ssp[:, :ncol], AF.Sqrt, extra_reads=[eps_t], bias=eps_t[:, 0:1], scale=1.0 / D)
        kb.op("dve", lambda e: e.reciprocal(out=rstd[:, :ncol], in_=rstd[:, :ncol]), reads=[rstd], writes=[rstd])
        for k in range(KC):
            tmp = tmps[k % 2]
            kb.stt(tmp, tmp[:, :ncol], xt, xt[:, k, :ncol], a_sc[:, k, s:s + 1], rstd, rstd[:, :ncol], ALU.mult, ALU.mult, sr=[a_sc])
            kb.act(h2, h2[:, k, :ncol], tmp, tmp[:, :ncol], AF.Identity, extra_reads=[mod], bias=mod[:, 8 + k, s:s + 1], scale=1.0)
        kb.cp(h2_bf, h2_bf[:, :, :ncol], h2, h2[:, :, :ncol], eng="pool")
        if dbg:
            for k in range(KC):
                kb.dma("sp", d_h2[k * 128:(k + 1) * 128, t0:t0 + ncol], h2[:, k, :ncol], reads=[h2], writes=[])
        for sb0 in range(0, ncol, 128):
            nt = min(128, ncol - sb0)
            for hb in range(0, 16, 2):
                wt_ = wqs[nld % 2]; nld += 1
                kb.dma("sp", wt_[:], wqv[:, :, hb * 128:hb * 128 + WQW], writes=[wt_])
                qpp = P[4 + (hb // 2) % 2]
                for j in range(2):
                    for k in range(KC):
                        kb.mm(qpp, qpp[:, j * 128:j * 128 + nt], wt_, wt_[:, k, j * 128:(j + 1) * 128],
                              h2, h2[:, k, sb0:sb0 + nt], start=(k == 0), stop=(k == KC - 1))
                qp = qps[(hb // 2) % 2]
                kb.cp(qp, qp[:, :, :nt], qpp, qpp[:, 0:256].rearrange("p (a b) -> p a b", b=128)[:, :, :nt])
                for j in range(2):
                    hp = hb + j
                    sp_ = P[hp // 4]
                    kb.mm(sp_, sp_[:nt, (hp % 4) * 128:(hp % 4 + 1) * 128], qp, qp[:, j, :nt], subk, subk[:, hp, :])
            for bnk in range(4):
                kb.cp(s_sb, s_sb[:nt, bnk * 4:(bnk + 1) * 4, :], P[bnk], P[bnk][:nt, :].rearrange("p (a b) -> p a b", b=128),
                      eng=("act" if bnk % 2 == 0 else "dve"))
            for hp in range(16):
                h, p_ = hp // 2, hp % 2
                sv8a = sv[:nt, h, p_, 0:8]; sv8b = sv[:nt, h, p_, 8:16]
                kb.op("dve", lambda e, hp=hp, o=sv8a: e.max(out=o, in_=s_sb[:nt, hp, :]), reads=[s_sb], writes=[sv])
                kb.op("dve", lambda e, hp=hp, o=sv8a, h=h, p_=p_: e.max_index(out=si[:nt, h, p_, 0:8], in_max=o, in_values=s_sb[:nt, hp, :]),
                      reads=[s_sb, sv], writes=[si])
                kb.op("dve", lambda e, hp=hp, o=sv8a: e.match_replace(out=mtmp[:nt, :], in_to_replace=o, in_values=s_sb[:nt, hp, :], imm_value=-1e30),
                      reads=[s_sb, sv], writes=[mtmp])
                kb.op("dve", lambda e, o=sv8b: e.max(out=o, in_=mtmp[:nt, :]), reads=[mtmp], writes=[sv])
                kb.op("dve", lambda e, o=sv8b, h=h, p_=p_: e.max_index(out=si[:nt, h, p_, 8:16], in_max=o, in_values=mtmp[:nt, :]),
                      reads=[mtmp, sv], writes=[si])
            sva = sv[:nt, :, 0, :]
            svb = sv[:nt, :, 1, :]
            in0 = bass.AP(sva.tensor, sva.offset, [list(sva.ap[0]), [1, 16], [32, 8], [0, 16]])
            in1 = bass.AP(svb.tensor, svb.offset, [list(svb.ap[0]), [0, 16], [32, 8], [1, 16]])
            kb.op("dve", lambda e: e.tensor_tensor(out=cand[:nt], in0=in0, in1=in1, op=ALU.add), reads=[sv], writes=[cand])
            for h in range(8):
                ch = cand[:nt, :, h, :]
                kb.op("dve", lambda e, h=h, ch=ch: e.max(out=c16[:nt, h, 0:8], in_=ch), reads=[cand], writes=[c16])
                kb.op("dve", lambda e, h=h, ch=ch: e.match_replace(out=ctmp[:nt], in_to_replace=c16[:nt, h, 0:8], in_values=ch, imm_value=-1e30),
                      reads=[cand, c16], writes=[ctmp])
                kb.op("dve", lambda e, h=h: e.max(out=c16[:nt, h, 8:16], in_=ctmp[:nt]), reads=[ctmp], writes=[c16])
            kb.ts(nmx, nmx[:nt, :], c16, c16[:nt, :, 0], -1.0, ALU.mult)
            E = s_sb
            Ev = E[:nt].rearrange("p a b -> p (a b)").rearrange("p (a h b) -> p a h b", a=16, h=8)
            for h in range(8):
                kb.act(E, Ev[:, :, h, :], cand, cand[:nt, :, h, :], AF.Exp, extra_reads=[nmx], bias=nmx[:nt, h:h + 1], scale=1.0)
            for h in range(8):
                kb.stt(E, Ev[:, :, h, :], cand, cand[:nt, :, h, :], c16[:nt, h, 15:16], E, Ev[:, :, h, :], ALU.is_ge, ALU.mult, sr=[c16])
            Eperm = bass.AP(Ev.tensor, Ev.offset, [list(Ev.ap[0]), [16, 8], [128, 16], [1, 16]])
            kb.op("dve", lambda e: e.tensor_reduce(out=zz[:nt, :], in_=Eperm, axis=AX.XY, op=ALU.add), reads=[E], writes=[zz])
            kb.op("dve", lambda e: e.reciprocal(out=zz[:nt, :], in_=zz[:nt, :]), reads=[zz], writes=[zz])
            zb = zz[:nt, :]
            zbc = bass.AP(zb.tensor, zb.offset, [list(zb.ap[0]), [0, 16], [1, 8], [0, 16]])
            kb.op("dve", lambda e: e.tensor_tensor(out=w_bf[:nt], in0=Ev, in1=zbc, op=ALU.mult), reads=[E, zz], writes=[w_bf])
            for p_ in range(2):
                kb.cp(sif, sif[:nt, p_, :, :], si, si[:nt, :, p_, :], eng="dve")
            for p_ in range(2):
                tp = P[6]
                kb.tr(tp, tp[:, 0:nt], sif, sif[:nt, p_, :, :].rearrange("p a b -> p (a b)"), ident, ident[:nt, :nt])
                kb.cp(SIT, SIT[:, p_, sb0:sb0 + nt], tp, tp[:, 0:nt], eng="dve")
            for a0 in range(0, 16, 4):
                for aa in range(4):
                    a = a0 + aa
                    kb.tr(P7b, P7b[:, aa * 128:aa * 128 + nt], w_bf, w_bf[:nt, a, :, :].rearrange("p a b -> p (a b)"),
                          ident_bf, ident_bf[:nt, :nt])
                kb.cp(WT, WT[:, a0:a0 + 4, sb0:sb0 + nt], P7b, P7b[:, 0:512].rearrange("p (a b) -> p a b", b=128)[:, :, :nt])
        for tq in range(0, ncol, 4):
            g4 = (tq // 4) % 3
            XP = P[g4]; GP = P[3 + g4]
            xs = XS[g4]
            o2 = oh2q[g4]; o1 = oh1q[g4]; w_ = wbq[g4]
            for tt_ in range(4):
                t = tq + tt_
                kb.ts(o2, o2[:, tt_, :], iot_bf, iot_bf[:], SIT[:, 1, t:t + 1], ALU.is_equal, sr=[SIT])
                kb.ts(o1, o1[:, tt_, :], iot_bf, iot_bf[:], SIT[:, 0, t:t + 1], ALU.is_equal, sr=[SIT])
            for tt_ in range(4):
                wc = WT[:, :, tq + tt_]
                wbc = bass.AP(wc.tensor, wc.offset, [list(wc.ap[0]), [0, 8], [TW, 16]])
                kb.op("pool", lambda e, w_=w_, wbc=wbc, tt_=tt_: e.tensor_tensor(
                    out=w_[:, tt_, :].rearrange("p (a b) -> p a b", b=16),
                    in0=bm[:].rearrange("p (a b) -> p a b", b=16), in1=wbc, op=ALU.mult),
                    reads=[bm, WT], writes=[w_])
            for tt_ in range(4):
                kb.mm(XP, XP[:, tt_ * 128:(tt_ + 1) * 128], w_, w_[:, tt_, :], o2, o2[:, tt_, :])
            kb.cp(xs, xs[:], XP, XP[:].rearrange("p (a b) -> p a b", b=128))
            for tt_ in range(4):
                kb.mm(GP, GP[:, tt_ * 128:(tt_ + 1) * 128], xs, xs[:, tt_, :], o1, o1[:, tt_, :])
            kb.op("act", lambda e, GP=GP, tq=tq: e.activation(out=GT[:, tq:tq + 4, :], in_=GP[:].rearrange("p (a b) -> p a b", b=128), func=AF.Copy),
                  reads=[GP], writes=[GT])
        for c in range(NCH):
            ub = u_bf[c % NUB]; vb = v_bf[c % NUB]
            kb.dma("sp", ub[:], uB[c], reads=[uBv[c]], writes=[ub])
            kb.dma("sp", vb[:], vB[c * 128:(c + 1) * 128, :], reads=[vBv[c]], writes=[vb])
            ap_ = P[4 + c % 2]
            for k in range(KC):
                kb.mm(ap_, ap_[:, :ncol], ub, ub[:, k, :], h2_bf, h2_bf[:, k, :ncol], start=(k == 0), stop=(k == KC - 1))
            g_ = gl[c % 2]; a_ = ga[c % 2]
            kb.act(g_, g_[:, :ncol], ap_, ap_[:, :ncol], AF.Gelu)
            kb.tt(a_, a_[:, :ncol], g_, g_[:, :ncol], GT, GT[:, :ncol, c], ALU.mult, eng="pool")
            for m in range(KC):
                op_ = P[m // 2]
                kb.mm(op_, op_[:, (m % 2) * 256:(m % 2) * 256 + ncol], vb, vb[:, m * 128:(m + 1) * 128], a_, a_[:, :ncol],
                      start=(c == 0 and m % 2 == 0), stop=(c == NCH - 1), skip=True)
        for m in range(KC):
            op_ = P[m // 2]
            o_ = ost[m % 2]
            if dbg:
                o2_ = dbgp
                kb.cp(o2_, o2_[:, :ncol], op_, op_[:, (m % 2) * 256:(m % 2) * 256 + ncol], eng="dve")
                kb.dma("sp", d_peer[m * 128:(m + 1) * 128, t0:t0 + ncol], o2_[:, :ncol], reads=[o2_], writes=[])
            kb.stt(o_, o_[:, :ncol], op_, op_[:, (m % 2) * 256:(m % 2) * 256 + ncol], mod[:, 24 + m, s:s + 1], xt, xt[:, m, :ncol],
                   ALU.mult, ALU.add, sr=[mod])
            kb.dma("sp", xoT[m * 128:(m + 1) * 128, t0:t0 + ncol], o_[:, :ncol], reads=[o_], writes=[])
    return kb


DEPTH = 4
B_ = 2
T_ = 16384
CTX_ = 256
NCORE = 8
NLAT_C = T_ // 4
NCTX_C = CTX_ // 4


def _lay_c(c_b, c_ctx):
    cT = np.zeros((128, 8, 2), np.float32)
    cT[:, :, 0] = c_b.reshape(8, 128).T
    cT[:, :, 1] = c_ctx.reshape(8, 128).T
    return cT


def _fm(v):
    return np.ascontiguousarray(np.asarray(v, np.float32).reshape(-1, 128).T)


def _dn_inputs(pb, j, conv_w, A_log, dt_bias, dn_norm_w, cm, TL=T_, TC=CTX_):
    PADW = TL + 4 + TC + 4
    qkvP = np.zeros((128, 3, PADW), np.float32)
    cwT = np.zeros((128, 3, 5), np.float32)
    for w in range(3):
        rows = slice(1024 + w * 512 + j * 128, 1024 + w * 512 + (j + 1) * 128)
        qkvP[:, w, 2:2 + TL] = pb[rows, :TL]
        qkvP[:, w, TL + 6:TL + 6 + TC] = pb[rows, TL:]
        cwT[:, w, :] = conv_w[:, w * 512 + j * 128: w * 512 + (j + 1) * 128].T
    gateT = np.ascontiguousarray(pb[2560 + j * 128: 2560 + (j + 1) * 128])
    baT = np.ascontiguousarray(pb[[3072 + j, 3076 + j, 3080 + j, 3084 + j]])
    cst = np.zeros((128, 8), np.float32)
    cst[:, 0] = A_log[0, j]; cst[:, 1] = A_log[1, j]; cst[:, 2] = dt_bias[0, j]; cst[:, 3] = dt_bias[1, j]
    cst[:, 4] = dn_norm_w
    return {"qkvP": qkvP, "gateT": gateT, "baT": baT, "cwT": cwT, "cst": cst, "cm": cm}


def kernel(x, c, ctx, c_ctx, ada_w, ada_b, norm1_w, norm2_w, w_in, attn_qnorm_w, attn_knorm_w,
           dn_conv_w, dn_A_log, dn_dt_bias, dn_norm_w, w_out, peer_wq, peer_subkeys, peer_u, peer_v):
    f32 = np.float32
    x = np.asarray(x, f32); ctx = np.asarray(ctx, f32); c = np.asarray(c, f32); c_ctx = np.asarray(c_ctx, f32)
    ada_w = np.asarray(ada_w, f32); ada_b = np.asarray(ada_b, f32)
    w_in = np.asarray(w_in, f32); w_out = np.asarray(w_out, f32)
    peer_wq = np.asarray(peer_wq, f32); peer_subkeys = np.asarray(peer_subkeys, f32)
    peer_u = np.asarray(peer_u, f32); peer_v = np.asarray(peer_v, f32)
    dn_conv_w = np.asarray(dn_conv_w, f32)

    XT = []
    for core in range(NCORE):
        b, r = core // 4, core % 4
        xs = np.concatenate([x[b, r * NLAT_C:(r + 1) * NLAT_C], ctx[b, r * NCTX_C:(r + 1) * NCTX_C]], axis=0)
        XT.append(np.ascontiguousarray(xs.T))
    cTs = [_lay_c(c[core // 4], c_ctx) for core in range(NCORE)]
    cos, sin = rope_tables()
    cosT = np.ascontiguousarray(cos.T); sinT = np.ascontiguousarray(sin.T)
    RmT = rot_matrix_T(); ident = np.eye(128, dtype=f32)
    cm = dn_consts(); bmc = peer_consts()

    for l in range(DEPTH):
        ada_bT = np.ascontiguousarray(ada_b[l].reshape(48, 128).T)
        kbA = build_A()
        adaA = np.ascontiguousarray(ada_w[l][:, :2048])
        n1T = _fm(norm1_w[l])
        resA = kbA.run([{"xT": XT[core], "cT": cTs[core], "ada_w": adaA, "ada_bT": ada_bT, "n1T": n1T, "w_in": w_in[l]}
                        for core in range(NCORE)]).results
        pbs = []
        for b in range(B_):
            cs = range(4 * b, 4 * b + 4)
            pbs.append(np.concatenate([resA[cc]["pT"][:, :NLAT_C] for cc in cs] + [resA[cc]["pT"][:, NLAT_C:] for cc in cs], axis=1))
        del resA
        kbB = build_attn()
        qkw = np.stack([np.asarray(attn_qnorm_w[l], f32), np.asarray(attn_knorm_w[l], f32)], axis=1).astype(f32)
        mapsB = []
        for core in range(NCORE):
            b, j = core // 4, core % 4
            pb = pbs[b]
            mapsB.append({"qT": np.ascontiguousarray(pb[j * 128:(j + 1) * 128]),
                          "kT": np.ascontiguousarray(pb[512 + (j // 2) * 128: 512 + (j // 2 + 1) * 128]),
                          "vT": np.ascontiguousarray(pb[768 + (j // 2) * 128: 768 + (j // 2 + 1) * 128]),
                          "cosT": cosT, "sinT": sinT, "RmT": RmT, "ident": ident, "qkw": qkw})
        resB = kbB.run(mapsB).results
        del mapsB
        kbD = build_dn()
        mapsD = [_dn_inputs(pbs[core // 4], core % 4, dn_conv_w[l], np.asarray(dn_A_log[l], f32),
                            np.asarray(dn_dt_bias[l], f32), np.asarray(dn_norm_w[l], f32), cm) for core in range(NCORE)]
        resD = kbD.run(mapsD).results
        del mapsD, pbs
        mixb = []
        for b in range(B_):
            mixb.append(np.concatenate([resB[4 * b + j]["oT"] for j in range(4)] + [resD[4 * b + j]["yT"] for j in range(4)], axis=0))
        del resB, resD
        kbC = build_C()
        adaC = np.ascontiguousarray(ada_w[l][:, 2048:])
        n2T = _fm(norm2_w[l])
        subkT = np.ascontiguousarray(peer_subkeys[l].reshape(16, 128, 128).transpose(0, 2, 1))
        uTr = np.ascontiguousarray(peer_u[l].reshape(128, 128, 8, 128).transpose(0, 3, 2, 1))
        mapsC = []
        for core in range(NCORE):
            b, r = core // 4, core % 4
            mx = np.concatenate([mixb[b][:, r * NLAT_C:(r + 1) * NLAT_C], mixb[b][:, T_ + r * NCTX_C: T_ + (r + 1) * NCTX_C]], axis=1)
            mapsC.append({"xT": XT[core], "mixT": np.ascontiguousarray(mx), "cT": cTs[core], "ada_w": adaC, "ada_bT": ada_bT,
                          "n2T": n2T, "w_out": w_out[l], "wq": peer_wq[l], "subkT": subkT, "uTr": uTr, "vtab": peer_v[l],
                          "bm": bmc, "ident": ident})
        resC = kbC.run(mapsC).results
        del mapsC, mixb, uTr
        XT = [np.ascontiguousarray(resC[core]["xoT"]) for core in range(NCORE)]
        del resC

    out = np.empty((B_, T_, 1024), f32)
    for core in range(NCORE):
        b, r = core // 4, core % 4
        out[b, r * NLAT_C:(r + 1) * NLAT_C, :] = XT[core][:, :NLAT_C].T
    return out
```

```python
import numpy as np
import concourse.bass as bass
import concourse.mybir as mybir
from concourse.bass_utils import run_bass_kernel_spmd

F32 = mybir.dt.float32
BF16 = mybir.dt.bfloat16
U32 = mybir.dt.uint32
AF = mybir.ActivationFunctionType
ALU = mybir.AluOpType
AX = mybir.AxisListType


class Tile:
    def __init__(self, h, is_dram=False, base=None):
        self.h = h
        self.lw = None
        self.rd = {}
        self.is_dram = is_dram
        if base is not None:
            self._ap = base
        else:
            self._ap = h.ap() if is_dram else h[:]

    def __getitem__(self, idx):
        return self._ap[idx]

    excl = False

    def view(self, idx):
        return Tile(self.h, self.is_dram, base=self._ap[idx])

    def sub(self, idx):
        return SubTile(self, self._ap[idx])


class SubTile:
    def __init__(self, parent, ap):
        self.parent = parent
        self._ap = ap

    def __getitem__(self, idx):
        return self._ap[idx]

    @property
    def excl(self):
        return self.parent.excl

    @property
    def lw(self):
        return self.parent.lw

    @lw.setter
    def lw(self, v):
        self.parent.lw = v

    @property
    def rd(self):
        return self.parent.rd

    @rd.setter
    def rd(self, v):
        self.parent.rd = v


class KB:
    ND = 24

    def __init__(self, num_devices=None):
        if num_devices:
            nc = bass.Bass("TRN2", target_bir_lowering=False, num_devices=num_devices)
        else:
            nc = bass.Bass("TRN2", target_bir_lowering=False)
        self.nc = nc
        self.engs = {"pe": nc.tensor, "dve": nc.vector, "act": nc.scalar,
                     "pool": nc.gpsimd, "sp": nc.sync}
        self.sems = {k: nc.alloc_semaphore("s_" + k) for k in self.engs}
        self.cnt = {k: 0 for k in self.engs}
        self.known = {k: {} for k in self.engs}
        self.dsem = []
        self.dq = {}
        for q, n in (("sp", 16), ("pool", 12), ("act", 2), ("dve", 2), ("pe", 2)):
            self.dq[q] = []
            for i in range(n):
                k = "d%s%d" % (q, i)
                self.sems[k] = nc.alloc_semaphore("s_" + k)
                self.dsem.append(k)
                self.dq[q].append(k)
        self.dval = {k: 0 for k in self.dsem}
        self.dnext = {q: 0 for q in self.dq}
        self.nuniq = 0
        try:
            nc.allow_low_precision("bf16 matmul operands, fp32 accumulate")
        except Exception:
            pass
        try:
            nc.allow_non_contiguous_dma("strided layouts")
        except Exception:
            pass

    def _nm(self, p):
        self.nuniq += 1
        return "%s_%d" % (p, self.nuniq)

    def sb(self, shape, dt=F32, name="sb"):
        return Tile(self.nc.alloc_sbuf_tensor(self._nm(name), list(shape), dt))

    def ps(self, shape, dt=F32, name="ps"):
        t = Tile(self.nc.alloc_psum_tensor(self._nm(name), list(shape), dt))
        t.excl = True
        return t

    def dram_in(self, name, shape, dt=F32):
        return Tile(self.nc.dram_tensor(name, list(shape), dt, kind="ExternalInput"), True)

    def dram_out(self, name, shape, dt=F32):
        return Tile(self.nc.dram_tensor(name, list(shape), dt, kind="ExternalOutput"), True)

    def dram_tmp(self, name, shape, dt=F32):
        return Tile(self.nc.dram_tensor(name, list(shape), dt, kind="Internal"), True)

    def _waits(self, eng, reads, writes):
        need = {}

        def req(ev):
            if ev is None:
                return
            k, v = ev
            if k == "pe" and eng == "pe":
                return
            if need.get(k, 0) < v:
                need[k] = v

        for t in reads:
            req(t.lw)
        for t in writes:
            req(t.lw)
            for k, v in t.rd.items():
                req((k, v))
        e = self.engs[eng]
        kn = self.known[eng]
        for k, v in need.items():
            if kn.get(k, 0) >= v:
                continue
            e.wait_ge(self.sems[k], v)
            kn[k] = v

    def _mark(self, ev, reads, writes):
        k, v = ev
        for t in reads:
            if t.rd.get(k, 0) < v:
                t.rd[k] = v
        for t in writes:
            t.lw = ev
            t.rd = {}

    rec = None

    def replay(self, *lists):
        self.rec = None
        n = max(len(l) for l in lists)
        for i in range(n):
            for l in lists:
                if i < len(l):
                    kind, a, fn, r, w, kw = l[i]
                    if kind == "op":
                        self.op(a, fn, r, w)
                    else:
                        self.dma(a, fn[0], fn[1], r, w, **kw)

    def op(self, eng, fn, reads=(), writes=()):
        if self.rec is not None:
            self.rec.append(("op", eng, fn, list(reads), list(writes), None))
            return
        xr = [t for t in reads if t.excl]
        if xr:
            reads = [t for t in reads if not t.excl]
            writes = list(writes) + xr
        self._waits(eng, reads, writes)
        inst = fn(self.engs[eng])
        self.cnt[eng] += 1
        inst.then_inc(self.sems[eng], 1)
        self._mark((eng, self.cnt[eng]), reads, writes)

    def dma(self, q, out, in_, reads=(), writes=(), **kw):
        if self.rec is not None:
            self.rec.append(("dma", q, (out, in_), list(reads), list(writes), kw))
            return
        pool_ = self.dq[q]
        k = pool_[self.dnext[q]]
        self.dnext[q] = (self.dnext[q] + 1) % len(pool_)
        e = self.engs[q]
        kn = self.known[q]
        if kn.get(k, 0) < self.dval[k]:
            e.wait_ge(self.sems[k], self.dval[k])
            kn[k] = self.dval[k]
        self._waits(q, reads, writes)
        inst = e.dma_start(out=out, in_=in_, **kw)
        self.dval[k] += 16
        inst.then_inc(self.sems[k], 16)
        self._mark((k, self.dval[k]), reads, writes)

    def barrier(self):
        for q, e in self.engs.items():
            kn = self.known[q]
            for k in self.dsem:
                if self.dval[k] > kn.get(k, 0):
                    e.wait_ge(self.sems[k], self.dval[k])
                    kn[k] = self.dval[k]
            for k in ("pe", "dve", "act", "pool", "sp"):
                if k != q and self.cnt[k] > kn.get(k, 0):
                    e.wait_ge(self.sems[k], self.cnt[k])
                    kn[k] = self.cnt[k]

    def finish(self):
        e = self.engs["sp"]
        kn = self.known["sp"]
        for k in self.dsem:
            if self.dval[k] > kn.get(k, 0):
                e.wait_ge(self.sems[k], self.dval[k])
                kn[k] = self.dval[k]
        for k in ("pe", "dve", "act", "pool"):
            if self.cnt[k] > kn.get(k, 0):
                e.wait_ge(self.sems[k], self.cnt[k])
                kn[k] = self.cnt[k]

    def mm(self, out_t, out_ap, lhsT_t, lhsT_ap, rhs_t, rhs_ap, start=True, stop=True, skip=False):
        if skip:
            self.op("pe", lambda e: e.matmul(out_ap, lhsT_ap, rhs_ap, start=start, stop=stop, skip_group_check=True),
                    reads=[lhsT_t, rhs_t], writes=[out_t])
        else:
            self.op("pe", lambda e: e.matmul(out_ap, lhsT_ap, rhs_ap, start=start, stop=stop),
                    reads=[lhsT_t, rhs_t], writes=[out_t])

    def tr(self, out_t, out_ap, in_t, in_ap, ident_t, ident_ap):
        self.op("pe", lambda e: e.transpose(out_ap, in_ap, ident_ap),
                reads=[in_t, ident_t], writes=[out_t])

    def act(self, out_t, out_ap, in_t, in_ap, func, extra_reads=(), eng="act", **kw):
        self.op(eng, lambda e: e.activation(out=out_ap, in_=in_ap, func=func, **kw),
                reads=[in_t] + list(extra_reads), writes=[out_t])

    def tt(self, ot, oap, at, aap, bt, bap, op, eng="dve"):
        self.op(eng, lambda e: e.tensor_tensor(out=oap, in0=aap, in1=bap, op=op), reads=[at, bt], writes=[ot])

    def ts(self, ot, oap, at, aap, s1, op0, s2=None, op1=None, sr=(), eng="dve"):
        if op1 is None:
            self.op(eng, lambda e: e.tensor_scalar(out=oap, in0=aap, scalar1=s1, scalar2=None, op0=op0),
                    reads=[at] + list(sr), writes=[ot])
        else:
            self.op(eng, lambda e: e.tensor_scalar(out=oap, in0=aap, scalar1=s1, scalar2=s2, op0=op0, op1=op1),
                    reads=[at] + list(sr), writes=[ot])

    def stt(self, ot, oap, at, aap, sc, bt, bap, op0, op1, sr=()):
        self.op("dve", lambda e: e.scalar_tensor_tensor(out=oap, in0=aap, scalar=sc, in1=bap, op0=op0, op1=op1),
                reads=[at, bt] + list(sr), writes=[ot])

    def cp(self, ot, oap, it, iap, eng="act"):
        if eng == "act":
            self.op("act", lambda e: e.activation(out=oap, in_=iap, func=AF.Copy), reads=[it], writes=[ot])
        else:
            self.op(eng, lambda e: e.tensor_copy(out=oap, in_=iap), reads=[it], writes=[ot])

    def run(self, in_maps, n=8, trace=False):
        self.finish()
        if trace:
            return run_bass_kernel_spmd(self.nc, in_maps, core_ids=list(range(n)), trace=True)
        return run_bass_kernel_spmd(self.nc, in_maps, core_ids=list(range(n)))


D = 1024
KC = 8
INW = 3088


def emit_mod(kb, cT, ada_w, ada_bT, j0, j1, wt=None, bw=512, modp=None, col_base=0):
    nj = j1 - j0
    sc = kb.sb([128, KC, 2], name="silu_c")
    craw = kb.sb([128, KC, 2], name="craw")
    kb.dma("sp", craw[:], cT[:], writes=[craw])
    kb.act(sc, sc[:], craw, craw[:], AF.Silu)
    if modp is None:
        modp = kb.ps([128, nj, 2], name="modp")
    else:
        modp = modp.sub((slice(None), slice(0, nj * 2)))
        modp = SubTile(modp.parent, modp[:].rearrange("p (a b) -> p a b", b=2))
    if wt is None:
        wt = [kb.sb([128, KC, bw], name="adaw") for _ in range(2)]
    ada_v = ada_w[:].rearrange("(k p) n -> p k n", p=128)
    npb = bw // 128
    for jb in range(0, nj, npb):
        t = wt[(jb // npb) % 2]
        c0 = (j0 + jb) * 128 - col_base
        kb.dma("sp", t[:], ada_v[:, :, c0:c0 + bw], writes=[t])
        for jj in range(npb):
            j = jb + jj
            for k in range(KC):
                kb.mm(modp, modp[:, j, :], t, t[:, k, jj * 128:(jj + 1) * 128], sc, sc[:, k, :],
                      start=(k == 0), stop=(k == KC - 1))
    bT = kb.sb([128, nj], name="adab")
    kb.dma("sp", bT[:], ada_bT[:, j0:j1], writes=[bT])
    mod = kb.sb([128, nj, 2], name="mod")
    for s in range(2):
        kb.op("dve", lambda e, s=s: e.tensor_tensor(out=mod[:, :, s], in0=modp[:, :, s], in1=bT[:], op=ALU.add),
              reads=[modp, bT], writes=[mod])
    return mod


def emit_norm_mod(kb, xt, hT, ones, eps_t, a_sc, b_sh, s, ncol, sqs, ssp, rstd, hdt_tmp):
    for k in range(KC):
        sq = sqs[k % 2]
        kb.act(sq, sq[:, :ncol], xt, xt[:, k, :ncol], AF.Square)
        kb.mm(ssp, ssp[:, :ncol], ones, ones[:], sq, sq[:, :ncol], start=(k == 0), stop=(k == KC - 1))
    kb.act(rstd, rstd[:, :ncol], ssp, ssp[:, :ncol], AF.Sqrt, extra_reads=[eps_t], bias=eps_t[:, 0:1], scale=1.0 / D)
    kb.op("dve", lambda e: e.reciprocal(out=rstd[:, :ncol], in_=rstd[:, :ncol]), reads=[rstd], writes=[rstd])
    for k in range(KC):
        tmp = hdt_tmp[k % 2]
        kb.op("dve", lambda e, k=k, tmp=tmp: e.scalar_tensor_tensor(
            out=tmp[:, :ncol], in0=xt[:, k, :ncol], scalar=a_sc[:, k, s:s + 1], in1=rstd[:, :ncol],
            op0=ALU.mult, op1=ALU.mult), reads=[xt, a_sc, rstd], writes=[tmp])
        kb.act(hT, hT[:, k, :ncol], tmp, tmp[:, :ncol], AF.Identity, extra_reads=[b_sh],
               bias=b_sh[:, k, s:s + 1], scale=1.0)


def build_A(NLAT=4096, NCTX=64):
    kb = KB()
    NT = NLAT + NCTX
    xT = kb.dram_in("xT", [D, NT])
    cT = kb.dram_in("cT", [128, KC, 2])
    ada_w = kb.dram_in("ada_w", [D, 2 * D])
    ada_bT = kb.dram_in("ada_bT", [128, 48])
    n1T = kb.dram_in("n1T", [128, KC])
    w_in = kb.dram_in("w_in", [D, INW])
    pT = kb.dram_out("pT", [INW, NT])

    ones = kb.sb([128, 128], name="ones")
    kb.op("dve", lambda e: e.memset(ones[:], 1.0), writes=[ones])
    eps_t = kb.sb([128, 1], name="eps")
    kb.op("dve", lambda e: e.memset(eps_t[:], 1e-6), writes=[eps_t])

    w_bf = kb.sb([128, KC, INW], BF16, name="w_bf")
    wv = w_in[:].rearrange("(k p) n -> p k n", p=128)
    for k in range(KC):
        kb.dma("pool", w_bf[:, k, :], wv[:, k, :], writes=[w_bf])

    mod = emit_mod(kb, cT, ada_w, ada_bT, 0, 16)
    n1 = kb.sb([128, KC], name="n1")
    kb.dma("sp", n1[:], n1T[:], writes=[n1])
    a_sc = kb.sb([128, KC, 2], name="a_sc")
    b_sh = kb.sb([128, KC, 2], name="b_sh")
    for s in range(2):
        kb.op("dve", lambda e, s=s: e.scalar_tensor_tensor(
            out=a_sc[:, :, s], in0=mod[:, 8:16, s], scalar=1.0, in1=n1[:], op0=ALU.add, op1=ALU.mult),
            reads=[mod, n1], writes=[a_sc])
        kb.op("dve", lambda e, s=s: e.tensor_copy(out=b_sh[:, :, s], in_=mod[:, 0:8, s]), reads=[mod], writes=[b_sh])

    xts = [kb.sb([128, KC, 512], name="xt") for _ in range(2)]
    hTs = [kb.sb([128, KC, 512], BF16, name="hT") for _ in range(2)]
    sqs = [kb.sb([128, 512], name="sq") for _ in range(2)]
    tmps = [kb.sb([128, 512], name="tmp") for _ in range(2)]
    rstd = kb.sb([128, 512], name="rstd")
    ssp = kb.ps([128, 512], name="ssp")
    pps = [kb.ps([128, 512], name="pp") for _ in range(4)]
    stg = [kb.sb([128, 512], name="stg") for _ in range(4)]
    xv = xT[:].rearrange("(k p) n -> p k n", p=128)

    tiles = [(t0, 512, 0) for t0 in range(0, NLAT, 512)]
    if NCTX:
        tiles.append((NLAT, NCTX, 1))
    MCH = [(m * 128, 128) for m in range(INW // 128)] + [(INW // 128 * 128, INW % 128)]
    cnt = 0
    def load_x(ti):
        t0, ncol, s = tiles[ti]
        for k in range(KC):
            kb.dma("sp", xts[ti % 2][:, k, :ncol], xv[:, k, t0:t0 + ncol], writes=[xts[ti % 2]])
    load_x(0)
    for ti, (t0, ncol, s) in enumerate(tiles):
        xt = xts[ti % 2]
        hT = hTs[ti % 2]
        if ti + 1 < len(tiles):
            load_x(ti + 1)
        emit_norm_mod(kb, xt, hT, ones, eps_t, a_sc, b_sh, s, ncol, sqs, ssp, rstd, tmps)
        for (m0, mw) in MCH:
            pp = pps[cnt % 4]
            st = stg[cnt % 4]
            for k in range(KC):
                kb.mm(pp, pp[:mw, :ncol], w_bf, w_bf[:, k, m0:m0 + mw], hT, hT[:, k, :ncol],
                      start=(k == 0), stop=(k == KC - 1))
            if cnt % 2 == 0:
                kb.act(st, st[:mw, :ncol], pp, pp[:mw, :ncol], AF.Copy)
            else:
                kb.op("dve", lambda e, st=st, pp=pp, mw=mw: e.tensor_copy(out=st[:mw, :ncol], in_=pp[:mw, :ncol]),
                      reads=[pp], writes=[st])
            kb.dma("sp", pT[m0:m0 + mw, t0:t0 + ncol], st[:mw, :ncol], reads=[st], writes=[])
            cnt += 1
    return kb


HD = 128


def build_attn(TL=16384, TC=256):
    kb = KB()
    TT = TL + TC
    qT = kb.dram_in("qT", [HD, TT])
    kT = kb.dram_in("kT", [HD, TT])
    vT = kb.dram_in("vT", [HD, TT])
    cosT = kb.dram_in("cosT", [HD, TL])
    sinT = kb.dram_in("sinT", [HD, TL])
    RmT_d = kb.dram_in("RmT", [HD, HD])
    ident_d = kb.dram_in("ident", [HD, HD])
    qkw_d = kb.dram_in("qkw", [HD, 2])
    oT = kb.dram_out("oT", [HD, TT])

    ones = kb.sb([128, 128], name="ones")
    kb.op("dve", lambda e: e.memset(ones[:], 1.0), writes=[ones])
    ones_bf = kb.sb([128, 128], BF16, name="ones_bf")
    kb.op("dve", lambda e: e.memset(ones_bf[:], 1.0), writes=[ones_bf])
    eps_t = kb.sb([128, 1], name="eps")
    kb.op("dve", lambda e: e.memset(eps_t[:], 1e-6), writes=[eps_t])
    RmT = kb.sb([128, 128], name="RmT")
    ident = kb.sb([128, 128], name="ident")
    qkw = kb.sb([128, 2], name="qkw")
    kb.dma("sp", RmT[:], RmT_d[:], writes=[RmT])
    kb.dma("sp", ident[:], ident_d[:], writes=[ident])
    kb.dma("sp", qkw[:], qkw_d[:], writes=[qkw])

    Q_bf = kb.sb([128, TT], BF16, name="Q_bf")
    K_bf = kb.sb([128, TT], BF16, name="K_bf")
    NKT = TT // 128
    V_bf = kb.sb([128, NKT, 128], BF16, name="V_bf")

    P = [kb.ps([128, 512], name="P") for _ in range(8)]
    raw = [kb.sb([128, 512], name="raw") for _ in range(2)]
    cs = [kb.sb([128, 512], name="cs") for _ in range(2)]
    sn = [kb.sb([128, 512], name="sn") for _ in range(2)]
    sq = kb.sb([128, 512], name="sq")
    rstd = kb.sb([128, 512], name="rstd")
    xn = kb.sb([128, 512], name="xn")
    t1 = kb.sb([128, 512], name="t1")
    t2 = kb.sb([128, 512], name="t2")

    tiles = [(t0, 512, True) for t0 in range(0, TL, 512)]
    for t0 in range(TL, TT, 512):
        tiles.append((t0, min(512, TT - t0), False))
    it = 0
    for ti, (t0, nc_, lat) in enumerate(tiles):
        if lat:
            c_t = cs[ti % 2]; s_t = sn[ti % 2]
            kb.dma("sp", c_t[:, :nc_], cosT[:, t0:t0 + nc_], writes=[c_t])
            kb.dma("sp", s_t[:, :nc_], sinT[:, t0:t0 + nc_], writes=[s_t])
        for wi, (src, dst) in enumerate(((qT, Q_bf), (kT, K_bf))):
            r = raw[it % 2]; it += 1
            kb.dma("sp", r[:, :nc_], src[:, t0:t0 + nc_], writes=[r])
            kb.act(sq, sq[:, :nc_], r, r[:, :nc_], AF.Square)
            kb.mm(P[0], P[0][:, :nc_], ones, ones[:], sq, sq[:, :nc_])
            kb.act(rstd, rstd[:, :nc_], P[0], P[0][:, :nc_], AF.Sqrt, extra_reads=[eps_t], bias=eps_t[:, 0:1], scale=1.0 / HD)
            kb.op("dve", lambda e: e.reciprocal(out=rstd[:, :nc_], in_=rstd[:, :nc_]), reads=[rstd], writes=[rstd])
            if lat:
                kb.op("dve", lambda e, r=r, wi=wi: e.scalar_tensor_tensor(
                    out=xn[:, :nc_], in0=r[:, :nc_], scalar=qkw[:, wi:wi + 1], in1=rstd[:, :nc_], op0=ALU.mult, op1=ALU.mult),
                    reads=[r, qkw, rstd], writes=[xn])
                kb.mm(P[1], P[1][:, :nc_], RmT, RmT[:], xn, xn[:, :nc_])
                kb.op("dve", lambda e, c_t=c_t: e.tensor_tensor(out=t1[:, :nc_], in0=xn[:, :nc_], in1=c_t[:, :nc_], op=ALU.mult),
                      reads=[xn, c_t], writes=[t1])
                kb.op("dve", lambda e, s_t=s_t: e.tensor_tensor(out=t2[:, :nc_], in0=P[1][:, :nc_], in1=s_t[:, :nc_], op=ALU.mult),
                      reads=[P[1], s_t], writes=[t2])
                kb.op("dve", lambda e, dst=dst: e.tensor_tensor(out=dst[:, t0:t0 + nc_], in0=t1[:, :nc_], in1=t2[:, :nc_], op=ALU.add),
                      reads=[t1, t2], writes=[dst])
            else:
                kb.op("dve", lambda e, r=r, wi=wi, dst=dst: e.scalar_tensor_tensor(
                    out=dst[:, t0:t0 + nc_], in0=r[:, :nc_], scalar=qkw[:, wi:wi + 1], in1=rstd[:, :nc_], op0=ALU.mult, op1=ALU.mult),
                    reads=[r, qkw, rstd], writes=[dst])
        r = raw[it % 2]; it += 1
        kb.dma("sp", r[:, :nc_], vT[:, t0:t0 + nc_], writes=[r])
        nb = nc_ // 128
        for bi in range(nb):
            kb.tr(P[2], P[2][:, bi * 128:(bi + 1) * 128], r, r[:, bi * 128:(bi + 1) * 128], ident, ident[:])
        kt0 = t0 // 128
        kb.act(V_bf, V_bf[:, kt0:kt0 + nb, :], P[2], P[2][:, :nb * 128].rearrange("p (a b) -> p a b", b=128), AF.Copy)

    scale = float(HD) ** -0.5
    pts = [kb.sb([128, 512], BF16, name="pt") for _ in range(3)]
    rs = kb.sb([128, 512], name="rs")
    ost = [kb.sb([128, 512], name="ost") for _ in range(2)]
    acc = [(P[0], P[1]), (P[5], P[6])]
    sacc = [[kb.sb([128, 512], name="sacc") for _ in range(2)] for _ in range(2)]
    stot = kb.sb([128, 512], name="stot")
    sTs = [P[3], P[4], P[7]]
    qtiles = [(q0, 512, 0, NKT) for q0 in range(0, TL, 512)]
    for q0 in range(TL, TT, 512):
        qtiles.append((q0, min(512, TT - q0), TL // 128, NKT))
    work = []
    for qi, (q0, nq, kta, ktb) in enumerate(qtiles):
        for kt in range(kta, ktb):
            work.append((qi, q0, nq, kt, kt == kta, kt == ktb - 1))

    def issue_s(n):
        qi, q0, nq, kt, first, last = work[n]
        sT = sTs[n % 3]
        kb.mm(sT, sT[:, :nq], K_bf, K_bf[:, kt * 128:(kt + 1) * 128], Q_bf, Q_bf[:, q0:q0 + nq])

    issue_s(0)
    for n in range(len(work)):
        qi, q0, nq, kt, first, last = work[n]
        oP, sP = acc[qi % 2]
        sT = sTs[n % 3]; pt = pts[n % 3]
        if n + 1 < len(work):
            issue_s(n + 1)
        kb.act(pt, pt[:, :nq], sT, sT[:, :nq], AF.Exp, scale=scale)
        kb.mm(oP, oP[:, :nq], V_bf, V_bf[:, kt, :], pt, pt[:, :nq], start=first, stop=last)
        kk = (kt - work[n][3] + (kt % 2)) % 2 if False else (kt % 2)
        sa = sacc[qi % 2][kk]
        eng_ = "dve" if kk == 0 else "pool"
        kfirst = (kt - (TL // 128 if q0 >= TL else 0)) < 2
        if kfirst:
            kb.cp(sa, sa[:, :nq], pt, pt[:, :nq], eng=eng_)
        else:
            kb.tt(sa, sa[:, :nq], sa, sa[:, :nq], pt, pt[:, :nq], ALU.add, eng=eng_)
        if last:
            st = ost[qi % 2]
            kb.tt(stot, stot[:, :nq], sacc[qi % 2][0], sacc[qi % 2][0][:, :nq], sacc[qi % 2][1], sacc[qi % 2][1][:, :nq], ALU.add)
            kb.mm(sP, sP[:, :nq], ones, ones[:], stot, stot[:, :nq])
            kb.op("dve", lambda e, sP=sP, nq=nq: e.reciprocal(out=rs[:, :nq], in_=sP[:, :nq]), reads=[sP], writes=[rs])
            kb.op("dve", lambda e, oP=oP, st=st, nq=nq: e.tensor_tensor(out=st[:, :nq], in0=oP[:, :nq], in1=rs[:, :nq], op=ALU.mult),
                  reads=[oP, rs], writes=[st])
            kb.dma("sp", oT[:, q0:q0 + nq], st[:, :nq], reads=[st], writes=[])
    return kb


def rope_tables(T=16384, GRID_W=64):
    rows = T // GRID_W
    row = np.repeat(np.arange(rows, dtype=np.float32), GRID_W)
    col = np.tile(np.arange(GRID_W, dtype=np.float32), rows)
    axis_dim = HD // 2
    inv_freq = (10000.0 ** (-np.arange(0, axis_dim, 2, dtype=np.float32) / axis_dim)).astype(np.float32)
    ang_r = row[:, None] * inv_freq[None, :]
    ang_c = col[:, None] * inv_freq[None, :]
    ang = np.concatenate([ang_r, ang_r, ang_c, ang_c], axis=-1)
    return np.cos(ang).astype(np.float32), np.sin(ang).astype(np.float32)


def rot_matrix_T():
    R = np.zeros((128, 128), np.float32)
    for dp in range(128):
        if (dp % 64) < 32:
            R[dp, dp + 32] = -1.0
        else:
            R[dp, dp - 32] = 1.0
    return np.ascontiguousarray(R.T)


def dn_consts():
    p = np.arange(128)[:, None]
    q = np.arange(128)[None, :]
    same = (p // 64) == (q // 64)
    cm = np.zeros((6, 128, 128), np.float32)
    cm[0] = np.eye(128)
    cm[1] = (same & (q <= p))
    cm[2] = (same & (q >= p))
    cm[3] = same
    cm[4] = (p < 64) * np.ones((1, 128))
    cm[5] = (p >= 64) * np.ones((1, 128))
    return cm


def build_dn(TL=16384, TC=256, stage=9):
    kb = KB()
    TT = TL + TC
    NB = TT // 128
    NBL = TL // 128
    PADW = TL + 4 + TC + 4
    qkvP = kb.dram_in("qkvP", [128, 3, PADW])
    gateT = kb.dram_in("gateT", [128, TT])
    baT = kb.dram_in("baT", [4, TT])
    cwT = kb.dram_in("cwT", [128, 3, 5])
    cst_d = kb.dram_in("cst", [128, 8])
    cm_d = kb.dram_in("cm", [6, 128, 128])
    yT = kb.dram_out("yT", [128, TT])
    oS = [kb.dram_tmp("oF", [128, TT]), kb.dram_tmp("oB", [128, TT])]
    oSv = [[oS[d].view((slice(None), slice(n * 128, (n + 1) * 128))) for n in range(NB)] for d in range(2)]

    cmt = kb.sb([128, 6, 128], name="cm")
    kb.dma("sp", cmt[:], cm_d[:].rearrange("c p q -> p c q"), writes=[cmt])
    ident_ap = cmt[:, 0, :]
    LO = cmt[:, 1, :]; UP = cmt[:, 2, :]; BLK = cmt[:, 3, :]
    HALF = [cmt[:, 4, :], cmt[:, 5, :]]
    ones = kb.sb([128, 128], name="ones")
    kb.op("dve", lambda e: e.memset(ones[:], 1.0), writes=[ones])
    eps_t = kb.sb([128, 1], name="eps")
    kb.op("dve", lambda e: e.memset(eps_t[:], 1e-6), writes=[eps_t])
    one_c = kb.sb([128, 1], name="onec")
    kb.op("dve", lambda e: e.memset(one_c[:], 1.0), writes=[one_c])
    msk = kb.sb([128, 3, 128], name="msk")
    kb.ts(msk, msk[:, 0, :], cmt, LO, -1e4, ALU.mult, 1e4, ALU.add)
    kb.ts(msk, msk[:, 1, :], cmt, UP, -1e4, ALU.mult, 1e4, ALU.add)
    kb.ts(msk, msk[:, 2, :], cmt, ident_ap, -1.0, ALU.mult, 1.0, ALU.add)
    MLO = msk[:, 0, :]; MUP = msk[:, 1, :]; NOTI = msk[:, 2, :]
    cw = kb.sb([128, 3, 5], name="cw")
    kb.dma("sp", cw[:], cwT[:], writes=[cw])
    cst = kb.sb([128, 8], name="cst")
    kb.dma("sp", cst[:], cst_d[:], writes=[cst])
    nega = kb.sb([128, 2], name="nega")
    kb.act(nega, nega[:], cst, cst[:, 0:2], AF.Exp)
    kb.ts(nega, nega[:], nega, nega[:], -1.0, ALU.mult)

    banks = [kb.ps([128, 512], name="bank") for _ in range(8)]

    def v(b, c0, w):
        return banks[b].sub((slice(None), slice(c0, c0 + w)))

    TS = kb.sb([128, NB, 4], name="TS")
    bat = [kb.sb([4, 2048], name="bat") for _ in range(2)]
    pts = [v(0, 0, 512), v(1, 0, 512)]
    for pi, c0 in enumerate(range(0, TT, 2048)):
        w = min(2048, TT - c0)
        bt = bat[pi % 2]
        kb.dma("sp", bt[:, :w], baT[:, c0:c0 + w], writes=[bt])
        pt = pts[pi % 2]
        nb = w // 128
        for i in range(nb):
            kb.tr(pt, pt[:, i * 4:(i + 1) * 4], bt, bt[:, i * 128:(i + 1) * 128], cmt, cmt[0:4, 0, 0:4])
        n0 = c0 // 128
        kb.cp(TS, TS[:, n0:n0 + nb, :], pt, pt[:, :nb * 4].rearrange("p (a b) -> p a b", b=4), eng="dve")
    if stage == 0:
        o = kb.dram_out("dTS", [128, NB * 4])
        kb.dma("sp", o[:], TS[:].rearrange("p a b -> p (a b)"), reads=[TS], writes=[])
        o = kb.dram_out("dmsk", [128, 3 * 128])
        kb.dma("sp", o[:], msk[:].rearrange("p a b -> p (a b)"), reads=[msk], writes=[])
        o = kb.dram_out("dnega", [128, 2])
        kb.dma("sp", o[:], nega[:], reads=[nega], writes=[])
        return kb
    BETA = kb.sb([128, NB, 2], name="BETA")
    kb.act(BETA, BETA[:], TS, TS[:, :, 0:2], AF.Sigmoid)
    G = kb.sb([128, NB, 2], name="G")
    for d in range(2):
        kb.act(G, G[:, :, d], TS, TS[:, :, 2 + d], AF.Exp, extra_reads=[cst], bias=cst[:, 2 + d:3 + d], scale=1.0)
        kb.act(G, G[:, :, d], G, G[:, :, d], AF.Ln, extra_reads=[one_c], bias=one_c[:, 0:1], scale=1.0)
        kb.ts(G, G[:, :, d], G, G[:, :, d], nega[:, d:d + 1], ALU.mult, sr=[nega])
    GC = kb.sb([128, NB, 2], name="GC")
    GT = kb.sb([128, NB, 2], name="GT")
    EGL = kb.sb([128, NB, 2, 2], name="EGL")
    pa = v(2, 0, 512)
    for d in range(2):
        kb.mm(pa, pa[:, 0:NB], cmt, (UP if d == 0 else LO), G, G[:, :, d])
        kb.cp(GC, GC[:, :, d], pa, pa[:, 0:NB], eng="dve")
        kb.mm(pa, pa[:, 0:NB], cmt, BLK, G, G[:, :, d])
        kb.cp(GT, GT[:, :, d], pa, pa[:, 0:NB], eng="dve")
        for h in range(2):
            kb.mm(pa, pa[:, 0:NB], cmt, HALF[h], G, G[:, :, d])
            kb.act(EGL, EGL[:, :, d, h], pa, pa[:, 0:NB], AF.Exp)
    EGC = kb.sb([128, NB, 2], name="EGC")
    kb.act(EGC, EGC[:], GC, GC[:], AF.Exp)
    EKL = kb.sb([128, NB, 2], name="EKL")
    kb.tt(EKL, EKL[:], GT, GT[:], GC, GC[:], ALU.subtract)
    kb.act(EKL, EKL[:], EKL, EKL[:], AF.Exp)
    BEG = kb.sb([128, NB, 2], name="BEG")
    kb.tt(BEG, BEG[:], BETA, BETA[:], EGC, EGC[:], ALU.mult)
    NBETA = kb.sb([128, NB, 2], name="NBETA")
    kb.ts(NBETA, NBETA[:], BETA, BETA[:], -1.0, ALU.mult)
    EKLH = kb.sb([128, NB, 2, 2], name="EKLH")
    kb.op("dve", lambda e: e.memset(EKLH[:], 0.0), writes=[EKLH])
    for h in range(2):
        kb.cp(EKLH, EKLH[h * 64:(h + 1) * 64, :, :, h], EKL, EKL[h * 64:(h + 1) * 64, :, :], eng="dve")

    if stage == 1:
        for nm, t, sh in (("dGC", GC, [128, NB * 2]), ("dBETA", BETA, [128, NB * 2]), ("dEGL", EGL, [128, NB * 4]),
                          ("dEKLH", EKLH, [128, NB * 4]), ("dG", G, [128, NB * 2])):
            o = kb.dram_out(nm, sh)
            kb.dma("sp", o[:], t[:].rearrange("p a b -> p (a b)") if len(sh) == 2 and nm in ("dGC", "dBETA", "dG") else t[:].rearrange("p a b c -> p (a b c)"), reads=[t], writes=[])
        return kb
    kb.barrier()
    def mk(name, shape=(128, 128)):
        return [kb.sb(list(shape), name=name + str(d)) for d in range(2)]

    XIN = mk("xin", (128, 3, 132)); ACC = mk("acc", (128, 3, 128)); Y = mk("y", (128, 3, 128))
    SQ = mk("sq", (128, 2, 128)); RN = mk("rn", (128, 2, 128)); QN = mk("qn"); KN = mk("kn")
    VB = mk("vb"); KBG = mk("kbg"); KEL = mk("kel", (128, 2, 128)); DG = mk("dg"); T0 = mk("t0")
    X1 = mk("x1"); X2 = mk("x2"); DM = mk("dm"); DMT = mk("dmt"); EBC = mk("ebc"); QG = mk("qg"); QKM = mk("qkm")
    AD = mk("ad"); MTa = [mk("mta"), mk("mtb")]; Ma = [mk("ma"), mk("mb")]
    Ra = [mk("ra"), mk("rb")]
    VN = mk("vn"); OSB = mk("osb"); OTS = mk("ots")
    U2 = [mk("u_a"), mk("u_b")]; WT2 = [mk("wt_a"), mk("wt_b")]; QG2 = [mk("qg_a"), mk("qg_b")]
    QKM2 = [mk("qkm_a"), mk("qkm_b")]; KEL2 = [mk("kel_a", (128, 2, 128)), mk("kel_b", (128, 2, 128))]
    S = [mk("s0"), mk("s1")]
    for d in range(2):
        kb.op("dve", lambda e, d=d: e.memset(VN[d][:], 0.0), writes=[VN[d]])
        kb.op("dve", lambda e, d=d: e.memset(S[0][d][:], 0.0), writes=[S[0][d]])
    spar = [0, 0]
    P_ss = [v(4 * d + 0, 0, 256) for d in range(2)]
    P_g = [v(4 * d + 0, 256, 128) for d in range(2)]
    P_uw = [v(4 * d + 0, 384, 128) for d in range(2)]
    P_tr = [v(4 * d + 1, 0, 256) for d in range(2)]
    P_kk = [v(4 * d + 1, 256, 256) for d in range(2)]
    P_mm = [v(4 * d + 2, 0, 256) for d in range(2)]
    P_m0 = [v(4 * d + 2, 256, 128) for d in range(2)]
    P_r = [v(4 * d + 2, 384, 128) for d in range(2)]
    P_p1 = [v(4 * d + 3, 0, 128) for d in range(2)]
    P_o = [v(4 * d + 3, 128, 128) for d in range(2)]
    P_s = [v(4 * d + 3, 256, 128) for d in range(2)]
    P_ot = [v(4 * d + 3, 384, 128) for d in range(2)]

    def prep(n, d, pp=0):
        U = U2[pp]; WT = WT2[pp]; QG = QG2[pp]; QKM = QKM2[pp]; KEL = KEL2[pp]
        c0 = n * 128 if n < NBL else TL + 4 + (n - NBL) * 128
        xin = XIN[d]; acc = ACC[d]; y = Y[d]
        kb.dma("sp", xin[:], qkvP[:, :, c0:c0 + 132], writes=[xin])
        for w in range(3):
            kb.ts(acc, acc[:, w, :], xin, xin[:, w, 0:128], cw[:, w, 0:1], ALU.mult, sr=[cw])
            for jj in range(1, 5):
                kb.stt(acc, acc[:, w, :], xin, xin[:, w, jj:jj + 128], cw[:, w, jj:jj + 1], acc, acc[:, w, :],
                       ALU.mult, ALU.add, sr=[cw])
        kb.act(y, y[:], acc, acc[:], AF.Silu)
        if stage == 21: return
        sq = SQ[d]; rn = RN[d]; pss = P_ss[d]
        kb.act(sq, sq[:], y, y[:, 0:2, :], AF.Square)
        kb.mm(pss, pss[:], ones, ones[:], sq, sq[:].rearrange("p a b -> p (a b)"))
        kb.act(rn, rn[:].rearrange("p a b -> p (a b)"), pss, pss[:], AF.Sqrt, extra_reads=[eps_t], bias=eps_t[:, 0:1], scale=1.0)
        kb.op("dve", lambda e: e.reciprocal(out=rn[:], in_=rn[:]), reads=[rn], writes=[rn])
        qn = QN[d]; kn = KN[d]
        kb.stt(qn, qn[:], y, y[:, 0, :], float(HD) ** -0.5, rn, rn[:, 0, :], ALU.mult, ALU.mult)
        kb.tt(kn, kn[:], y, y[:, 1, :], rn, rn[:, 1, :], ALU.mult)
        if stage == 22: return
        ptr = P_tr[d]
        kb.tr(ptr, ptr[:, 0:128], kn, kn[:], cmt, ident_ap)
        kb.tr(ptr, ptr[:, 128:256], y, y[:, 2, :], cmt, ident_ap)
        kb.ts(KBG[d], KBG[d][:], ptr, ptr[:, 0:128], BEG[:, n, d:d + 1], ALU.mult, sr=[BEG])
        for h in range(2):
            kb.ts(KEL[d], KEL[d][:, h, :], ptr, ptr[:, 0:128], EKLH[:, n, d, h:h + 1], ALU.mult, sr=[EKLH])
        kb.ts(VB[d], VB[d][:], ptr, ptr[:, 128:256], BETA[:, n, d:d + 1], ALU.mult, sr=[BETA])
        if stage == 23: return
        pkk = P_kk[d]
        kb.mm(pkk, pkk[:, 0:128], kn, kn[:], kn, kn[:])
        kb.mm(pkk, pkk[:, 128:256], kn, kn[:], qn, qn[:])
        kb.ts(DG[d], DG[d][:], cmt, ident_ap, GC[:, n, d:d + 1], ALU.mult, sr=[GC])
        pg = P_g[d]
        kb.mm(pg, pg[:], ones, ones[:], DG[d], DG[d][:])
        kb.ts(T0[d], T0[d][:], pg, pg[:], GC[:, n, d:d + 1], ALU.subtract, sr=[GC])
        kb.act(EBC[d], EBC[d][:], pg, pg[:], AF.Exp)
        if stage == 24: return
        M1 = MLO if d == 0 else MUP
        M2 = MUP if d == 0 else MLO
        kb.tt(X1[d], X1[d][:], T0[d], T0[d][:], msk, M1, ALU.add)
        kb.tt(X2[d], X2[d][:], T0[d], T0[d][:], msk, M2, ALU.subtract)
        kb.act(DM[d], DM[d][:], X1[d], X1[d][:], AF.Exp, scale=-1.0)
        kb.act(DMT[d], DMT[d][:], X2[d], X2[d][:], AF.Exp)
        kb.tt(QG[d], QG[d][:], qn, qn[:], EBC[d], EBC[d][:], ALU.mult)
        kb.tt(QKM[d], QKM[d][:], pkk, pkk[:, 128:256], DMT[d], DMT[d][:], ALU.mult)
        kb.tt(AD[d], AD[d][:], pkk, pkk[:, 0:128], DM[d], DM[d][:], ALU.mult)
        if stage == 25: return
        mt = MTa[0][d]
        kb.stt(mt, mt[:], AD[d], AD[d][:], NBETA[:, n, d:d + 1], msk, NOTI, ALU.mult, ALU.mult, sr=[NBETA])
        if stage == 261: return
        pm0 = P_m0[d]
        kb.tr(pm0, pm0[:], mt, mt[:], cmt, ident_ap)
        if stage == 262: return
        m = Ma[0][d]
        kb.cp(m, m[:], pm0, pm0[:])
        if stage == 263: return
        r = Ra[0][d]
        kb.tt(r, r[:], m, m[:], cmt, ident_ap, ALU.add)
        if stage == 26: return
        pmm = P_mm[d]; pr = P_r[d]
        for k in range(1, 6):
            mtp = MTa[(k - 1) % 2][d]; mp = Ma[(k - 1) % 2][d]
            mtn = MTa[k % 2][d]; mn = Ma[k % 2][d]
            kb.mm(pmm, pmm[:, 0:128], mp, mp[:], mtp, mtp[:])
            kb.cp(mtn, mtn[:], pmm, pmm[:, 0:128])
            if k < 5:
                kb.mm(pmm, pmm[:, 128:256], mtp, mtp[:], mp, mp[:])
                kb.cp(mn, mn[:], pmm, pmm[:, 128:256], eng="dve")
            rp = Ra[(k - 1) % 2][d]; rn_ = Ra[k % 2][d]
            kb.mm(pr, pr[:], mtn, mtn[:], rp, rp[:])
            kb.tt(rn_, rn_[:], pr, pr[:], rp, rp[:], ALU.add)
        if stage == 27: return
        R = Ra[5 % 2][d]
        puw = P_uw[d]
        kb.mm(puw, puw[:], R, R[:], VB[d], VB[d][:])
        kb.cp(U[d], U[d][:], puw, puw[:])
        kb.mm(puw, puw[:], KBG[d], KBG[d][:], R, R[:])
        kb.cp(WT[d], WT[d][:], puw, puw[:])

    def scan(n, d, h, pp=0):
        U = U2[pp]; WT = WT2[pp]; QG = QG2[pp]; QKM = QKM2[pp]; KEL = KEL2[pp]
        rows = slice(h * 64, (h + 1) * 64)
        Sc = S[spar[d]][d]; Sn = S[1 - spar[d]][d]
        p1 = P_p1[d]; po = P_o[d]; ps_ = P_s[d]
        kb.mm(p1, p1[:], WT[d], WT[d][:], Sc, Sc[:])
        if stage == 291: return
        kb.tt(VN[d], VN[d][rows, :], U[d], U[d][rows, :], p1, p1[rows, :], ALU.subtract)
        if stage == 292: return
        kb.mm(po, po[:], QG[d], QG[d][:], Sc, Sc[:], start=True, stop=False)
        kb.mm(po, po[:], QKM[d], QKM[d][:], VN[d], VN[d][:], start=False, stop=True)
        if stage == 293: return
        kb.cp(OSB[d], OSB[d][rows, :], po, po[rows, :])
        if stage == 294: return
        kb.mm(ps_, ps_[:], KEL[d], KEL[d][:, h, :], VN[d], VN[d][:])
        if stage == 295: return
        kb.stt(Sn, Sn[:], Sc, Sc[:], EGL[:, n, d, h:h + 1], ps_, ps_[:], ALU.mult, ALU.add, sr=[EGL])
        spar[d] = 1 - spar[d]

    def fin(n, d):
        pot = P_ot[d]
        kb.tr(pot, pot[:], OSB[d], OSB[d][:], cmt, ident_ap)
        kb.cp(OTS[d], OTS[d][:], pot, pot[:], eng="dve")
        kb.dma("sp", oS[d][:, n * 128:(n + 1) * 128], OTS[d][:], reads=[OTS[d]], writes=[oSv[d][n]])

    NBC = NB - NBL
    ordF = list(range(NBL, NB)) + list(range(NBL))
    ordB = list(range(NB - 1, NBL - 1, -1)) + list(range(NBL - 1, -1, -1))
    if 20 < stage < 30 or 260 < stage < 300:
        prep(ordF[0], 0)
        if stage == 29 or stage > 290:
            scan(ordF[0], 0, 0)
    if stage == 9:
        kb.rec = []
        prep(ordF[0], 0, 0)
        PF0 = kb.rec
        kb.rec = []
        prep(ordB[0], 1, 0)
        PB0 = kb.rec
        kb.replay(PF0, PB0)
    for t in range(NB if stage != 2 else 1):
        if 20 < stage < 30 or 260 < stage < 300: break
        nf, nb_ = ordF[t], ordB[t]
        pp = t % 2
        kb.rec = []
        scan(nf, 0, 0, pp); scan(nf, 0, 1, pp); fin(nf, 0)
        SF = kb.rec
        kb.rec = []
        scan(nb_, 1, 1, pp); scan(nb_, 1, 0, pp); fin(nb_, 1)
        SB = kb.rec
        PF = []; PB = []
        if t + 1 < NB:
            kb.rec = []
            prep(ordF[t + 1], 0, 1 - pp)
            PF = kb.rec
            kb.rec = []
            prep(ordB[t + 1], 1, 1 - pp)
            PB = kb.rec
        kb.replay(SF, SB, PF, PB)
    if stage in (2, 3) or 20 < stage < 30 or 260 < stage < 300:
        for nm, t in (("dY", Y[0]), ("dQN", QN[0]), ("dKN", KN[0]), ("dU", U2[0][0]), ("dWT", WT2[0][0]), ("dOSB", OSB[0]),
                      ("dR", Ra[1][0]), ("dMT0", MTa[0][0]), ("dOSB1", OSB[1]), ("dS0", S[spar[0]][0])):
            sh = [128, 384] if nm == "dY" else [128, 128]
            o = kb.dram_out(nm, sh)
            kb.dma("sp", o[:], t[:].rearrange("p a b -> p (a b)") if nm == "dY" else t[:], reads=[t], writes=[])
        return kb
    kb.barrier()
    oa = [kb.sb([128, 512], name="oa") for _ in range(2)]
    ob = [kb.sb([128, 512], name="ob") for _ in range(2)]
    gt = [kb.sb([128, 512], name="gt") for _ in range(2)]
    osum = kb.sb([128, 512], name="osum"); sq2 = kb.sb([128, 512], name="sq2"); rs = kb.sb([128, 512], name="rs")
    yo = [kb.sb([128, 512], name="yo") for _ in range(2)]
    pf = v(0, 0, 512)
    for ti, c0 in enumerate(range(0, TT, 512)):
        w = min(512, TT - c0)
        a = oa[ti % 2]; b = ob[ti % 2]; g = gt[ti % 2]; yy = yo[ti % 2]
        blks = range(c0 // 128, (c0 + w) // 128)
        kb.dma("sp", a[:, :w], oS[0][:, c0:c0 + w], reads=[oSv[0][n] for n in blks], writes=[a])
        kb.dma("sp", b[:, :w], oS[1][:, c0:c0 + w], reads=[oSv[1][n] for n in blks], writes=[b])
        kb.dma("sp", g[:, :w], gateT[:, c0:c0 + w], writes=[g])
        kb.tt(osum, osum[:, :w], a, a[:, :w], b, b[:, :w], ALU.add)
        kb.act(sq2, sq2[:, :w], osum, osum[:, :w], AF.Square)
        kb.mm(pf, pf[:, :w], ones, ones[:], sq2, sq2[:, :w])
        kb.act(rs, rs[:, :w], pf, pf[:, :w], AF.Sqrt, extra_reads=[eps_t], bias=eps_t[:, 0:1], scale=1.0 / HD)
        kb.op("dve", lambda e: e.reciprocal(out=rs[:, :w], in_=rs[:, :w]), reads=[rs], writes=[rs])
        kb.act(g, g[:, :w], g, g[:, :w], AF.Silu)
        kb.stt(osum, osum[:, :w], osum, osum[:, :w], cst[:, 4:5], rs, rs[:, :w], ALU.mult, ALU.mult, sr=[cst])
        kb.tt(yy, yy[:, :w], osum, osum[:, :w], g, g[:, :w], ALU.mult)
        kb.dma("sp", yT[:, c0:c0 + w], yy[:, :w], reads=[yy], writes=[])
    return kb


NEXP = 16384
NCH = NEXP // 128


def peer_consts():
    bm = np.zeros((128, 8, 16), np.float32)
    for p in range(128):
        bm[p, p // 16, :] = 1.0
    return bm.reshape(128, 128)


def build_C(NLAT=4096, NCTX=64, TW=256, dbg=False):
    kb = KB()
    NT = NLAT + NCTX
    xT = kb.dram_in("xT", [D, NT])
    mixT = kb.dram_in("mixT", [D, NT])
    cT = kb.dram_in("cT", [128, KC, 2])
    ada_w = kb.dram_in("ada_w", [D, 4 * D])
    ada_bT = kb.dram_in("ada_bT", [128, 48])
    n2T = kb.dram_in("n2T", [128, KC])
    w_out = kb.dram_in("w_out", [D, D])
    wq = kb.dram_in("wq", [D, 2048])
    subkT = kb.dram_in("subkT", [16, 128, 128])
    uTr = kb.dram_in("uTr", [NCH, 128, KC, 128])
    vtab = kb.dram_in("vtab", [NEXP, D])
    bm_d = kb.dram_in("bm", [128, 128])
    id_d = kb.dram_in("ident", [128, 128])
    xoT = kb.dram_out("xoT", [D, NT])
    if dbg:
        d_h2 = kb.dram_out("d_h2", [D, NT])
        d_peer = kb.dram_out("d_peer", [D, NT])
        d_x1 = kb.dram_out("d_x1", [D, NT])

    ones = kb.sb([128, 128], name="ones")
    kb.op("dve", lambda e: e.memset(ones[:], 1.0), writes=[ones])
    eps_t = kb.sb([128, 1], name="eps")
    kb.op("dve", lambda e: e.memset(eps_t[:], 1e-6), writes=[eps_t])
    iot = kb.sb([128, 128], name="iota")
    kb.op("pool", lambda e: e.iota(iot[:], pattern=[[1, 128]], base=0, channel_multiplier=0,
                                   allow_small_or_imprecise_dtypes=True), writes=[iot])
    iot_bf = kb.sb([128, 128], BF16, name="iota_bf")
    kb.cp(iot_bf, iot_bf[:], iot, iot[:], eng="pool")
    bm = kb.sb([128, 128], name="bm")
    kb.dma("sp", bm[:], bm_d[:], writes=[bm])
    ident = kb.sb([128, 128], name="ident")
    kb.dma("sp", ident[:], id_d[:], writes=[ident])
    ident_bf = kb.sb([128, 128], BF16, name="ident_bf")
    kb.cp(ident_bf, ident_bf[:], ident, ident[:], eng="dve")
    subk = kb.sb([128, 16, 128], name="subk")
    kb.dma("sp", subk[:], subkT[:].rearrange("c p q -> p c q"), writes=[subk])
    wo_bf = kb.sb([128, KC, D], BF16, name="wo_bf")
    wov = w_out[:].rearrange("(k p) n -> p k n", p=128)
    for k in range(KC):
        kb.dma("pool", wo_bf[:, k, :], wov[:, k, :], writes=[wo_bf])

    P = [kb.ps([128, 512], name="P") for _ in range(7)]
    P7b = kb.ps([128, 1024], BF16, name="P7b")
    WQW = 256
    wqs = [kb.sb([128, KC, WQW], name="wqs") for _ in range(2)]
    uB = kb.dram_tmp("uB", [NCH, 128, KC, 128], BF16)
    vB = kb.dram_tmp("vB", [NEXP, D], BF16)
    uBv = [uB.view((c,)) for c in range(NCH)]
    vBv = [vB.view((slice(c * 128, (c + 1) * 128),)) for c in range(NCH)]
    mod = emit_mod(kb, cT, ada_w, ada_bT, 16, 48, wt=wqs, bw=WQW, modp=P[0], col_base=2 * D)
    for c in range(NCH):
        kb.dma("pool", uB[c], uTr[c], writes=[uBv[c]])
        kb.dma("pool", vB[c * 128:(c + 1) * 128, :], vtab[c * 128:(c + 1) * 128, :], writes=[vBv[c]])
    n2 = kb.sb([128, KC], name="n2")
    kb.dma("sp", n2[:], n2T[:], writes=[n2])
    a_sc = kb.sb([128, KC, 2], name="a_sc")
    for s in range(2):
        kb.stt(a_sc, a_sc[:, :, s], mod, mod[:, 16:24, s], 1.0, n2, n2[:], ALU.add, ALU.mult)

    wqv = wq[:].rearrange("(k p) n -> p k n", p=128)
    xt = kb.sb([128, KC, TW], name="xt")
    mx_bf = kb.sb([128, KC, TW], BF16, name="mx_bf")
    h2 = kb.sb([128, KC, TW], name="h2")
    h2_bf = kb.sb([128, KC, TW], BF16, name="h2_bf")
    sqs = [kb.sb([128, TW], name="sq") for _ in range(2)]
    tmps = [kb.sb([128, TW], name="tmp") for _ in range(2)]
    rstd = kb.sb([128, TW], name="rstd")
    qps = [kb.sb([128, 2, 128], name="qps") for _ in range(2)]
    s_sb = kb.sb([128, 16, 128], name="s_sb")
    cand = kb.sb([128, 16, 8, 16], name="cand")
    mtmp = kb.sb([128, 128], name="mtmp")
    ctmp = kb.sb([128, 16, 16], name="ctmp")
    sv = kb.sb([128, 8, 2, 16], name="sv")
    si = kb.sb([128, 8, 2, 16], U32, name="si")
    sif = kb.sb([128, 2, 8, 16], name="sif")
    c16 = kb.sb([128, 8, 16], name="c16")
    nmx = kb.sb([128, 8], name="nmx")
    zz = kb.sb([128, 8], name="zz")
    w_bf = kb.sb([128, 16, 8, 16], BF16, name="w_bf")
    WT = kb.sb([128, 16, TW], BF16, name="WT")
    SIT = kb.sb([128, 2, TW], name="SIT")
    GT = kb.sb([128, TW, NCH], BF16, name="GT")
    oh1q = [kb.sb([128, 4, 128], BF16, name="oh1q") for _ in range(3)]
    oh2q = [kb.sb([128, 4, 128], BF16, name="oh2q") for _ in range(3)]
    wbq = [kb.sb([128, 4, 128], BF16, name="wbq") for _ in range(3)]
    XS = [kb.sb([128, 4, 128], BF16, name="XS") for _ in range(3)]
    NUB = 2 if dbg else 3
    dbgp = kb.sb([128, TW], name="dbgp") if dbg else None
    u_bf = [kb.sb([128, KC, 128], BF16, name="u_bf") for _ in range(NUB)]
    v_bf = [kb.sb([128, D], BF16, name="v_bf") for _ in range(NUB)]
    gl = [kb.sb([128, TW], name="gl") for _ in range(2)]
    ga = [kb.sb([128, TW], BF16, name="ga") for _ in range(2)]
    ost = [kb.sb([128, TW], name="ost") for _ in range(2)]
    xv = xT[:].rearrange("(k p) n -> p k n", p=128)
    mv = mixT[:].rearrange("(k p) n -> p k n", p=128)

    tiles = [(t0, TW, 0) for t0 in range(0, NLAT, TW)]
    if NCTX:
        tiles.append((NLAT, NCTX, 1))
    nld = 0
    for ti, (t0, ncol, s) in enumerate(tiles):
        for k in range(KC):
            kb.dma("sp", xt[:, k, :ncol], xv[:, k, t0:t0 + ncol], writes=[xt])
            kb.dma("pool", mx_bf[:, k, :ncol], mv[:, k, t0:t0 + ncol], writes=[mx_bf])
        for m in range(KC):
            pp = P[4 + m % 2]
            for k in range(KC):
                kb.mm(pp, pp[:, :ncol], wo_bf, wo_bf[:, k, m * 128:(m + 1) * 128], mx_bf, mx_bf[:, k, :ncol],
                      start=(k == 0), stop=(k == KC - 1))
            kb.stt(xt, xt[:, m, :ncol], pp, pp[:, :ncol], mod[:, m, s:s + 1], xt, xt[:, m, :ncol], ALU.mult, ALU.add, sr=[mod])
        if dbg:
            for k in range(KC):
                kb.dma("sp", d_x1[k * 128:(k + 1) * 128, t0:t0 + ncol], xt[:, k, :ncol], reads=[xt], writes=[])
        ssp = P[6]
        for k in range(KC):
            sq = sqs[k % 2]
            kb.act(sq, sq[:, :ncol], xt, xt[:, k, :ncol], AF.Square)
            kb.mm(ssp, ssp[:, :ncol], ones, ones[:], sq, sq[:, :ncol], start=(k == 0), stop=(k == KC - 1))
        kb.act(rstd, rstd[:, :ncol], ssp, ssp[:, :ncol], AF.Sqrt, extra_reads=[eps_t], bias=eps_t[:, 0:1], scale=1.0 / D)
        kb.op("dve", lambda e: e.reciprocal(out=rstd[:, :ncol], in_=rstd[:, :ncol]), reads=[rstd], writes=[rstd])
        for k in range(KC):
            tmp = tmps[k % 2]
            kb.stt(tmp, tmp[:, :ncol], xt, xt[:, k, :ncol], a_sc[:, k, s:s + 1], rstd, rstd[:, :ncol], ALU.mult, ALU.mult, sr=[a_sc])
            kb.act(h2, h2[:, k, :ncol], tmp, tmp[:, :ncol], AF.Identity, extra_reads=[mod], bias=mod[:, 8 + k, s:s + 1], scale=1.0)
        kb.cp(h2_bf, h2_bf[:, :, :ncol], h2, h2[:, :, :ncol], eng="pool")
        if dbg:
            for k in range(KC):
                kb.dma("sp", d_h2[k * 128:(k + 1) * 128, t0:t0 + ncol], h2[:, k, :ncol], reads=[h2], writes=[])
        for sb0 in range(0, ncol, 128):
            nt = min(128, ncol - sb0)
            for hb in range(0, 16, 2):
                wt_ = wqs[nld % 2]; nld += 1
                kb.dma("sp", wt_[:], wqv[:, :, hb * 128:hb * 128 + WQW], writes=[wt_])
                qpp = P[4 + (hb // 2) % 2]
                for j in range(2):
                    for k in range(KC):
                        kb.mm(qpp, qpp[:, j * 128:j * 128 + nt], wt_, wt_[:, k, j * 128:(j + 1) * 128],
                              h2, h2[:, k, sb0:sb0 + nt], start=(k == 0), stop=(k == KC - 1))
                qp = qps[(hb // 2) % 2]
                kb.cp(qp, qp[:, :, :nt], qpp, qpp[:, 0:256].rearrange("p (a b) -> p a b", b=128)[:, :, :nt])
                for j in range(2):
                    hp = hb + j
                    sp_ = P[hp // 4]
                    kb.mm(sp_, sp_[:nt, (hp % 4) * 128:(hp % 4 + 1) * 128], qp, qp[:, j, :nt], subk, subk[:, hp, :])
            for bnk in range(4):
                kb.cp(s_sb, s_sb[:nt, bnk * 4:(bnk + 1) * 4, :], P[bnk], P[bnk][:nt, :].rearrange("p (a b) -> p a b", b=128),
                      eng=("act" if bnk % 2 == 0 else "dve"))
            for hp in range(16):
                h, p_ = hp // 2, hp % 2
                sv8a = sv[:nt, h, p_, 0:8]; sv8b = sv[:nt, h, p_, 8:16]
                kb.op("dve", lambda e, hp=hp, o=sv8a: e.max(out=o, in_=s_sb[:nt, hp, :]), reads=[s_sb], writes=[sv])
                kb.op("dve", lambda e, hp=hp, o=sv8a, h=h, p_=p_: e.max_index(out=si[:nt, h, p_, 0:8], in_max=o, in_values=s_sb[:nt, hp, :]),
                      reads=[s_sb, sv], writes=[si])
                kb.op("dve", lambda e, hp=hp, o=sv8a: e.match_replace(out=mtmp[:nt, :], in_to_replace=o, in_values=s_sb[:nt, hp, :], imm_value=-1e30),
                      reads=[s_sb, sv], writes=[mtmp])
                kb.op("dve", lambda e, o=sv8b: e.max(out=o, in_=mtmp[:nt, :]), reads=[mtmp], writes=[sv])
                kb.op("dve", lambda e, o=sv8b, h=h, p_=p_: e.max_index(out=si[:nt, h, p_, 8:16], in_max=o, in_values=mtmp[:nt, :]),
                      reads=[mtmp, sv], writes=[si])
            sva = sv[:nt, :, 0, :]
            svb = sv[:nt, :, 1, :]
            in0 = bass.AP(sva.tensor, sva.offset, [list(sva.ap[0]), [1, 16], [32, 8], [0, 16]])
            in1 = bass.AP(svb.tensor, svb.offset, [list(svb.ap[0]), [0, 16], [32, 8], [1, 16]])
            kb.op("dve", lambda e: e.tensor_tensor(out=cand[:nt], in0=in0, in1=in1, op=ALU.add), reads=[sv], writes=[cand])
            for h in range(8):
                ch = cand[:nt, :, h, :]
                kb.op("dve", lambda e, h=h, ch=ch: e.max(out=c16[:nt, h, 0:8], in_=ch), reads=[cand], writes=[c16])
                kb.op("dve", lambda e, h=h, ch=ch: e.match_replace(out=ctmp[:nt], in_to_replace=c16[:nt, h, 0:8], in_values=ch, imm_value=-1e30),
                      reads=[cand, c16], writes=[ctmp])
                kb.op("dve", lambda e, h=h: e.max(out=c16[:nt, h, 8:16], in_=ctmp[:nt]), reads=[ctmp], writes=[c16])
            kb.ts(nmx, nmx[:nt, :], c16, c16[:nt, :, 0], -1.0, ALU.mult)
            E = s_sb
            Ev = E[:nt].rearrange("p a b -> p (a b)").rearrange("p (a h b) -> p a h b", a=16, h=8)
            for h in range(8):
                kb.act(E, Ev[:, :, h, :], cand, cand[:nt, :, h, :], AF.Exp, extra_reads=[nmx], bias=nmx[:nt, h:h + 1], scale=1.0)
            for h in range(8):
                kb.stt(E, Ev[:, :, h, :], cand, cand[:nt, :, h, :], c16[:nt, h, 15:16], E, Ev[:, :, h, :], ALU.is_ge, ALU.mult, sr=[c16])
            Eperm = bass.AP(Ev.tensor, Ev.offset, [list(Ev.ap[0]), [16, 8], [128, 16], [1, 16]])
            kb.op("dve", lambda e: e.tensor_reduce(out=zz[:nt, :], in_=Eperm, axis=AX.XY, op=ALU.add), reads=[E], writes=[zz])
            kb.op("dve", lambda e: e.reciprocal(out=zz[:nt, :], in_=zz[:nt, :]), reads=[zz], writes=[zz])
            zb = zz[:nt, :]
            zbc = bass.AP(zb.tensor, zb.offset, [list(zb.ap[0]), [0, 16], [1, 8], [0, 16]])
            kb.op("dve", lambda e: e.tensor_tensor(out=w_bf[:nt], in0=Ev, in1=zbc, op=ALU.mult), reads=[E, zz], writes=[w_bf])
            for p_ in range(2):
                kb.cp(sif, sif[:nt, p_, :, :], si, si[:nt, :, p_, :], eng="dve")
            for p_ in range(2):
                tp = P[6]
                kb.tr(tp, tp[:, 0:nt], sif, sif[:nt, p_, :, :].rearrange("p a b -> p (a b)"), ident, ident[:nt, :nt])
                kb.cp(SIT, SIT[:, p_, sb0:sb0 + nt], tp, tp[:, 0:nt], eng="dve")
            for a0 in range(0, 16, 4):
                for aa in range(4):
                    a = a0 + aa
                    kb.tr(P7b, P7b[:, aa * 128:aa * 128 + nt], w_bf, w_bf[:nt, a, :, :].rearrange("p a b -> p (a b)"),
                          ident_bf, ident_bf[:nt, :nt])
                kb.cp(WT, WT[:, a0:a0 + 4, sb0:sb0 + nt], P7b, P7b[:, 0:512].rearrange("p (a b) -> p a b", b=128)[:, :, :nt])
        for tq in range(0, ncol, 4):
            g4 = (tq // 4) % 3
            XP = P[g4]; GP = P[3 + g4]
            xs = XS[g4]
            o2 = oh2q[g4]; o1 = oh1q[g4]; w_ = wbq[g4]
            for tt_ in range(4):
                t = tq + tt_
                kb.ts(o2, o2[:, tt_, :], iot_bf, iot_bf[:], SIT[:, 1, t:t + 1], ALU.is_equal, sr=[SIT])
                kb.ts(o1, o1[:, tt_, :], iot_bf, iot_bf[:], SIT[:, 0, t:t + 1], ALU.is_equal, sr=[SIT])
            for tt_ in range(4):
                wc = WT[:, :, tq + tt_]
                wbc = bass.AP(wc.tensor, wc.offset, [list(wc.ap[0]), [0, 8], [TW, 16]])
                kb.op("pool", lambda e, w_=w_, wbc=wbc, tt_=tt_: e.tensor_tensor(
                    out=w_[:, tt_, :].rearrange("p (a b) -> p a b", b=16),
                    in0=bm[:].rearrange("p (a b) -> p a b", b=16), in1=wbc, op=ALU.mult),
                    reads=[bm, WT], writes=[w_])
            for tt_ in range(4):
                kb.mm(XP, XP[:, tt_ * 128:(tt_ + 1) * 128], w_, w_[:, tt_, :], o2, o2[:, tt_, :])
            kb.cp(xs, xs[:], XP, XP[:].rearrange("p (a b) -> p a b", b=128))
            for tt_ in range(4):
                kb.mm(GP, GP[:, tt_ * 128:(tt_ + 1) * 128], xs, xs[:, tt_, :], o1, o1[:, tt_, :])
            kb.op("act", lambda e, GP=GP, tq=tq: e.activation(out=GT[:, tq:tq + 4, :], in_=GP[:].rearrange("p (a b) -> p a b", b=128), func=AF.Copy),
                  reads=[GP], writes=[GT])
        for c in range(NCH):
            ub = u_bf[c % NUB]; vb = v_bf[c % NUB]
            kb.dma("sp", ub[:], uB[c], reads=[uBv[c]], writes=[ub])
            kb.dma("sp", vb[:], vB[c * 128:(c + 1) * 128, :], reads=[vBv[c]], writes=[vb])
            ap_ = P[4 + c % 2]
            for k in range(KC):
                kb.mm(ap_, ap_[:, :ncol], ub, ub[:, k, :], h2_bf, h2_bf[:, k, :ncol], start=(k == 0), stop=(k == KC - 1))
            g_ = gl[c % 2]; a_ = ga[c % 2]
            kb.act(g_, g_[:, :ncol], ap_, ap_[:, :ncol], AF.Gelu)
            kb.tt(a_, a_[:, :ncol], g_, g_[:, :ncol], GT, GT[:, :ncol, c], ALU.mult, eng="pool")
            for m in range(KC):
                op_ = P[m // 2]
                kb.mm(op_, op_[:, (m % 2) * 256:(m % 2) * 256 + ncol], vb, vb[:, m * 128:(m + 1) * 128], a_, a_[:, :ncol],
                      start=(c == 0 and m % 2 == 0), stop=(c == NCH - 1), skip=True)
        for m in range(KC):
            op_ = P[m // 2]
            o_ = ost[m % 2]
            if dbg:
                o2_ = dbgp
                kb.cp(o2_, o2_[:, :ncol], op_, op_[:, (m % 2) * 256:(m % 2) * 256 + ncol], eng="dve")
                kb.dma("sp", d_peer[m * 128:(m + 1) * 128, t0:t0 + ncol], o2_[:, :ncol], reads=[o2_], writes=[])
            kb.stt(o_, o_[:, :ncol], op_, op_[:, (m % 2) * 256:(m % 2) * 256 + ncol], mod[:, 24 + m, s:s + 1], xt, xt[:, m, :ncol],
                   ALU.mult, ALU.add, sr=[mod])
            kb.dma("sp", xoT[m * 128:(m + 1) * 128, t0:t0 + ncol], o_[:, :ncol], reads=[o_], writes=[])
    return kb


DEPTH = 4
B_ = 2
T_ = 16384
CTX_ = 256
NCORE = 8
NLAT_C = T_ // 4
NCTX_C = CTX_ // 4


def _lay_c(c_b, c_ctx):
    cT = np.zeros((128, 8, 2), np.float32)
    cT[:, :, 0] = c_b.reshape(8, 128).T
    cT[:, :, 1] = c_ctx.reshape(8, 128).T
    return cT


def _fm(v):
    return np.ascontiguousarray(np.asarray(v, np.float32).reshape(-1, 128).T)


def _dn_inputs(pb, j, conv_w, A_log, dt_bias, dn_norm_w, cm, TL=T_, TC=CTX_):
    PADW = TL + 4 + TC + 4
    qkvP = np.zeros((128, 3, PADW), np.float32)
    cwT = np.zeros((128, 3, 5), np.float32)
    for w in range(3):
        rows = slice(1024 + w * 512 + j * 128, 1024 + w * 512 + (j + 1) * 128)
        qkvP[:, w, 2:2 + TL] = pb[rows, :TL]
        qkvP[:, w, TL + 6:TL + 6 + TC] = pb[rows, TL:]
        cwT[:, w, :] = conv_w[:, w * 512 + j * 128: w * 512 + (j + 1) * 128].T
    gateT = np.ascontiguousarray(pb[2560 + j * 128: 2560 + (j + 1) * 128])
    baT = np.ascontiguousarray(pb[[3072 + j, 3076 + j, 3080 + j, 3084 + j]])
    cst = np.zeros((128, 8), np.float32)
    cst[:, 0] = A_log[0, j]; cst[:, 1] = A_log[1, j]; cst[:, 2] = dt_bias[0, j]; cst[:, 3] = dt_bias[1, j]
    cst[:, 4] = dn_norm_w
    return {"qkvP": qkvP, "gateT": gateT, "baT": baT, "cwT": cwT, "cst": cst, "cm": cm}


def kernel(x, c, ctx, c_ctx, ada_w, ada_b, norm1_w, norm2_w, w_in, attn_qnorm_w, attn_knorm_w,
           dn_conv_w, dn_A_log, dn_dt_bias, dn_norm_w, w_out, peer_wq, peer_subkeys, peer_u, peer_v):
    f32 = np.float32
    x = np.asarray(x, f32); ctx = np.asarray(ctx, f32); c = np.asarray(c, f32); c_ctx = np.asarray(c_ctx, f32)
    ada_w = np.asarray(ada_w, f32); ada_b = np.asarray(ada_b, f32)
    w_in = np.asarray(w_in, f32); w_out = np.asarray(w_out, f32)
    peer_wq = np.asarray(peer_wq, f32); peer_subkeys = np.asarray(peer_subkeys, f32)
    peer_u = np.asarray(peer_u, f32); peer_v = np.asarray(peer_v, f32)
    dn_conv_w = np.asarray(dn_conv_w, f32)

    XT = []
    for core in range(NCORE):
        b, r = core // 4, core % 4
        xs = np.concatenate([x[b, r * NLAT_C:(r + 1) * NLAT_C], ctx[b, r * NCTX_C:(r + 1) * NCTX_C]], axis=0)
        XT.append(np.ascontiguousarray(xs.T))
    cTs = [_lay_c(c[core // 4], c_ctx) for core in range(NCORE)]
    cos, sin = rope_tables()
    cosT = np.ascontiguousarray(cos.T); sinT = np.ascontiguousarray(sin.T)
    RmT = rot_matrix_T(); ident = np.eye(128, dtype=f32)
    cm = dn_consts(); bmc = peer_consts()

    for l in range(DEPTH):
        ada_bT = np.ascontiguousarray(ada_b[l].reshape(48, 128).T)
        kbA = build_A()
        adaA = np.ascontiguousarray(ada_w[l][:, :2048])
        n1T = _fm(norm1_w[l])
        resA = kbA.run([{"xT": XT[core], "cT": cTs[core], "ada_w": adaA, "ada_bT": ada_bT, "n1T": n1T, "w_in": w_in[l]}
                        for core in range(NCORE)]).results
        pbs = []
        for b in range(B_):
            cs = range(4 * b, 4 * b + 4)
            pbs.append(np.concatenate([resA[cc]["pT"][:, :NLAT_C] for cc in cs] + [resA[cc]["pT"][:, NLAT_C:] for cc in cs], axis=1))
        del resA
        kbB = build_attn()
        qkw = np.stack([np.asarray(attn_qnorm_w[l], f32), np.asarray(attn_knorm_w[l], f32)], axis=1).astype(f32)
        mapsB = []
        for core in range(NCORE):
            b, j = core // 4, core % 4
            pb = pbs[b]
            mapsB.append({"qT": np.ascontiguousarray(pb[j * 128:(j + 1) * 128]),
                          "kT": np.ascontiguousarray(pb[512 + (j // 2) * 128: 512 + (j // 2 + 1) * 128]),
                          "vT": np.ascontiguousarray(pb[768 + (j // 2) * 128: 768 + (j // 2 + 1) * 128]),
                          "cosT": cosT, "sinT": sinT, "RmT": RmT, "ident": ident, "qkw": qkw})
        resB = kbB.run(mapsB).results
        del mapsB
        kbD = build_dn()
        mapsD = [_dn_inputs(pbs[core // 4], core % 4, dn_conv_w[l], np.asarray(dn_A_log[l], f32),
                            np.asarray(dn_dt_bias[l], f32), np.asarray(dn_norm_w[l], f32), cm) for core in range(NCORE)]
        resD = kbD.run(mapsD).results
        del mapsD, pbs
        mixb = []
        for b in range(B_):
            mixb.append(np.concatenate([resB[4 * b + j]["oT"] for j in range(4)] + [resD[4 * b + j]["yT"] for j in range(4)], axis=0))
        del resB, resD
        last = (l == DEPTH - 1)
        kbC = build_C(NLAT=NLAT_C, NCTX=0 if last else NCTX_C)
        adaC = np.ascontiguousarray(ada_w[l][:, 2048:])
        n2T = _fm(norm2_w[l])
        subkT = np.ascontiguousarray(peer_subkeys[l].reshape(16, 128, 128).transpose(0, 2, 1))
        uTr = np.ascontiguousarray(peer_u[l].reshape(128, 128, 8, 128).transpose(0, 3, 2, 1))
        mapsC = []
        for core in range(NCORE):
            b, r = core // 4, core % 4
            if last:
                mx = mixb[b][:, r * NLAT_C:(r + 1) * NLAT_C]
                xin = np.ascontiguousarray(XT[core][:, :NLAT_C])
            else:
                mx = np.concatenate([mixb[b][:, r * NLAT_C:(r + 1) * NLAT_C], mixb[b][:, T_ + r * NCTX_C: T_ + (r + 1) * NCTX_C]], axis=1)
                xin = XT[core]
            mapsC.append({"xT": xin, "mixT": np.ascontiguousarray(mx), "cT": cTs[core], "ada_w": adaC, "ada_bT": ada_bT,
                          "n2T": n2T, "w_out": w_out[l], "wq": peer_wq[l], "subkT": subkT, "uTr": uTr, "vtab": peer_v[l],
                          "bm": bmc, "ident": ident})
        resC = kbC.run(mapsC).results
        del mapsC, mixb, uTr
        XT = [np.ascontiguousarray(resC[core]["xoT"]) for core in range(NCORE)]
        del resC

    out = np.empty((B_, T_, 1024), f32)
    for core in range(NCORE):
        b, r = core // 4, core % 4
        out[b, r * NLAT_C:(r + 1) * NLAT_C, :] = XT[core][:, :NLAT_C].T
    return out
```

```python
import numpy as np
import concourse.bass as bass
import concourse.mybir as mybir
from concourse.bass_utils import run_bass_kernel_spmd

F32 = mybir.dt.float32
BF16 = mybir.dt.bfloat16
U32 = mybir.dt.uint32
AF = mybir.ActivationFunctionType
ALU = mybir.AluOpType
AX = mybir.AxisListType


class Tile:
    def __init__(self, h, is_dram=False, base=None):
        self.h = h
        self.lw = None
        self.rd = {}
        self.is_dram = is_dram
        if base is not None:
            self._ap = base
        else:
            self._ap = h.ap() if is_dram else h[:]

    def __getitem__(self, idx):
        return self._ap[idx]

    excl = False

    def view(self, idx):
        return Tile(self.h, self.is_dram, base=self._ap[idx])

    def sub(self, idx):
        return SubTile(self, self._ap[idx])


class SubTile:
    def __init__(self, parent, ap):
        self.parent = parent
        self._ap = ap

    def __getitem__(self, idx):
        return self._ap[idx]

    @property
    def excl(self):
        return self.parent.excl

    @property
    def lw(self):
        return self.parent.lw

    @lw.setter
    def lw(self, v):
        self.parent.lw = v

    @property
    def rd(self):
        return self.parent.rd

    @rd.setter
    def rd(self, v):
        self.parent.rd = v


class KB:
    ND = 24

    def __init__(self, num_devices=None):
        if num_devices:
            nc = bass.Bass("TRN2", target_bir_lowering=False, num_devices=num_devices)
        else:
            nc = bass.Bass("TRN2", target_bir_lowering=False)
        self.nc = nc
        self.engs = {"pe": nc.tensor, "dve": nc.vector, "act": nc.scalar,
                     "pool": nc.gpsimd, "sp": nc.sync}
        self.sems = {k: nc.alloc_semaphore("s_" + k) for k in self.engs}
        self.cnt = {k: 0 for k in self.engs}
        self.known = {k: {} for k in self.engs}
        self.dsem = []
        self.dq = {}
        for q, n in (("sp", 16), ("pool", 12), ("act", 2), ("dve", 2), ("pe", 2)):
            self.dq[q] = []
            for i in range(n):
                k = "d%s%d" % (q, i)
                self.sems[k] = nc.alloc_semaphore("s_" + k)
                self.dsem.append(k)
                self.dq[q].append(k)
        self.dval = {k: 0 for k in self.dsem}
        self.dnext = {q: 0 for q in self.dq}
        self.nuniq = 0
        try:
            nc.allow_low_precision("bf16 matmul operands, fp32 accumulate")
        except Exception:
            pass
        try:
            nc.allow_non_contiguous_dma("strided layouts")
        except Exception:
            pass

    def _nm(self, p):
        self.nuniq += 1
        return "%s_%d" % (p, self.nuniq)

    def sb(self, shape, dt=F32, name="sb"):
        return Tile(self.nc.alloc_sbuf_tensor(self._nm(name), list(shape), dt))

    def ps(self, shape, dt=F32, name="ps"):
        t = Tile(self.nc.alloc_psum_tensor(self._nm(name), list(shape), dt))
        t.excl = True
        return t

    def dram_in(self, name, shape, dt=F32):
        return Tile(self.nc.dram_tensor(name, list(shape), dt, kind="ExternalInput"), True)

    def dram_out(self, name, shape, dt=F32):
        return Tile(self.nc.dram_tensor(name, list(shape), dt, kind="ExternalOutput"), True)

    def dram_tmp(self, name, shape, dt=F32):
        return Tile(self.nc.dram_tensor(name, list(shape), dt, kind="Internal"), True)

    def _waits(self, eng, reads, writes):
        need = {}

        def req(ev):
            if ev is None:
                return
            k, v = ev
            if k == "pe" and eng == "pe":
                return
            if need.get(k, 0) < v:
                need[k] = v

        for t in reads:
            req(t.lw)
        for t in writes:
            req(t.lw)
            for k, v in t.rd.items():
                req((k, v))
        e = self.engs[eng]
        kn = self.known[eng]
        for k, v in need.items():
            if kn.get(k, 0) >= v:
                continue
            e.wait_ge(self.sems[k], v)
            kn[k] = v

    def _mark(self, ev, reads, writes):
        k, v = ev
        for t in reads:
            if t.rd.get(k, 0) < v:
                t.rd[k] = v
        for t in writes:
            t.lw = ev
            t.rd = {}

    rec = None

    def replay(self, *lists):
        self.rec = None
        n = max(len(l) for l in lists)
        for i in range(n):
            for l in lists:
                if i < len(l):
                    kind, a, fn, r, w, kw = l[i]
                    if kind == "op":
                        self.op(a, fn, r, w)
                    else:
                        self.dma(a, fn[0], fn[1], r, w, **kw)

    def op(self, eng, fn, reads=(), writes=()):
        if self.rec is not None:
            self.rec.append(("op", eng, fn, list(reads), list(writes), None))
            return
        xr = [t for t in reads if t.excl]
        if xr:
            reads = [t for t in reads if not t.excl]
            writes = list(writes) + xr
        self._waits(eng, reads, writes)
        inst = fn(self.engs[eng])
        self.cnt[eng] += 1
        inst.then_inc(self.sems[eng], 1)
        self._mark((eng, self.cnt[eng]), reads, writes)

    def dma(self, q, out, in_, reads=(), writes=(), **kw):
        if self.rec is not None:
            self.rec.append(("dma", q, (out, in_), list(reads), list(writes), kw))
            return
        pool_ = self.dq[q]
        k = pool_[self.dnext[q]]
        self.dnext[q] = (self.dnext[q] + 1) % len(pool_)
        e = self.engs[q]
        kn = self.known[q]
        if kn.get(k, 0) < self.dval[k]:
            e.wait_ge(self.sems[k], self.dval[k])
            kn[k] = self.dval[k]
        self._waits(q, reads, writes)
        inst = e.dma_start(out=out, in_=in_, **kw)
        self.dval[k] += 16
        inst.then_inc(self.sems[k], 16)
        self._mark((k, self.dval[k]), reads, writes)

    def barrier(self):
        for q, e in self.engs.items():
            kn = self.known[q]
            for k in self.dsem:
                if self.dval[k] > kn.get(k, 0):
                    e.wait_ge(self.sems[k], self.dval[k])
                    kn[k] = self.dval[k]
            for k in ("pe", "dve", "act", "pool", "sp"):
                if k != q and self.cnt[k] > kn.get(k, 0):
                    e.wait_ge(self.sems[k], self.cnt[k])
                    kn[k] = self.cnt[k]

    def finish(self):
        e = self.engs["sp"]
        kn = self.known["sp"]
        for k in self.dsem:
            if self.dval[k] > kn.get(k, 0):
                e.wait_ge(self.sems[k], self.dval[k])
                kn[k] = self.dval[k]
        for k in ("pe", "dve", "act", "pool"):
            if self.cnt[k] > kn.get(k, 0):
                e.wait_ge(self.sems[k], self.cnt[k])
                kn[k] = self.cnt[k]

    def mm(self, out_t, out_ap, lhsT_t, lhsT_ap, rhs_t, rhs_ap, start=True, stop=True, skip=False):
        if skip:
            self.op("pe", lambda e: e.matmul(out_ap, lhsT_ap, rhs_ap, start=start, stop=stop, skip_group_check=True),
                    reads=[lhsT_t, rhs_t], writes=[out_t])
        else:
            self.op("pe", lambda e: e.matmul(out_ap, lhsT_ap, rhs_ap, start=start, stop=stop),
                    reads=[lhsT_t, rhs_t], writes=[out_t])

    def tr(self, out_t, out_ap, in_t, in_ap, ident_t, ident_ap):
        self.op("pe", lambda e: e.transpose(out_ap, in_ap, ident_ap),
                reads=[in_t, ident_t], writes=[out_t])

    def act(self, out_t, out_ap, in_t, in_ap, func, extra_reads=(), eng="act", **kw):
        self.op(eng, lambda e: e.activation(out=out_ap, in_=in_ap, func=func, **kw),
                reads=[in_t] + list(extra_reads), writes=[out_t])

    def tt(self, ot, oap, at, aap, bt, bap, op, eng="dve"):
        self.op(eng, lambda e: e.tensor_tensor(out=oap, in0=aap, in1=bap, op=op), reads=[at, bt], writes=[ot])

    def ts(self, ot, oap, at, aap, s1, op0, s2=None, op1=None, sr=(), eng="dve"):
        if op1 is None:
            self.op(eng, lambda e: e.tensor_scalar(out=oap, in0=aap, scalar1=s1, scalar2=None, op0=op0),
                    reads=[at] + list(sr), writes=[ot])
        else:
            self.op(eng, lambda e: e.tensor_scalar(out=oap, in0=aap, scalar1=s1, scalar2=s2, op0=op0, op1=op1),
                    reads=[at] + list(sr), writes=[ot])

    def stt(self, ot, oap, at, aap, sc, bt, bap, op0, op1, sr=()):
        self.op("dve", lambda e: e.scalar_tensor_tensor(out=oap, in0=aap, scalar=sc, in1=bap, op0=op0, op1=op1),
                reads=[at, bt] + list(sr), writes=[ot])

    def cp(self, ot, oap, it, iap, eng="act"):
        if eng == "act":
            self.op("act", lambda e: e.activation(out=oap, in_=iap, func=AF.Copy), reads=[it], writes=[ot])
        else:
            self.op(eng, lambda e: e.tensor_copy(out=oap, in_=iap), reads=[it], writes=[ot])

    def run(self, in_maps, n=8, trace=False):
        self.finish()
        if trace:
            return run_bass_kernel_spmd(self.nc, in_maps, core_ids=list(range(n)), trace=True)
        return run_bass_kernel_spmd(self.nc, in_maps, core_ids=list(range(n)))


D = 1024
KC = 8
INW = 3088


def emit_mod(kb, cT, ada_w, ada_bT, j0, j1, wt=None, bw=512, modp=None, col_base=0):
    nj = j1 - j0
    sc = kb.sb([128, KC, 2], name="silu_c")
    craw = kb.sb([128, KC, 2], name="craw")
    kb.dma("sp", craw[:], cT[:], writes=[craw])
    kb.act(sc, sc[:], craw, craw[:], AF.Silu)
    if modp is None:
        modp = kb.ps([128, nj, 2], name="modp")
    else:
        modp = modp.sub((slice(None), slice(0, nj * 2)))
        modp = SubTile(modp.parent, modp[:].rearrange("p (a b) -> p a b", b=2))
    if wt is None:
        wt = [kb.sb([128, KC, bw], name="adaw") for _ in range(2)]
    ada_v = ada_w[:].rearrange("(k p) n -> p k n", p=128)
    npb = bw // 128
    for jb in range(0, nj, npb):
        t = wt[(jb // npb) % 2]
        c0 = (j0 + jb) * 128 - col_base
        kb.dma("sp", t[:], ada_v[:, :, c0:c0 + bw], writes=[t])
        for jj in range(npb):
            j = jb + jj
            for k in range(KC):
                kb.mm(modp, modp[:, j, :], t, t[:, k, jj * 128:(jj + 1) * 128], sc, sc[:, k, :],
                      start=(k == 0), stop=(k == KC - 1))
    bT = kb.sb([128, nj], name="adab")
    kb.dma("sp", bT[:], ada_bT[:, j0:j1], writes=[bT])
    mod = kb.sb([128, nj, 2], name="mod")
    for s in range(2):
        kb.op("dve", lambda e, s=s: e.tensor_tensor(out=mod[:, :, s], in0=modp[:, :, s], in1=bT[:], op=ALU.add),
              reads=[modp, bT], writes=[mod])
    return mod


def emit_norm_mod(kb, xt, hT, ones, eps_t, a_sc, b_sh, s, ncol, sqs, ssp, rstd, hdt_tmp):
    for k in range(KC):
        sq = sqs[k % 2]
        kb.act(sq, sq[:, :ncol], xt, xt[:, k, :ncol], AF.Square)
        kb.mm(ssp, ssp[:, :ncol], ones, ones[:], sq, sq[:, :ncol], start=(k == 0), stop=(k == KC - 1))
    kb.act(rstd, rstd[:, :ncol], ssp, ssp[:, :ncol], AF.Sqrt, extra_reads=[eps_t], bias=eps_t[:, 0:1], scale=1.0 / D)
    kb.op("dve", lambda e: e.reciprocal(out=rstd[:, :ncol], in_=rstd[:, :ncol]), reads=[rstd], writes=[rstd])
    for k in range(KC):
        tmp = hdt_tmp[k % 2]
        kb.op("dve", lambda e, k=k, tmp=tmp: e.scalar_tensor_tensor(
            out=tmp[:, :ncol], in0=xt[:, k, :ncol], scalar=a_sc[:, k, s:s + 1], in1=rstd[:, :ncol],
            op0=ALU.mult, op1=ALU.mult), reads=[xt, a_sc, rstd], writes=[tmp])
        kb.act(hT, hT[:, k, :ncol], tmp, tmp[:, :ncol], AF.Identity, extra_reads=[b_sh],
               bias=b_sh[:, k, s:s + 1], scale=1.0)


def build_A(NLAT=4096, NCTX=64):
    kb = KB()
    NT = NLAT + NCTX
    xT = kb.dram_in("xT", [D, NT])
    cT = kb.dram_in("cT", [128, KC, 2])
    ada_w = kb.dram_in("ada_w", [D, 2 * D])
    ada_bT = kb.dram_in("ada_bT", [128, 48])
    n1T = kb.dram_in("n1T", [128, KC])
    w_in = kb.dram_in("w_in", [D, INW])
    pT = kb.dram_out("pT", [INW, NT])

    ones = kb.sb([128, 128], name="ones")
    kb.op("dve", lambda e: e.memset(ones[:], 1.0), writes=[ones])
    eps_t = kb.sb([128, 1], name="eps")
    kb.op("dve", lambda e: e.memset(eps_t[:], 1e-6), writes=[eps_t])

    w_bf = kb.sb([128, KC, INW], BF16, name="w_bf")
    wv = w_in[:].rearrange("(k p) n -> p k n", p=128)
    for k in range(KC):
        kb.dma("pool", w_bf[:, k, :], wv[:, k, :], writes=[w_bf])

    mod = emit_mod(kb, cT, ada_w, ada_bT, 0, 16)
    n1 = kb.sb([128, KC], name="n1")
    kb.dma("sp", n1[:], n1T[:], writes=[n1])
    a_sc = kb.sb([128, KC, 2], name="a_sc")
    b_sh = kb.sb([128, KC, 2], name="b_sh")
    for s in range(2):
        kb.op("dve", lambda e, s=s: e.scalar_tensor_tensor(
            out=a_sc[:, :, s], in0=mod[:, 8:16, s], scalar=1.0, in1=n1[:], op0=ALU.add, op1=ALU.mult),
            reads=[mod, n1], writes=[a_sc])
        kb.op("dve", lambda e, s=s: e.tensor_copy(out=b_sh[:, :, s], in_=mod[:, 0:8, s]), reads=[mod], writes=[b_sh])

    xts = [kb.sb([128, KC, 512], name="xt") for _ in range(2)]
    hTs = [kb.sb([128, KC, 512], BF16, name="hT") for _ in range(2)]
    sqs = [kb.sb([128, 512], name="sq") for _ in range(2)]
    tmps = [kb.sb([128, 512], name="tmp") for _ in range(2)]
    rstd = kb.sb([128, 512], name="rstd")
    ssp = kb.ps([128, 512], name="ssp")
    pps = [kb.ps([128, 512], name="pp") for _ in range(4)]
    stg = [kb.sb([128, 512], name="stg") for _ in range(4)]
    xv = xT[:].rearrange("(k p) n -> p k n", p=128)

    tiles = [(t0, 512, 0) for t0 in range(0, NLAT, 512)]
    if NCTX:
        tiles.append((NLAT, NCTX, 1))
    MCH = [(m * 128, 128) for m in range(INW // 128)] + [(INW // 128 * 128, INW % 128)]
    cnt = 0
    def load_x(ti):
        t0, ncol, s = tiles[ti]
        for k in range(KC):
            kb.dma("sp", xts[ti % 2][:, k, :ncol], xv[:, k, t0:t0 + ncol], writes=[xts[ti % 2]])
    load_x(0)
    for ti, (t0, ncol, s) in enumerate(tiles):
        xt = xts[ti % 2]
        hT = hTs[ti % 2]
        if ti + 1 < len(tiles):
            load_x(ti + 1)
        emit_norm_mod(kb, xt, hT, ones, eps_t, a_sc, b_sh, s, ncol, sqs, ssp, rstd, tmps)
        for (m0, mw) in MCH:
            pp = pps[cnt % 4]
            st = stg[cnt % 4]
            for k in range(KC):
                kb.mm(pp, pp[:mw, :ncol], w_bf, w_bf[:, k, m0:m0 + mw], hT, hT[:, k, :ncol],
                      start=(k == 0), stop=(k == KC - 1))
            if cnt % 2 == 0:
                kb.act(st, st[:mw, :ncol], pp, pp[:mw, :ncol], AF.Copy)
            else:
                kb.op("dve", lambda e, st=st, pp=pp, mw=mw: e.tensor_copy(out=st[:mw, :ncol], in_=pp[:mw, :ncol]),
                      reads=[pp], writes=[st])
            kb.dma("sp", pT[m0:m0 + mw, t0:t0 + ncol], st[:mw, :ncol], reads=[st], writes=[])
            cnt += 1
    return kb


HD = 128


def build_attn(TL=16384, TC=256):
    kb = KB()
    TT = TL + TC
    qT = kb.dram_in("qT", [HD, TT])
    kT = kb.dram_in("kT", [HD, TT])
    vT = kb.dram_in("vT", [HD, TT])
    cosT = kb.dram_in("cosT", [HD, TL])
    sinT = kb.dram_in("sinT", [HD, TL])
    RmT_d = kb.dram_in("RmT", [HD, HD])
    ident_d = kb.dram_in("ident", [HD, HD])
    qkw_d = kb.dram_in("qkw", [HD, 2])
    oT = kb.dram_out("oT", [HD, TT])

    ones = kb.sb([128, 128], name="ones")
    kb.op("dve", lambda e: e.memset(ones[:], 1.0), writes=[ones])
    ones_bf = kb.sb([128, 128], BF16, name="ones_bf")
    kb.op("dve", lambda e: e.memset(ones_bf[:], 1.0), writes=[ones_bf])
    eps_t = kb.sb([128, 1], name="eps")
    kb.op("dve", lambda e: e.memset(eps_t[:], 1e-6), writes=[eps_t])
    RmT = kb.sb([128, 128], name="RmT")
    ident = kb.sb([128, 128], name="ident")
    qkw = kb.sb([128, 2], name="qkw")
    kb.dma("sp", RmT[:], RmT_d[:], writes=[RmT])
    kb.dma("sp", ident[:], ident_d[:], writes=[ident])
    kb.dma("sp", qkw[:], qkw_d[:], writes=[qkw])

    Q_bf = kb.sb([128, TT], BF16, name="Q_bf")
    K_bf = kb.sb([128, TT], BF16, name="K_bf")
    NKT = TT // 128
    V_bf = kb.sb([128, NKT, 128], BF16, name="V_bf")

    P = [kb.ps([128, 512], name="P") for _ in range(8)]
    raws = [[kb.sb([128, 512], name="raw") for _ in range(2)] for _ in range(3)]
    cs = [kb.sb([128, 512], name="cs") for _ in range(2)]
    sn = [kb.sb([128, 512], name="sn") for _ in range(2)]
    sqs = [kb.sb([128, 512], name="sq") for _ in range(2)]
    rstds = [kb.sb([128, 512], name="rstd") for _ in range(2)]
    xns = [kb.sb([128, 512], name="xn") for _ in range(2)]
    t1s = [kb.sb([128, 512], name="t1") for _ in range(2)]
    t2s = [kb.sb([128, 512], name="t2") for _ in range(2)]
    pps = [(P[0], P[1]), (P[5], P[6])]

    tiles = [(t0, 512, True) for t0 in range(0, TL, 512)]
    for t0 in range(TL, TT, 512):
        tiles.append((t0, min(512, TT - t0), False))
    for ti, (t0, nc_, lat) in enumerate(tiles):
        if lat:
            c_t = cs[ti % 2]; s_t = sn[ti % 2]
            kb.dma("sp", c_t[:, :nc_], cosT[:, t0:t0 + nc_], writes=[c_t])
            kb.dma("sp", s_t[:, :nc_], sinT[:, t0:t0 + nc_], writes=[s_t])
        chains = []
        for wi, (src, dst) in enumerate(((qT, Q_bf), (kT, K_bf))):
            kb.rec = []
            r = raws[wi][ti % 2]
            sq = sqs[wi]; rstd = rstds[wi]; xn = xns[wi]; t1 = t1s[wi]; t2 = t2s[wi]
            pa, pb_ = pps[wi]
            kb.dma("sp", r[:, :nc_], src[:, t0:t0 + nc_], writes=[r])
            kb.act(sq, sq[:, :nc_], r, r[:, :nc_], AF.Square)
            kb.mm(pa, pa[:, :nc_], ones, ones[:], sq, sq[:, :nc_])
            kb.act(rstd, rstd[:, :nc_], pa, pa[:, :nc_], AF.Sqrt, extra_reads=[eps_t], bias=eps_t[:, 0:1], scale=1.0 / HD)
            kb.op("dve", lambda e, rstd=rstd: e.reciprocal(out=rstd[:, :nc_], in_=rstd[:, :nc_]), reads=[rstd], writes=[rstd])
            if lat:
                kb.op("dve", lambda e, r=r, wi=wi, xn=xn, rstd=rstd: e.scalar_tensor_tensor(
                    out=xn[:, :nc_], in0=r[:, :nc_], scalar=qkw[:, wi:wi + 1], in1=rstd[:, :nc_], op0=ALU.mult, op1=ALU.mult),
                    reads=[r, qkw, rstd], writes=[xn])
                kb.mm(pb_, pb_[:, :nc_], RmT, RmT[:], xn, xn[:, :nc_])
                kb.op("dve", lambda e, c_t=c_t, xn=xn, t1=t1: e.tensor_tensor(out=t1[:, :nc_], in0=xn[:, :nc_], in1=c_t[:, :nc_], op=ALU.mult),
                      reads=[xn, c_t], writes=[t1])
                kb.op("dve", lambda e, s_t=s_t, pb_=pb_, t2=t2: e.tensor_tensor(out=t2[:, :nc_], in0=pb_[:, :nc_], in1=s_t[:, :nc_], op=ALU.mult),
                      reads=[pb_, s_t], writes=[t2])
                kb.op("dve", lambda e, dst=dst, t1=t1, t2=t2: e.tensor_tensor(out=dst[:, t0:t0 + nc_], in0=t1[:, :nc_], in1=t2[:, :nc_], op=ALU.add),
                      reads=[t1, t2], writes=[dst])
            else:
                kb.op("dve", lambda e, r=r, wi=wi, dst=dst, rstd=rstd: e.scalar_tensor_tensor(
                    out=dst[:, t0:t0 + nc_], in0=r[:, :nc_], scalar=qkw[:, wi:wi + 1], in1=rstd[:, :nc_], op0=ALU.mult, op1=ALU.mult),
                    reads=[r, qkw, rstd], writes=[dst])
            chains.append(kb.rec)
        kb.rec = []
        r = raws[2][ti % 2]
        kb.dma("sp", r[:, :nc_], vT[:, t0:t0 + nc_], writes=[r])
        nb = nc_ // 128
        for bi in range(nb):
            kb.tr(P[2], P[2][:, bi * 128:(bi + 1) * 128], r, r[:, bi * 128:(bi + 1) * 128], ident, ident[:])
        kt0 = t0 // 128
        kb.act(V_bf, V_bf[:, kt0:kt0 + nb, :], P[2], P[2][:, :nb * 128].rearrange("p (a b) -> p a b", b=128), AF.Copy)
        chains.append(kb.rec)
        kb.replay(*chains)

    scale = float(HD) ** -0.5
    pts = [kb.sb([128, 512], BF16, name="pt") for _ in range(3)]
    rs = kb.sb([128, 512], name="rs")
    ost = [kb.sb([128, 512], name="ost") for _ in range(2)]
    acc = [(P[0], P[1]), (P[5], P[6])]
    sacc = [[kb.sb([128, 512], name="sacc") for _ in range(2)] for _ in range(2)]
    stot = kb.sb([128, 512], name="stot")
    sTs = [P[3], P[4], P[7]]
    qtiles = [(q0, 512, 0, NKT) for q0 in range(0, TL, 512)]
    for q0 in range(TL, TT, 512):
        qtiles.append((q0, min(512, TT - q0), TL // 128, NKT))
    work = []
    for qi, (q0, nq, kta, ktb) in enumerate(qtiles):
        for kt in range(kta, ktb):
            work.append((qi, q0, nq, kt, kt == kta, kt == ktb - 1))

    def issue_s(n):
        qi, q0, nq, kt, first, last = work[n]
        sT = sTs[n % 3]
        kb.mm(sT, sT[:, :nq], K_bf, K_bf[:, kt * 128:(kt + 1) * 128], Q_bf, Q_bf[:, q0:q0 + nq])

    issue_s(0)
    for n in range(len(work)):
        qi, q0, nq, kt, first, last = work[n]
        oP, sP = acc[qi % 2]
        sT = sTs[n % 3]; pt = pts[n % 3]
        if n + 1 < len(work):
            issue_s(n + 1)
        kb.act(pt, pt[:, :nq], sT, sT[:, :nq], AF.Exp, scale=scale)
        kb.mm(oP, oP[:, :nq], V_bf, V_bf[:, kt, :], pt, pt[:, :nq], start=first, stop=last)
        kk = (kt - work[n][3] + (kt % 2)) % 2 if False else (kt % 2)
        sa = sacc[qi % 2][kk]
        eng_ = "dve" if kk == 0 else "pool"
        kfirst = (kt - (TL // 128 if q0 >= TL else 0)) < 2
        if kfirst:
            kb.cp(sa, sa[:, :nq], pt, pt[:, :nq], eng=eng_)
        else:
            kb.tt(sa, sa[:, :nq], sa, sa[:, :nq], pt, pt[:, :nq], ALU.add, eng=eng_)
        if last:
            st = ost[qi % 2]
            kb.tt(stot, stot[:, :nq], sacc[qi % 2][0], sacc[qi % 2][0][:, :nq], sacc[qi % 2][1], sacc[qi % 2][1][:, :nq], ALU.add)
            kb.mm(sP, sP[:, :nq], ones, ones[:], stot, stot[:, :nq])
            kb.op("dve", lambda e, sP=sP, nq=nq: e.reciprocal(out=rs[:, :nq], in_=sP[:, :nq]), reads=[sP], writes=[rs])
            kb.op("dve", lambda e, oP=oP, st=st, nq=nq: e.tensor_tensor(out=st[:, :nq], in0=oP[:, :nq], in1=rs[:, :nq], op=ALU.mult),
                  reads=[oP, rs], writes=[st])
            kb.dma("sp", oT[:, q0:q0 + nq], st[:, :nq], reads=[st], writes=[])
    return kb


def rope_tables(T=16384, GRID_W=64):
    rows = T // GRID_W
    row = np.repeat(np.arange(rows, dtype=np.float32), GRID_W)
    col = np.tile(np.arange(GRID_W, dtype=np.float32), rows)
    axis_dim = HD // 2
    inv_freq = (10000.0 ** (-np.arange(0, axis_dim, 2, dtype=np.float32) / axis_dim)).astype(np.float32)
    ang_r = row[:, None] * inv_freq[None, :]
    ang_c = col[:, None] * inv_freq[None, :]
    ang = np.concatenate([ang_r, ang_r, ang_c, ang_c], axis=-1)
    return np.cos(ang).astype(np.float32), np.sin(ang).astype(np.float32)


def rot_matrix_T():
    R = np.zeros((128, 128), np.float32)
    for dp in range(128):
        if (dp % 64) < 32:
            R[dp, dp + 32] = -1.0
        else:
            R[dp, dp - 32] = 1.0
    return np.ascontiguousarray(R.T)


def dn_consts():
    p = np.arange(128)[:, None]
    q = np.arange(128)[None, :]
    same = (p // 64) == (q // 64)
    cm = np.zeros((6, 128, 128), np.float32)
    cm[0] = np.eye(128)
    cm[1] = (same & (q <= p))
    cm[2] = (same & (q >= p))
    cm[3] = same
    cm[4] = (p < 64) * np.ones((1, 128))
    cm[5] = (p >= 64) * np.ones((1, 128))
    return cm


def build_dn(TL=16384, TC=256, stage=9):
    kb = KB()
    TT = TL + TC
    NB = TT // 128
    NBL = TL // 128
    PADW = TL + 4 + TC + 4
    qkvP = kb.dram_in("qkvP", [128, 3, PADW])
    gateT = kb.dram_in("gateT", [128, TT])
    baT = kb.dram_in("baT", [4, TT])
    cwT = kb.dram_in("cwT", [128, 3, 5])
    cst_d = kb.dram_in("cst", [128, 8])
    cm_d = kb.dram_in("cm", [6, 128, 128])
    yT = kb.dram_out("yT", [128, TT])
    oS = [kb.dram_tmp("oF", [128, TT]), kb.dram_tmp("oB", [128, TT])]
    oSv = [[oS[d].view((slice(None), slice(n * 128, (n + 1) * 128))) for n in range(NB)] for d in range(2)]

    cmt = kb.sb([128, 6, 128], name="cm")
    kb.dma("sp", cmt[:], cm_d[:].rearrange("c p q -> p c q"), writes=[cmt])
    ident_ap = cmt[:, 0, :]
    LO = cmt[:, 1, :]; UP = cmt[:, 2, :]; BLK = cmt[:, 3, :]
    HALF = [cmt[:, 4, :], cmt[:, 5, :]]
    ones = kb.sb([128, 128], name="ones")
    kb.op("dve", lambda e: e.memset(ones[:], 1.0), writes=[ones])
    eps_t = kb.sb([128, 1], name="eps")
    kb.op("dve", lambda e: e.memset(eps_t[:], 1e-6), writes=[eps_t])
    one_c = kb.sb([128, 1], name="onec")
    kb.op("dve", lambda e: e.memset(one_c[:], 1.0), writes=[one_c])
    msk = kb.sb([128, 3, 128], name="msk")
    kb.ts(msk, msk[:, 0, :], cmt, LO, -1e4, ALU.mult, 1e4, ALU.add)
    kb.ts(msk, msk[:, 1, :], cmt, UP, -1e4, ALU.mult, 1e4, ALU.add)
    kb.ts(msk, msk[:, 2, :], cmt, ident_ap, -1.0, ALU.mult, 1.0, ALU.add)
    MLO = msk[:, 0, :]; MUP = msk[:, 1, :]; NOTI = msk[:, 2, :]
    cw = kb.sb([128, 3, 5], name="cw")
    kb.dma("sp", cw[:], cwT[:], writes=[cw])
    cst = kb.sb([128, 8], name="cst")
    kb.dma("sp", cst[:], cst_d[:], writes=[cst])
    nega = kb.sb([128, 2], name="nega")
    kb.act(nega, nega[:], cst, cst[:, 0:2], AF.Exp)
    kb.ts(nega, nega[:], nega, nega[:], -1.0, ALU.mult)

    banks = [kb.ps([128, 512], name="bank") for _ in range(8)]

    def v(b, c0, w):
        return banks[b].sub((slice(None), slice(c0, c0 + w)))

    TS = kb.sb([128, NB, 4], name="TS")
    bat = [kb.sb([4, 2048], name="bat") for _ in range(2)]
    pts = [v(0, 0, 512), v(1, 0, 512)]
    for pi, c0 in enumerate(range(0, TT, 2048)):
        w = min(2048, TT - c0)
        bt = bat[pi % 2]
        kb.dma("sp", bt[:, :w], baT[:, c0:c0 + w], writes=[bt])
        pt = pts[pi % 2]
        nb = w // 128
        for i in range(nb):
            kb.tr(pt, pt[:, i * 4:(i + 1) * 4], bt, bt[:, i * 128:(i + 1) * 128], cmt, cmt[0:4, 0, 0:4])
        n0 = c0 // 128
        kb.cp(TS, TS[:, n0:n0 + nb, :], pt, pt[:, :nb * 4].rearrange("p (a b) -> p a b", b=4), eng="dve")
    if stage == 0:
        o = kb.dram_out("dTS", [128, NB * 4])
        kb.dma("sp", o[:], TS[:].rearrange("p a b -> p (a b)"), reads=[TS], writes=[])
        o = kb.dram_out("dmsk", [128, 3 * 128])
        kb.dma("sp", o[:], msk[:].rearrange("p a b -> p (a b)"), reads=[msk], writes=[])
        o = kb.dram_out("dnega", [128, 2])
        kb.dma("sp", o[:], nega[:], reads=[nega], writes=[])
        return kb
    BETA = kb.sb([128, NB, 2], name="BETA")
    kb.act(BETA, BETA[:], TS, TS[:, :, 0:2], AF.Sigmoid)
    G = kb.sb([128, NB, 2], name="G")
    for d in range(2):
        kb.act(G, G[:, :, d], TS, TS[:, :, 2 + d], AF.Exp, extra_reads=[cst], bias=cst[:, 2 + d:3 + d], scale=1.0)
        kb.act(G, G[:, :, d], G, G[:, :, d], AF.Ln, extra_reads=[one_c], bias=one_c[:, 0:1], scale=1.0)
        kb.ts(G, G[:, :, d], G, G[:, :, d], nega[:, d:d + 1], ALU.mult, sr=[nega])
    GC = kb.sb([128, NB, 2], name="GC")
    GT = kb.sb([128, NB, 2], name="GT")
    EGL = kb.sb([128, NB, 2, 2], name="EGL")
    pa = v(2, 0, 512)
    for d in range(2):
        kb.mm(pa, pa[:, 0:NB], cmt, (UP if d == 0 else LO), G, G[:, :, d])
        kb.cp(GC, GC[:, :, d], pa, pa[:, 0:NB], eng="dve")
        kb.mm(pa, pa[:, 0:NB], cmt, BLK, G, G[:, :, d])
        kb.cp(GT, GT[:, :, d], pa, pa[:, 0:NB], eng="dve")
        for h in range(2):
            kb.mm(pa, pa[:, 0:NB], cmt, HALF[h], G, G[:, :, d])
            kb.act(EGL, EGL[:, :, d, h], pa, pa[:, 0:NB], AF.Exp)
    EGC = kb.sb([128, NB, 2], name="EGC")
    kb.act(EGC, EGC[:], GC, GC[:], AF.Exp)
    EKL = kb.sb([128, NB, 2], name="EKL")
    kb.tt(EKL, EKL[:], GT, GT[:], GC, GC[:], ALU.subtract)
    kb.act(EKL, EKL[:], EKL, EKL[:], AF.Exp)
    BEG = kb.sb([128, NB, 2], name="BEG")
    kb.tt(BEG, BEG[:], BETA, BETA[:], EGC, EGC[:], ALU.mult)
    NBETA = kb.sb([128, NB, 2], name="NBETA")
    kb.ts(NBETA, NBETA[:], BETA, BETA[:], -1.0, ALU.mult)
    EKLH = kb.sb([128, NB, 2, 2], name="EKLH")
    kb.op("dve", lambda e: e.memset(EKLH[:], 0.0), writes=[EKLH])
    for h in range(2):
        kb.cp(EKLH, EKLH[h * 64:(h + 1) * 64, :, :, h], EKL, EKL[h * 64:(h + 1) * 64, :, :], eng="dve")

    if stage == 1:
        for nm, t, sh in (("dGC", GC, [128, NB * 2]), ("dBETA", BETA, [128, NB * 2]), ("dEGL", EGL, [128, NB * 4]),
                          ("dEKLH", EKLH, [128, NB * 4]), ("dG", G, [128, NB * 2])):
            o = kb.dram_out(nm, sh)
            kb.dma("sp", o[:], t[:].rearrange("p a b -> p (a b)") if len(sh) == 2 and nm in ("dGC", "dBETA", "dG") else t[:].rearrange("p a b c -> p (a b c)"), reads=[t], writes=[])
        return kb
    kb.barrier()
    def mk(name, shape=(128, 128)):
        return [kb.sb(list(shape), name=name + str(d)) for d in range(2)]

    XIN = mk("xin", (128, 3, 132)); ACC = mk("acc", (128, 3, 128)); Y = mk("y", (128, 3, 128))
    SQ = mk("sq", (128, 2, 128)); RN = mk("rn", (128, 2, 128)); QN = mk("qn"); KN = mk("kn")
    VB = mk("vb"); KBG = mk("kbg"); KEL = mk("kel", (128, 2, 128)); DG = mk("dg"); T0 = mk("t0")
    X1 = mk("x1"); X2 = mk("x2"); DM = mk("dm"); DMT = mk("dmt"); EBC = mk("ebc"); QG = mk("qg"); QKM = mk("qkm")
    AD = mk("ad"); MTa = [mk("mta"), mk("mtb")]; Ma = [mk("ma"), mk("mb")]
    Ra = [mk("ra"), mk("rb")]
    VN = mk("vn"); OSB = mk("osb"); OTS = mk("ots")
    U2 = [mk("u_a"), mk("u_b")]; WT2 = [mk("wt_a"), mk("wt_b")]; QG2 = [mk("qg_a"), mk("qg_b")]
    QKM2 = [mk("qkm_a"), mk("qkm_b")]; KEL2 = [mk("kel_a", (128, 2, 128)), mk("kel_b", (128, 2, 128))]
    S = [mk("s0"), mk("s1")]
    for d in range(2):
        kb.op("dve", lambda e, d=d: e.memset(VN[d][:], 0.0), writes=[VN[d]])
        kb.op("dve", lambda e, d=d: e.memset(S[0][d][:], 0.0), writes=[S[0][d]])
    spar = [0, 0]
    P_ss = [v(4 * d + 0, 0, 256) for d in range(2)]
    P_g = [v(4 * d + 0, 256, 128) for d in range(2)]
    P_uw = [v(4 * d + 0, 384, 128) for d in range(2)]
    P_tr = [v(4 * d + 1, 0, 256) for d in range(2)]
    P_kk = [v(4 * d + 1, 256, 256) for d in range(2)]
    P_mm = [v(4 * d + 2, 0, 256) for d in range(2)]
    P_m0 = [v(4 * d + 2, 256, 128) for d in range(2)]
    P_r = [v(4 * d + 2, 384, 128) for d in range(2)]
    P_p1 = [v(4 * d + 3, 0, 128) for d in range(2)]
    P_o = [v(4 * d + 3, 128, 128) for d in range(2)]
    P_s = [v(4 * d + 3, 256, 128) for d in range(2)]
    P_ot = [v(4 * d + 3, 384, 128) for d in range(2)]

    def prep(n, d, pp=0):
        U = U2[pp]; WT = WT2[pp]; QG = QG2[pp]; QKM = QKM2[pp]; KEL = KEL2[pp]
        c0 = n * 128 if n < NBL else TL + 4 + (n - NBL) * 128
        xin = XIN[d]; acc = ACC[d]; y = Y[d]
        kb.dma("sp", xin[:], qkvP[:, :, c0:c0 + 132], writes=[xin])
        for w in range(3):
            kb.ts(acc, acc[:, w, :], xin, xin[:, w, 0:128], cw[:, w, 0:1], ALU.mult, sr=[cw])
            for jj in range(1, 5):
                kb.stt(acc, acc[:, w, :], xin, xin[:, w, jj:jj + 128], cw[:, w, jj:jj + 1], acc, acc[:, w, :],
                       ALU.mult, ALU.add, sr=[cw])
        kb.act(y, y[:], acc, acc[:], AF.Silu)
        if stage == 21: return
        sq = SQ[d]; rn = RN[d]; pss = P_ss[d]
        kb.act(sq, sq[:], y, y[:, 0:2, :], AF.Square)
        kb.mm(pss, pss[:], ones, ones[:], sq, sq[:].rearrange("p a b -> p (a b)"))
        kb.act(rn, rn[:].rearrange("p a b -> p (a b)"), pss, pss[:], AF.Sqrt, extra_reads=[eps_t], bias=eps_t[:, 0:1], scale=1.0)
        kb.op("dve", lambda e: e.reciprocal(out=rn[:], in_=rn[:]), reads=[rn], writes=[rn])
        qn = QN[d]; kn = KN[d]
        kb.stt(qn, qn[:], y, y[:, 0, :], float(HD) ** -0.5, rn, rn[:, 0, :], ALU.mult, ALU.mult)
        kb.tt(kn, kn[:], y, y[:, 1, :], rn, rn[:, 1, :], ALU.mult)
        if stage == 22: return
        ptr = P_tr[d]
        kb.tr(ptr, ptr[:, 0:128], kn, kn[:], cmt, ident_ap)
        kb.tr(ptr, ptr[:, 128:256], y, y[:, 2, :], cmt, ident_ap)
        kb.ts(KBG[d], KBG[d][:], ptr, ptr[:, 0:128], BEG[:, n, d:d + 1], ALU.mult, sr=[BEG])
        for h in range(2):
            kb.ts(KEL[d], KEL[d][:, h, :], ptr, ptr[:, 0:128], EKLH[:, n, d, h:h + 1], ALU.mult, sr=[EKLH])
        kb.ts(VB[d], VB[d][:], ptr, ptr[:, 128:256], BETA[:, n, d:d + 1], ALU.mult, sr=[BETA])
        if stage == 23: return
        pkk = P_kk[d]
        kb.mm(pkk, pkk[:, 0:128], kn, kn[:], kn, kn[:])
        kb.mm(pkk, pkk[:, 128:256], kn, kn[:], qn, qn[:])
        kb.ts(DG[d], DG[d][:], cmt, ident_ap, GC[:, n, d:d + 1], ALU.mult, sr=[GC])
        pg = P_g[d]
        kb.mm(pg, pg[:], ones, ones[:], DG[d], DG[d][:])
        kb.ts(T0[d], T0[d][:], pg, pg[:], GC[:, n, d:d + 1], ALU.subtract, sr=[GC])
        kb.act(EBC[d], EBC[d][:], pg, pg[:], AF.Exp)
        if stage == 24: return
        M1 = MLO if d == 0 else MUP
        M2 = MUP if d == 0 else MLO
        kb.tt(X1[d], X1[d][:], T0[d], T0[d][:], msk, M1, ALU.add)
        kb.tt(X2[d], X2[d][:], T0[d], T0[d][:], msk, M2, ALU.subtract)
        kb.act(DM[d], DM[d][:], X1[d], X1[d][:], AF.Exp, scale=-1.0)
        kb.act(DMT[d], DMT[d][:], X2[d], X2[d][:], AF.Exp)
        kb.tt(QG[d], QG[d][:], qn, qn[:], EBC[d], EBC[d][:], ALU.mult)
        kb.tt(QKM[d], QKM[d][:], pkk, pkk[:, 128:256], DMT[d], DMT[d][:], ALU.mult)
        kb.tt(AD[d], AD[d][:], pkk, pkk[:, 0:128], DM[d], DM[d][:], ALU.mult)
        if stage == 25: return
        mt = MTa[0][d]
        kb.stt(mt, mt[:], AD[d], AD[d][:], NBETA[:, n, d:d + 1], msk, NOTI, ALU.mult, ALU.mult, sr=[NBETA])
        if stage == 261: return
        pm0 = P_m0[d]
        kb.tr(pm0, pm0[:], mt, mt[:], cmt, ident_ap)
        if stage == 262: return
        m = Ma[0][d]
        kb.cp(m, m[:], pm0, pm0[:])
        if stage == 263: return
        r = Ra[0][d]
        kb.tt(r, r[:], m, m[:], cmt, ident_ap, ALU.add)
        if stage == 26: return
        pmm = P_mm[d]; pr = P_r[d]
        for k in range(1, 6):
            mtp = MTa[(k - 1) % 2][d]; mp = Ma[(k - 1) % 2][d]
            mtn = MTa[k % 2][d]; mn = Ma[k % 2][d]
            kb.mm(pmm, pmm[:, 0:128], mp, mp[:], mtp, mtp[:])
            kb.cp(mtn, mtn[:], pmm, pmm[:, 0:128])
            if k < 5:
                kb.mm(pmm, pmm[:, 128:256], mtp, mtp[:], mp, mp[:])
                kb.cp(mn, mn[:], pmm, pmm[:, 128:256], eng="dve")
            rp = Ra[(k - 1) % 2][d]; rn_ = Ra[k % 2][d]
            kb.mm(pr, pr[:], mtn, mtn[:], rp, rp[:])
            kb.tt(rn_, rn_[:], pr, pr[:], rp, rp[:], ALU.add)
        if stage == 27: return
        R = Ra[5 % 2][d]
        puw = P_uw[d]
        kb.mm(puw, puw[:], R, R[:], VB[d], VB[d][:])
        kb.cp(U[d], U[d][:], puw, puw[:])
        kb.mm(puw, puw[:], KBG[d], KBG[d][:], R, R[:])
        kb.cp(WT[d], WT[d][:], puw, puw[:])

    def scan(n, d, h, pp=0):
        U = U2[pp]; WT = WT2[pp]; QG = QG2[pp]; QKM = QKM2[pp]; KEL = KEL2[pp]
        rows = slice(h * 64, (h + 1) * 64)
        Sc = S[spar[d]][d]; Sn = S[1 - spar[d]][d]
        p1 = P_p1[d]; po = P_o[d]; ps_ = P_s[d]
        kb.mm(p1, p1[:], WT[d], WT[d][:], Sc, Sc[:])
        if stage == 291: return
        kb.tt(VN[d], VN[d][rows, :], U[d], U[d][rows, :], p1, p1[rows, :], ALU.subtract)
        if stage == 292: return
        kb.mm(po, po[:], QG[d], QG[d][:], Sc, Sc[:], start=True, stop=False)
        kb.mm(po, po[:], QKM[d], QKM[d][:], VN[d], VN[d][:], start=False, stop=True)
        if stage == 293: return
        kb.cp(OSB[d], OSB[d][rows, :], po, po[rows, :])
        if stage == 294: return
        kb.mm(ps_, ps_[:], KEL[d], KEL[d][:, h, :], VN[d], VN[d][:])
        if stage == 295: return
        kb.stt(Sn, Sn[:], Sc, Sc[:], EGL[:, n, d, h:h + 1], ps_, ps_[:], ALU.mult, ALU.add, sr=[EGL])
        spar[d] = 1 - spar[d]

    def fin(n, d):
        pot = P_ot[d]
        kb.tr(pot, pot[:], OSB[d], OSB[d][:], cmt, ident_ap)
        kb.cp(OTS[d], OTS[d][:], pot, pot[:], eng="dve")
        kb.dma("sp", oS[d][:, n * 128:(n + 1) * 128], OTS[d][:], reads=[OTS[d]], writes=[oSv[d][n]])

    NBC = NB - NBL
    ordF = list(range(NBL, NB)) + list(range(NBL))
    ordB = list(range(NB - 1, NBL - 1, -1)) + list(range(NBL - 1, -1, -1))
    if 20 < stage < 30 or 260 < stage < 300:
        prep(ordF[0], 0)
        if stage == 29 or stage > 290:
            scan(ordF[0], 0, 0)
    if stage == 9:
        kb.rec = []
        prep(ordF[0], 0, 0)
        PF0 = kb.rec
        kb.rec = []
        prep(ordB[0], 1, 0)
        PB0 = kb.rec
        kb.replay(PF0, PB0)
    for t in range(NB if stage != 2 else 1):
        if 20 < stage < 30 or 260 < stage < 300: break
        nf, nb_ = ordF[t], ordB[t]
        pp = t % 2
        kb.rec = []
        scan(nf, 0, 0, pp); scan(nf, 0, 1, pp); fin(nf, 0)
        SF = kb.rec
        kb.rec = []
        scan(nb_, 1, 1, pp); scan(nb_, 1, 0, pp); fin(nb_, 1)
        SB = kb.rec
        PF = []; PB = []
        if t + 1 < NB:
            kb.rec = []
            prep(ordF[t + 1], 0, 1 - pp)
            PF = kb.rec
            kb.rec = []
            prep(ordB[t + 1], 1, 1 - pp)
            PB = kb.rec
        kb.replay(SF, SB, PF, PB)
    if stage in (2, 3) or 20 < stage < 30 or 260 < stage < 300:
        for nm, t in (("dY", Y[0]), ("dQN", QN[0]), ("dKN", KN[0]), ("dU", U2[0][0]), ("dWT", WT2[0][0]), ("dOSB", OSB[0]),
                      ("dR", Ra[1][0]), ("dMT0", MTa[0][0]), ("dOSB1", OSB[1]), ("dS0", S[spar[0]][0])):
            sh = [128, 384] if nm == "dY" else [128, 128]
            o = kb.dram_out(nm, sh)
            kb.dma("sp", o[:], t[:].rearrange("p a b -> p (a b)") if nm == "dY" else t[:], reads=[t], writes=[])
        return kb
    kb.barrier()
    oa = [kb.sb([128, 512], name="oa") for _ in range(2)]
    ob = [kb.sb([128, 512], name="ob") for _ in range(2)]
    gt = [kb.sb([128, 512], name="gt") for _ in range(2)]
    osum = kb.sb([128, 512], name="osum"); sq2 = kb.sb([128, 512], name="sq2"); rs = kb.sb([128, 512], name="rs")
    yo = [kb.sb([128, 512], name="yo") for _ in range(2)]
    pf = v(0, 0, 512)
    for ti, c0 in enumerate(range(0, TT, 512)):
        w = min(512, TT - c0)
        a = oa[ti % 2]; b = ob[ti % 2]; g = gt[ti % 2]; yy = yo[ti % 2]
        blks = range(c0 // 128, (c0 + w) // 128)
        kb.dma("sp", a[:, :w], oS[0][:, c0:c0 + w], reads=[oSv[0][n] for n in blks], writes=[a])
        kb.dma("sp", b[:, :w], oS[1][:, c0:c0 + w], reads=[oSv[1][n] for n in blks], writes=[b])
        kb.dma("sp", g[:, :w], gateT[:, c0:c0 + w], writes=[g])
        kb.tt(osum, osum[:, :w], a, a[:, :w], b, b[:, :w], ALU.add)
        kb.act(sq2, sq2[:, :w], osum, osum[:, :w], AF.Square)
        kb.mm(pf, pf[:, :w], ones, ones[:], sq2, sq2[:, :w])
        kb.act(rs, rs[:, :w], pf, pf[:, :w], AF.Sqrt, extra_reads=[eps_t], bias=eps_t[:, 0:1], scale=1.0 / HD)
        kb.op("dve", lambda e: e.reciprocal(out=rs[:, :w], in_=rs[:, :w]), reads=[rs], writes=[rs])
        kb.act(g, g[:, :w], g, g[:, :w], AF.Silu)
        kb.stt(osum, osum[:, :w], osum, osum[:, :w], cst[:, 4:5], rs, rs[:, :w], ALU.mult, ALU.mult, sr=[cst])
        kb.tt(yy, yy[:, :w], osum, osum[:, :w], g, g[:, :w], ALU.mult)
        kb.dma("sp", yT[:, c0:c0 + w], yy[:, :w], reads=[yy], writes=[])
    return kb


NEXP = 16384
NCH = NEXP // 128


def peer_consts():
    bm = np.zeros((128, 8, 16), np.float32)
    for p in range(128):
        bm[p, p // 16, :] = 1.0
    return bm.reshape(128, 128)


def build_C(NLAT=4096, NCTX=64, TW=256, dbg=False):
    kb = KB()
    NT = NLAT + NCTX
    xT = kb.dram_in("xT", [D, NT])
    mixT = kb.dram_in("mixT", [D, NT])
    cT = kb.dram_in("cT", [128, KC, 2])
    ada_w = kb.dram_in("ada_w", [D, 4 * D])
    ada_bT = kb.dram_in("ada_bT", [128, 48])
    n2T = kb.dram_in("n2T", [128, KC])
    w_out = kb.dram_in("w_out", [D, D])
    wq = kb.dram_in("wq", [D, 2048])
    subkT = kb.dram_in("subkT", [16, 128, 128])
    uTr = kb.dram_in("uTr", [NCH, 128, KC, 128])
    vtab = kb.dram_in("vtab", [NEXP, D])
    bm_d = kb.dram_in("bm", [128, 128])
    id_d = kb.dram_in("ident", [128, 128])
    xoT = kb.dram_out("xoT", [D, NT])
    if dbg:
        d_h2 = kb.dram_out("d_h2", [D, NT])
        d_peer = kb.dram_out("d_peer", [D, NT])
        d_x1 = kb.dram_out("d_x1", [D, NT])

    ones = kb.sb([128, 128], name="ones")
    kb.op("dve", lambda e: e.memset(ones[:], 1.0), writes=[ones])
    eps_t = kb.sb([128, 1], name="eps")
    kb.op("dve", lambda e: e.memset(eps_t[:], 1e-6), writes=[eps_t])
    iot = kb.sb([128, 128], name="iota")
    kb.op("pool", lambda e: e.iota(iot[:], pattern=[[1, 128]], base=0, channel_multiplier=0,
                                   allow_small_or_imprecise_dtypes=True), writes=[iot])
    iot_bf = kb.sb([128, 128], BF16, name="iota_bf")
    kb.cp(iot_bf, iot_bf[:], iot, iot[:], eng="pool")
    bm = kb.sb([128, 128], name="bm")
    kb.dma("sp", bm[:], bm_d[:], writes=[bm])
    ident = kb.sb([128, 128], name="ident")
    kb.dma("sp", ident[:], id_d[:], writes=[ident])
    ident_bf = kb.sb([128, 128], BF16, name="ident_bf")
    kb.cp(ident_bf, ident_bf[:], ident, ident[:], eng="dve")
    subk = kb.sb([128, 16, 128], name="subk")
    kb.dma("sp", subk[:], subkT[:].rearrange("c p q -> p c q"), writes=[subk])
    wo_bf = kb.sb([128, KC, D], BF16, name="wo_bf")
    wov = w_out[:].rearrange("(k p) n -> p k n", p=128)
    for k in range(KC):
        kb.dma("pool", wo_bf[:, k, :], wov[:, k, :], writes=[wo_bf])

    P = [kb.ps([128, 512], name="P") for _ in range(7)]
    P7b = kb.ps([128, 1024], BF16, name="P7b")
    WQW = 256
    wqs = [kb.sb([128, KC, WQW], name="wqs") for _ in range(2)]
    uB = kb.dram_tmp("uB", [NCH, 128, KC, 128], BF16)
    vB = kb.dram_tmp("vB", [NEXP, D], BF16)
    uBv = [uB.view((c,)) for c in range(NCH)]
    vBv = [vB.view((slice(c * 128, (c + 1) * 128),)) for c in range(NCH)]
    mod = emit_mod(kb, cT, ada_w, ada_bT, 16, 48, wt=wqs, bw=WQW, modp=P[0], col_base=2 * D)
    for c in range(NCH):
        kb.dma("pool", uB[c], uTr[c], writes=[uBv[c]])
        kb.dma("pool", vB[c * 128:(c + 1) * 128, :], vtab[c * 128:(c + 1) * 128, :], writes=[vBv[c]])
    n2 = kb.sb([128, KC], name="n2")
    kb.dma("sp", n2[:], n2T[:], writes=[n2])
    a_sc = kb.sb([128, KC, 2], name="a_sc")
    for s in range(2):
        kb.stt(a_sc, a_sc[:, :, s], mod, mod[:, 16:24, s], 1.0, n2, n2[:], ALU.add, ALU.mult)

    wqv = wq[:].rearrange("(k p) n -> p k n", p=128)
    xt = kb.sb([128, KC, TW], name="xt")
    mx_bf = kb.sb([128, KC, TW], BF16, name="mx_bf")
    h2 = kb.sb([128, KC, TW], name="h2")
    h2_bf = kb.sb([128, KC, TW], BF16, name="h2_bf")
    sqs = [kb.sb([128, TW], name="sq") for _ in range(2)]
    tmps = [kb.sb([128, TW], name="tmp") for _ in range(2)]
    rstd = kb.sb([128, TW], name="rstd")
    qps = [kb.sb([128, 2, 128], name="qps") for _ in range(2)]
    s_sbs = [kb.sb([128, 16, 128], name="s_sb") for _ in range(2)]
    cand = kb.sb([128, 16, 8, 16], name="cand")
    mtmp = kb.sb([128, 128], name="mtmp")
    ctmp = kb.sb([128, 16, 16], name="ctmp")
    sv = kb.sb([128, 8, 2, 16], name="sv")
    si = kb.sb([128, 8, 2, 16], U32, name="si")
    sif = kb.sb([128, 2, 8, 16], name="sif")
    c16 = kb.sb([128, 8, 16], name="c16")
    nmx = kb.sb([128, 8], name="nmx")
    zz = kb.sb([128, 8], name="zz")
    w_bf = kb.sb([128, 16, 8, 16], BF16, name="w_bf")
    WT = kb.sb([128, 16, TW], BF16, name="WT")
    SIT = kb.sb([128, 2, TW], name="SIT")
    GT = kb.sb([128, TW, NCH], BF16, name="GT")
    oh1q = [kb.sb([128, 4, 128], BF16, name="oh1q") for _ in range(3)]
    oh2q = [kb.sb([128, 4, 128], BF16, name="oh2q") for _ in range(3)]
    wbq = [kb.sb([128, 4, 128], BF16, name="wbq") for _ in range(3)]
    XS = [kb.sb([128, 4, 128], BF16, name="XS") for _ in range(3)]
    NUB = 2
    dbgp = kb.sb([128, TW], name="dbgp") if dbg else None
    u_bf = [kb.sb([128, KC, 128], BF16, name="u_bf") for _ in range(NUB)]
    v_bf = [kb.sb([128, D], BF16, name="v_bf") for _ in range(NUB)]
    gl = [kb.sb([128, TW], name="gl") for _ in range(2)]
    ga = [kb.sb([128, TW], BF16, name="ga") for _ in range(2)]
    ost = [kb.sb([128, TW], name="ost") for _ in range(2)]
    xv = xT[:].rearrange("(k p) n -> p k n", p=128)
    mv = mixT[:].rearrange("(k p) n -> p k n", p=128)

    tiles = [(t0, TW, 0) for t0 in range(0, NLAT, TW)]
    if NCTX:
        tiles.append((NLAT, NCTX, 1))
    nld = 0
    for ti, (t0, ncol, s) in enumerate(tiles):
        for k in range(KC):
            kb.dma("sp", xt[:, k, :ncol], xv[:, k, t0:t0 + ncol], writes=[xt])
            kb.dma("pool", mx_bf[:, k, :ncol], mv[:, k, t0:t0 + ncol], writes=[mx_bf])
        for m in range(KC):
            pp = P[4 + m % 2]
            for k in range(KC):
                kb.mm(pp, pp[:, :ncol], wo_bf, wo_bf[:, k, m * 128:(m + 1) * 128], mx_bf, mx_bf[:, k, :ncol],
                      start=(k == 0), stop=(k == KC - 1))
            kb.stt(xt, xt[:, m, :ncol], pp, pp[:, :ncol], mod[:, m, s:s + 1], xt, xt[:, m, :ncol], ALU.mult, ALU.add, sr=[mod])
        if dbg:
            for k in range(KC):
                kb.dma("sp", d_x1[k * 128:(k + 1) * 128, t0:t0 + ncol], xt[:, k, :ncol], reads=[xt], writes=[])
        ssp = P[6]
        for k in range(KC):
            sq = sqs[k % 2]
            kb.act(sq, sq[:, :ncol], xt, xt[:, k, :ncol], AF.Square)
            kb.mm(ssp, ssp[:, :ncol], ones, ones[:], sq, sq[:, :ncol], start=(k == 0), stop=(k == KC - 1))
        kb.act(rstd, rstd[:, :ncol], ssp, ssp[:, :ncol], AF.Sqrt, extra_reads=[eps_t], bias=eps_t[:, 0:1], scale=1.0 / D)
        kb.op("dve", lambda e: e.reciprocal(out=rstd[:, :ncol], in_=rstd[:, :ncol]), reads=[rstd], writes=[rstd])
        for k in range(KC):
            tmp = tmps[k % 2]
            kb.stt(tmp, tmp[:, :ncol], xt, xt[:, k, :ncol], a_sc[:, k, s:s + 1], rstd, rstd[:, :ncol], ALU.mult, ALU.mult, sr=[a_sc])
            kb.act(h2, h2[:, k, :ncol], tmp, tmp[:, :ncol], AF.Identity, extra_reads=[mod], bias=mod[:, 8 + k, s:s + 1], scale=1.0)
        kb.cp(h2_bf, h2_bf[:, :, :ncol], h2, h2[:, :, :ncol], eng="pool")
        if dbg:
            for k in range(KC):
                kb.dma("sp", d_h2[k * 128:(k + 1) * 128, t0:t0 + ncol], h2[:, k, :ncol], reads=[h2], writes=[])
        for sb0 in range(0, ncol, 128):
            nt = min(128, ncol - sb0)
            s_sb = s_sbs[(sb0 // 128) % 2]
            for hb in range(0, 16, 2):
                wt_ = wqs[nld % 2]; nld += 1
                kb.dma("sp", wt_[:], wqv[:, :, hb * 128:hb * 128 + WQW], writes=[wt_])
                qpp = P[4 + (hb // 2) % 2]
                for j in range(2):
                    for k in range(KC):
                        kb.mm(qpp, qpp[:, j * 128:j * 128 + nt], wt_, wt_[:, k, j * 128:(j + 1) * 128],
                              h2, h2[:, k, sb0:sb0 + nt], start=(k == 0), stop=(k == KC - 1))
                qp = qps[(hb // 2) % 2]
                kb.cp(qp, qp[:, :, :nt], qpp, qpp[:, 0:256].rearrange("p (a b) -> p a b", b=128)[:, :, :nt])
                for j in range(2):
                    hp = hb + j
                    sp_ = P[hp // 4]
                    kb.mm(sp_, sp_[:nt, (hp % 4) * 128:(hp % 4 + 1) * 128], qp, qp[:, j, :nt], subk, subk[:, hp, :])
            for bnk in range(4):
                kb.cp(s_sb, s_sb[:nt, bnk * 4:(bnk + 1) * 4, :], P[bnk], P[bnk][:nt, :].rearrange("p (a b) -> p a b", b=128),
                      eng=("act" if bnk % 2 == 0 else "dve"))
        for sb0 in range(0, ncol, 128):
            nt = min(128, ncol - sb0)
            s_sb = s_sbs[(sb0 // 128) % 2]
            for hp in range(16):
                h, p_ = hp // 2, hp % 2
                sv8a = sv[:nt, h, p_, 0:8]; sv8b = sv[:nt, h, p_, 8:16]
                kb.op("dve", lambda e, hp=hp, o=sv8a: e.max(out=o, in_=s_sb[:nt, hp, :]), reads=[s_sb], writes=[sv])
                kb.op("dve", lambda e, hp=hp, o=sv8a, h=h, p_=p_: e.max_index(out=si[:nt, h, p_, 0:8], in_max=o, in_values=s_sb[:nt, hp, :]),
                      reads=[s_sb, sv], writes=[si])
                kb.op("dve", lambda e, hp=hp, o=sv8a: e.match_replace(out=mtmp[:nt, :], in_to_replace=o, in_values=s_sb[:nt, hp, :], imm_value=-1e30),
                      reads=[s_sb, sv], writes=[mtmp])
                kb.op("dve", lambda e, o=sv8b: e.max(out=o, in_=mtmp[:nt, :]), reads=[mtmp], writes=[sv])
                kb.op("dve", lambda e, o=sv8b, h=h, p_=p_: e.max_index(out=si[:nt, h, p_, 8:16], in_max=o, in_values=mtmp[:nt, :]),
                      reads=[mtmp, sv], writes=[si])
            sva = sv[:nt, :, 0, :]
            svb = sv[:nt, :, 1, :]
            in0 = bass.AP(sva.tensor, sva.offset, [list(sva.ap[0]), [1, 16], [32, 8], [0, 16]])
            in1 = bass.AP(svb.tensor, svb.offset, [list(svb.ap[0]), [0, 16], [32, 8], [1, 16]])
            kb.op("dve", lambda e: e.tensor_tensor(out=cand[:nt], in0=in0, in1=in1, op=ALU.add), reads=[sv], writes=[cand])
            for h in range(8):
                ch = cand[:nt, :, h, :]
                kb.op("dve", lambda e, h=h, ch=ch: e.max(out=c16[:nt, h, 0:8], in_=ch), reads=[cand], writes=[c16])
                kb.op("dve", lambda e, h=h, ch=ch: e.match_replace(out=ctmp[:nt], in_to_replace=c16[:nt, h, 0:8], in_values=ch, imm_value=-1e30),
                      reads=[cand, c16], writes=[ctmp])
                kb.op("dve", lambda e, h=h: e.max(out=c16[:nt, h, 8:16], in_=ctmp[:nt]), reads=[ctmp], writes=[c16])
            kb.ts(nmx, nmx[:nt, :], c16, c16[:nt, :, 0], -1.0, ALU.mult)
            E = s_sb
            Ev = E[:nt].rearrange("p a b -> p (a b)").rearrange("p (a h b) -> p a h b", a=16, h=8)
            for h in range(8):
                kb.act(E, Ev[:, :, h, :], cand, cand[:nt, :, h, :], AF.Exp, extra_reads=[nmx], bias=nmx[:nt, h:h + 1], scale=1.0)
            for h in range(8):
                kb.stt(E, Ev[:, :, h, :], cand, cand[:nt, :, h, :], c16[:nt, h, 15:16], E, Ev[:, :, h, :], ALU.is_ge, ALU.mult, sr=[c16])
            Eperm = bass.AP(Ev.tensor, Ev.offset, [list(Ev.ap[0]), [16, 8], [128, 16], [1, 16]])
            kb.op("dve", lambda e: e.tensor_reduce(out=zz[:nt, :], in_=Eperm, axis=AX.XY, op=ALU.add), reads=[E], writes=[zz])
            kb.op("dve", lambda e: e.reciprocal(out=zz[:nt, :], in_=zz[:nt, :]), reads=[zz], writes=[zz])
            zb = zz[:nt, :]
            zbc = bass.AP(zb.tensor, zb.offset, [list(zb.ap[0]), [0, 16], [1, 8], [0, 16]])
            kb.op("dve", lambda e: e.tensor_tensor(out=w_bf[:nt], in0=Ev, in1=zbc, op=ALU.mult), reads=[E, zz], writes=[w_bf])
            for p_ in range(2):
                kb.cp(sif, sif[:nt, p_, :, :], si, si[:nt, :, p_, :], eng="dve")
            for p_ in range(2):
                tp = P[6]
                kb.tr(tp, tp[:, 0:nt], sif, sif[:nt, p_, :, :].rearrange("p a b -> p (a b)"), ident, ident[:nt, :nt])
                kb.cp(SIT, SIT[:, p_, sb0:sb0 + nt], tp, tp[:, 0:nt], eng="dve")
            for a0 in range(0, 16, 4):
                for aa in range(4):
                    a = a0 + aa
                    kb.tr(P7b, P7b[:, aa * 128:aa * 128 + nt], w_bf, w_bf[:nt, a, :, :].rearrange("p a b -> p (a b)"),
                          ident_bf, ident_bf[:nt, :nt])
                kb.cp(WT, WT[:, a0:a0 + 4, sb0:sb0 + nt], P7b, P7b[:, 0:512].rearrange("p (a b) -> p a b", b=128)[:, :, :nt])
        for tq in range(0, ncol, 4):
            g4 = (tq // 4) % 3
            XP = P[g4]; GP = P[3 + g4]
            xs = XS[g4]
            o2 = oh2q[g4]; o1 = oh1q[g4]; w_ = wbq[g4]
            for tt_ in range(4):
                t = tq + tt_
                kb.ts(o2, o2[:, tt_, :], iot_bf, iot_bf[:], SIT[:, 1, t:t + 1], ALU.is_equal, sr=[SIT])
                kb.ts(o1, o1[:, tt_, :], iot_bf, iot_bf[:], SIT[:, 0, t:t + 1], ALU.is_equal, sr=[SIT])
            for tt_ in range(4):
                wc = WT[:, :, tq + tt_]
                wbc = bass.AP(wc.tensor, wc.offset, [list(wc.ap[0]), [0, 8], [TW, 16]])
                kb.op("pool", lambda e, w_=w_, wbc=wbc, tt_=tt_: e.tensor_tensor(
                    out=w_[:, tt_, :].rearrange("p (a b) -> p a b", b=16),
                    in0=bm[:].rearrange("p (a b) -> p a b", b=16), in1=wbc, op=ALU.mult),
                    reads=[bm, WT], writes=[w_])
            for tt_ in range(4):
                kb.mm(XP, XP[:, tt_ * 128:(tt_ + 1) * 128], w_, w_[:, tt_, :], o2, o2[:, tt_, :])
            kb.cp(xs, xs[:], XP, XP[:].rearrange("p (a b) -> p a b", b=128))
            for tt_ in range(4):
                kb.mm(GP, GP[:, tt_ * 128:(tt_ + 1) * 128], xs, xs[:, tt_, :], o1, o1[:, tt_, :])
            kb.op("act", lambda e, GP=GP, tq=tq: e.activation(out=GT[:, tq:tq + 4, :], in_=GP[:].rearrange("p (a b) -> p a b", b=128), func=AF.Copy),
                  reads=[GP], writes=[GT])
        for c in range(NCH):
            ub = u_bf[c % NUB]; vb = v_bf[c % NUB]
            kb.dma("sp", ub[:], uB[c], reads=[uBv[c]], writes=[ub])
            kb.dma("sp", vb[:], vB[c * 128:(c + 1) * 128, :], reads=[vBv[c]], writes=[vb])
            ap_ = P[4 + c % 2]
            for k in range(KC):
                kb.mm(ap_, ap_[:, :ncol], ub, ub[:, k, :], h2_bf, h2_bf[:, k, :ncol], start=(k == 0), stop=(k == KC - 1))
            g_ = gl[c % 2]; a_ = ga[c % 2]
            kb.act(g_, g_[:, :ncol], ap_, ap_[:, :ncol], AF.Gelu)
            kb.tt(a_, a_[:, :ncol], g_, g_[:, :ncol], GT, GT[:, :ncol, c], ALU.mult, eng="pool")
            for m in range(KC):
                op_ = P[m // 2]
                kb.mm(op_, op_[:, (m % 2) * 256:(m % 2) * 256 + ncol], vb, vb[:, m * 128:(m + 1) * 128], a_, a_[:, :ncol],
                      start=(c == 0 and m % 2 == 0), stop=(c == NCH - 1), skip=True)
        for m in range(KC):
            op_ = P[m // 2]
            o_ = ost[m % 2]
            if dbg:
                o2_ = dbgp
                kb.cp(o2_, o2_[:, :ncol], op_, op_[:, (m % 2) * 256:(m % 2) * 256 + ncol], eng="dve")
                kb.dma("sp", d_peer[m * 128:(m + 1) * 128, t0:t0 + ncol], o2_[:, :ncol], reads=[o2_], writes=[])
            kb.stt(o_, o_[:, :ncol], op_, op_[:, (m % 2) * 256:(m % 2) * 256 + ncol], mod[:, 24 + m, s:s + 1], xt, xt[:, m, :ncol],
                   ALU.mult, ALU.add, sr=[mod])
            kb.dma("sp", xoT[m * 128:(m + 1) * 128, t0:t0 + ncol], o_[:, :ncol], reads=[o_], writes=[])
    return kb


DEPTH = 4
B_ = 2
T_ = 16384
CTX_ = 256
NCORE = 8
NLAT_C = T_ // 4
NCTX_C = CTX_ // 4


def _lay_c(c_b, c_ctx):
    cT = np.zeros((128, 8, 2), np.float32)
    cT[:, :, 0] = c_b.reshape(8, 128).T
    cT[:, :, 1] = c_ctx.reshape(8, 128).T
    return cT


def _fm(v):
    return np.ascontiguousarray(np.asarray(v, np.float32).reshape(-1, 128).T)


def _dn_inputs(pb, j, conv_w, A_log, dt_bias, dn_norm_w, cm, TL=T_, TC=CTX_):
    PADW = TL + 4 + TC + 4
    qkvP = np.zeros((128, 3, PADW), np.float32)
    cwT = np.zeros((128, 3, 5), np.float32)
    for w in range(3):
        rows = slice(1024 + w * 512 + j * 128, 1024 + w * 512 + (j + 1) * 128)
        qkvP[:, w, 2:2 + TL] = pb[rows, :TL]
        qkvP[:, w, TL + 6:TL + 6 + TC] = pb[rows, TL:]
        cwT[:, w, :] = conv_w[:, w * 512 + j * 128: w * 512 + (j + 1) * 128].T
    gateT = np.ascontiguousarray(pb[2560 + j * 128: 2560 + (j + 1) * 128])
    baT = np.ascontiguousarray(pb[[3072 + j, 3076 + j, 3080 + j, 3084 + j]])
    cst = np.zeros((128, 8), np.float32)
    cst[:, 0] = A_log[0, j]; cst[:, 1] = A_log[1, j]; cst[:, 2] = dt_bias[0, j]; cst[:, 3] = dt_bias[1, j]
    cst[:, 4] = dn_norm_w
    return {"qkvP": qkvP, "gateT": gateT, "baT": baT, "cwT": cwT, "cst": cst, "cm": cm}


def kernel(x, c, ctx, c_ctx, ada_w, ada_b, norm1_w, norm2_w, w_in, attn_qnorm_w, attn_knorm_w,
           dn_conv_w, dn_A_log, dn_dt_bias, dn_norm_w, w_out, peer_wq, peer_subkeys, peer_u, peer_v):
    f32 = np.float32
    x = np.asarray(x, f32); ctx = np.asarray(ctx, f32); c = np.asarray(c, f32); c_ctx = np.asarray(c_ctx, f32)
    ada_w = np.asarray(ada_w, f32); ada_b = np.asarray(ada_b, f32)
    w_in = np.asarray(w_in, f32); w_out = np.asarray(w_out, f32)
    peer_wq = np.asarray(peer_wq, f32); peer_subkeys = np.asarray(peer_subkeys, f32)
    peer_u = np.asarray(peer_u, f32); peer_v = np.asarray(peer_v, f32)
    dn_conv_w = np.asarray(dn_conv_w, f32)

    XT = []
    for core in range(NCORE):
        b, r = core // 4, core % 4
        xs = np.concatenate([x[b, r * NLAT_C:(r + 1) * NLAT_C], ctx[b, r * NCTX_C:(r + 1) * NCTX_C]], axis=0)
        XT.append(np.ascontiguousarray(xs.T))
    cTs = [_lay_c(c[core // 4], c_ctx) for core in range(NCORE)]
    cos, sin = rope_tables()
    cosT = np.ascontiguousarray(cos.T); sinT = np.ascontiguousarray(sin.T)
    RmT = rot_matrix_T(); ident = np.eye(128, dtype=f32)
    cm = dn_consts(); bmc = peer_consts()

    for l in range(DEPTH):
        ada_bT = np.ascontiguousarray(ada_b[l].reshape(48, 128).T)
        kbA = build_A()
        adaA = np.ascontiguousarray(ada_w[l][:, :2048])
        n1T = _fm(norm1_w[l])
        resA = kbA.run([{"xT": XT[core], "cT": cTs[core], "ada_w": adaA, "ada_bT": ada_bT, "n1T": n1T, "w_in": w_in[l]}
                        for core in range(NCORE)]).results
        pbs = []
        for b in range(B_):
            cs = range(4 * b, 4 * b + 4)
            pbs.append(np.concatenate([resA[cc]["pT"][:, :NLAT_C] for cc in cs] + [resA[cc]["pT"][:, NLAT_C:] for cc in cs], axis=1))
        del resA
        kbB = build_attn()
        qkw = np.stack([np.asarray(attn_qnorm_w[l], f32), np.asarray(attn_knorm_w[l], f32)], axis=1).astype(f32)
        mapsB = []
        for core in range(NCORE):
            b, j = core // 4, core % 4
            pb = pbs[b]
            mapsB.append({"qT": np.ascontiguousarray(pb[j * 128:(j + 1) * 128]),
                          "kT": np.ascontiguousarray(pb[512 + (j // 2) * 128: 512 + (j // 2 + 1) * 128]),
                          "vT": np.ascontiguousarray(pb[768 + (j // 2) * 128: 768 + (j // 2 + 1) * 128]),
                          "cosT": cosT, "sinT": sinT, "RmT": RmT, "ident": ident, "qkw": qkw})
        resB = kbB.run(mapsB).results
        del mapsB
        kbD = build_dn()
        mapsD = [_dn_inputs(pbs[core // 4], core % 4, dn_conv_w[l], np.asarray(dn_A_log[l], f32),
                            np.asarray(dn_dt_bias[l], f32), np.asarray(dn_norm_w[l], f32), cm) for core in range(NCORE)]
        resD = kbD.run(mapsD).results
        del mapsD, pbs
        mixb = []
        for b in range(B_):
            mixb.append(np.concatenate([resB[4 * b + j]["oT"] for j in range(4)] + [resD[4 * b + j]["yT"] for j in range(4)], axis=0))
        del resB, resD
        last = (l == DEPTH - 1)
        kbC = build_C(NLAT=NLAT_C, NCTX=0 if last else NCTX_C)
        adaC = np.ascontiguousarray(ada_w[l][:, 2048:])
        n2T = _fm(norm2_w[l])
        subkT = np.ascontiguousarray(peer_subkeys[l].reshape(16, 128, 128).transpose(0, 2, 1))
        uTr = np.ascontiguousarray(peer_u[l].reshape(128, 128, 8, 128).transpose(0, 3, 2, 1))
        mapsC = []
        for core in range(NCORE):
            b, r = core // 4, core % 4
            if last:
                mx = mixb[b][:, r * NLAT_C:(r + 1) * NLAT_C]
                xin = np.ascontiguousarray(XT[core][:, :NLAT_C])
            else:
                mx = np.concatenate([mixb[b][:, r * NLAT_C:(r + 1) * NLAT_C], mixb[b][:, T_ + r * NCTX_C: T_ + (r + 1) * NCTX_C]], axis=1)
                xin = XT[core]
            mapsC.append({"xT": xin, "mixT": np.ascontiguousarray(mx), "cT": cTs[core], "ada_w": adaC, "ada_bT": ada_bT,
                          "n2T": n2T, "w_out": w_out[l], "wq": peer_wq[l], "subkT": subkT, "uTr": uTr, "vtab": peer_v[l],
                          "bm": bmc, "ident": ident})
        resC = kbC.run(mapsC).results
        del mapsC, mixb, uTr
        XT = [np.ascontiguousarray(resC[core]["xoT"]) for core in range(NCORE)]
        del resC

    out = np.empty((B_, T_, 1024), f32)
    for core in range(NCORE):
        b, r = core // 4, core % 4
        out[b, r * NLAT_C:(r + 1) * NLAT_C, :] = XT[core][:, :NLAT_C].T
    return out
```
